# Optimizing a Trainium2 kernel written in Bass

```python
import math
import jax, jax.numpy as jnp
from jax import lax
import numpy as np

D_MODEL = 4096
BATCH = 4
SEQ = 4096
DEPTH = 1

RW_HEADS = 32
RW_HEAD = 64
RW_WIDTH = RW_HEADS * RW_HEAD
RW_DECAY_LORA = 96
RW_A_LORA = 96
RW_GATE_LORA = 256
RW_GN_EPS = 64e-5
RW_DECAY_SCALE = 0.606531

DN_HEADS = 16
DN_HEAD_K = 128
DN_HEAD_V = 128
DN_KEY = DN_HEADS * DN_HEAD_K
DN_VAL = DN_HEADS * DN_HEAD_V
DN_QKV = 2 * DN_KEY + DN_VAL
DN_CONV = 4
DN_CHUNK = 64

N_BRANCH = 2
RW_COLS = 3 * RW_WIDTH + RW_DECAY_LORA + RW_A_LORA + RW_GATE_LORA
DN_COLS = DN_QKV + 2 * DN_HEADS + DN_VAL
GATE_COLS = N_BRANCH * D_MODEL
IN_COLS = RW_COLS + DN_COLS + GATE_COLS

N_GROUPS = 8
EXPERTS_PER_GROUP = 8
N_EXPERTS = N_GROUPS * EXPERTS_PER_GROUP
TOP_K = 2
D_EXPERT = 768
MOE_BLOCK = 128

NORM_EPS = 1e-6

kernel_name = 'hybrid_rwkv7_gdn_hmoe_block'


def _split(t, sizes):
    idx = np.cumsum(np.array(sizes))[:-1].tolist()
    return jnp.split(t, idx, axis=-1)


def _rmsnorm(x, g):
    xf = x.astype(jnp.float32)
    y = xf * lax.rsqrt(jnp.mean(xf * xf, axis=-1, keepdims=True) + NORM_EPS)
    return (y * g.astype(jnp.float32)).astype(x.dtype)


def _l2norm(t):
    return t * lax.rsqrt(jnp.sum(t * t, axis=-1, keepdims=True) + NORM_EPS)


def _rwkv7_time_mix(h, mu, w0, w2, a0, a2, g2, k_k, k_a, r_k, ln_w, ln_b):
    B, S, _ = h.shape
    f32 = jnp.float32
    hd = (RW_HEADS, RW_HEAD)
    h_prev = jnp.pad(h, ((0, 0), (1, 0), (0, 0)))[:, :-1]
    hm = h + (h_prev - h) * mu
    r, wd, k, v, ad, gd = _split(hm, (RW_WIDTH, RW_DECAY_LORA, RW_WIDTH, RW_WIDTH, RW_A_LORA, RW_GATE_LORA))
    log_w = -RW_DECAY_SCALE * jax.nn.sigmoid((w0 + jnp.tanh(wd) @ w2).astype(f32))
    a = jax.nn.sigmoid((a0 + ad @ a2).astype(f32))
    g = (jax.nn.sigmoid(gd) @ g2).astype(f32)
    heads = lambda t: t.astype(f32).reshape(B, S, RW_HEADS, RW_HEAD)
    r_h = heads(r)
    v_h = heads(v)
    a_h = heads(a)
    w_h = jnp.exp(heads(log_w))
    kk = _l2norm(heads(k) * k_k.astype(f32).reshape(hd))
    k_h = heads(k) * (1.0 + (a_h - 1.0) * k_a.astype(f32).reshape(hd))

    def step(state, inp):
        r_t, w_t, k_t, v_t, kk_t, a_t = inp
        sk = jnp.einsum('bhvk,bhk->bhv', state, kk_t)
        state = (state * w_t[:, :, None, :]
                 - sk[..., None] * (kk_t * a_t)[:, :, None, :]
                 + v_t[..., None] * k_t[:, :, None, :])
        return state, jnp.einsum('bhvk,bhk->bhv', state, r_t)

    tm = lambda t: jnp.moveaxis(t, 1, 0)
    s0 = jnp.zeros((B, RW_HEADS, RW_HEAD, RW_HEAD), f32)
    _, o = lax.scan(step, s0, (tm(r_h), tm(w_h), tm(k_h), tm(v_h), tm(kk), tm(a_h)))
    o = jnp.moveaxis(o, 0, 1)
    mean = jnp.mean(o, axis=-1, keepdims=True)
    var = jnp.mean(jnp.square(o - mean), axis=-1, keepdims=True)
    o = (o - mean) * lax.rsqrt(var + RW_GN_EPS) * ln_w.astype(f32).reshape(hd) + ln_b.astype(f32).reshape(hd)
    bonus = jnp.sum(r_h * k_h * r_k.astype(f32), axis=-1, keepdims=True) * v_h
    o = (o + bonus).reshape(B, S, RW_WIDTH) * g
    return o.astype(h.dtype)


def _chunk_gated_delta_rule(q, k, v, beta, g):
    B, S, H, DK = q.shape
    DV = v.shape[-1]
    C = DN_CHUNK
    NC = S // C

    def chunks(t):
        t = jnp.moveaxis(t, 2, 1)
        return t.reshape((B, H, NC, C) + t.shape[3:])

    q, k, v, beta, g = chunks(q), chunks(k), chunks(v), chunks(beta), chunks(g)
    G = jnp.cumsum(g, axis=-1)
    causal = jnp.tril(jnp.ones((C, C), bool))
    strict = jnp.tril(jnp.ones((C, C), bool), -1)
    diff = G[..., :, None] - G[..., None, :]
    decay = jnp.where(causal, jnp.exp(jnp.where(causal, diff, 0.0)), 0.0)
    kb = k * beta[..., None]
    Lmat = jnp.where(strict, jnp.einsum('bhnik,bhnjk->bhnij', kb, k) * decay, 0.0)
    M = Lmat + jnp.eye(C, dtype=Lmat.dtype)
    rhs = jnp.concatenate([v * beta[..., None], kb * jnp.exp(G)[..., None]], axis=-1)
    sol = lax.linalg.triangular_solve(M, rhs, left_side=True, lower=True, unit_diagonal=True)
    u, w = sol[..., :DV], sol[..., DV:]
    attn = jnp.einsum('bhnik,bhnjk->bhnij', q, k) * decay
    q_dec = q * jnp.exp(G)[..., None]
    G_last = G[..., -1:]
    k_tail = k * jnp.exp(G_last - G)[..., None]
    chunk_decay = jnp.exp(G_last)[..., None]

    def step(state, inp):
        u_c, w_c, qd_c, a_c, kt_c, cd_c = inp
        v_new = u_c - jnp.einsum('bhck,bhkv->bhcv', w_c, state)
        o_c = jnp.einsum('bhck,bhkv->bhcv', qd_c, state) + jnp.einsum('bhij,bhjv->bhiv', a_c, v_new)
        state = state * cd_c + jnp.einsum('bhck,bhcv->bhkv', kt_c, v_new)
        return state, o_c

    nm = lambda t: jnp.moveaxis(t, 2, 0)
    s0 = jnp.zeros((B, H, DK, DV), q.dtype)
    _, o = lax.scan(step, s0, (nm(u), nm(w), nm(q_dec), nm(attn), nm(k_tail), nm(chunk_decay)))
    o = jnp.moveaxis(o, 0, 2).reshape(B, H, S, DV)
    return jnp.moveaxis(o, 1, 2)


def _gated_deltanet(h, conv_w, a_log, dt_bias, norm_w):
    B, S, _ = h.shape
    f32 = jnp.float32
    qkv, b, al, z = _split(h, (DN_QKV, DN_HEADS, DN_HEADS, DN_VAL))
    qkv = lax.conv_general_dilated(qkv, conv_w[:, None, :], (1,), [(DN_CONV - 1, 0)],
                                   dimension_numbers=('NWC', 'WIO', 'NWC'),
                                   feature_group_count=DN_QKV)
    qkv = jax.nn.silu(qkv)
    q, k, v = _split(qkv, (DN_KEY, DN_KEY, DN_VAL))
    q = _l2norm(q.astype(f32).reshape(B, S, DN_HEADS, DN_HEAD_K)) * (DN_HEAD_K ** -0.5)
    k = _l2norm(k.astype(f32).reshape(B, S, DN_HEADS, DN_HEAD_K))
    v = v.astype(f32).reshape(B, S, DN_HEADS, DN_HEAD_V)
    beta = jax.nn.sigmoid(b.astype(f32))
    g = -jnp.exp(a_log.astype(f32)) * jax.nn.softplus(al.astype(f32) + dt_bias.astype(f32))
    o = _chunk_gated_delta_rule(q, k, v, beta, g)
    o = o * lax.rsqrt(jnp.mean(o * o, axis=-1, keepdims=True) + NORM_EPS) * norm_w.astype(f32)
    o = o * jax.nn.silu(z.astype(f32).reshape(B, S, DN_HEADS, DN_HEAD_V))
    return o.reshape(B, S, DN_VAL).astype(h.dtype)


def _hier_moe(h, gr_w, gr_b, er_w, er_b, w1, w3, w2):
    B, S, D = h.shape
    T = B * S
    A = T * TOP_K
    xt = h.reshape(T, D)
    gprob = jax.nn.softmax((xt @ gr_w + gr_b).astype(jnp.float32), axis=-1)
    gp, gi = lax.top_k(gprob, 1)
    elog_all = (xt @ er_w + er_b).astype(jnp.float32).reshape(T, N_GROUPS, EXPERTS_PER_GROUP)
    elog = elog_all[jnp.arange(T), gi[:, 0]]
    ep, ei = lax.top_k(jax.nn.softmax(elog, axis=-1), TOP_K)
    weight = gp * ep / jnp.sum(ep, axis=-1, keepdims=True)
    eid = gi * EXPERTS_PER_GROUP + ei

    e_flat = eid.reshape(A)
    w_flat = weight.reshape(A).astype(h.dtype)
    tok_flat = jnp.repeat(jnp.arange(T, dtype=jnp.int32), TOP_K)
    order = jnp.argsort(e_flat)
    se, stok, sw = e_flat[order], tok_flat[order], w_flat[order]
    counts = jnp.zeros((N_EXPERTS,), jnp.int32).at[e_flat].add(1)
    offs = jnp.cumsum(counts) - counts
    pcounts = (counts + MOE_BLOCK - 1) // MOE_BLOCK * MOE_BLOCK
    pend = jnp.cumsum(pcounts)
    poffs = pend - pcounts
    dest = poffs[se] + (jnp.arange(A, dtype=jnp.int32) - offs[se])
    P = A + N_EXPERTS * MOE_BLOCK
    n_blocks = P // MOE_BLOCK
    buf_tok = jnp.full((P,), T, jnp.int32).at[dest].set(stok)
    buf_w = jnp.zeros((P,), h.dtype).at[dest].set(sw)
    block_e = jnp.minimum(jnp.searchsorted(pend, jnp.arange(n_blocks) * MOE_BLOCK, side='right'),
                          N_EXPERTS - 1).astype(jnp.int32)
    xpad = jnp.concatenate([xt, jnp.zeros((1, D), xt.dtype)], axis=0)

    def block_fn(args):
        tok_b, w_b, e_b = args
        xb = xpad[tok_b]
        act = jax.nn.silu(xb @ w1[e_b]) * (xb @ w3[e_b])
        return (act @ w2[e_b]) * w_b[:, None]

    yb = lax.map(block_fn, (buf_tok.reshape(n_blocks, MOE_BLOCK),
                            buf_w.reshape(n_blocks, MOE_BLOCK), block_e))
    out = jnp.zeros((T + 1, D), h.dtype).at[buf_tok].add(yb.reshape(P, D))[:T]
    return out.reshape(B, S, D)


def setup_inputs(seed: int = 0) -> dict:
    key = jax.random.key(seed)
    ks = iter(jax.random.split(key, 40))
    f32 = jnp.float32
    L = DEPTH

    def nrm(shape, scale):
        return jax.random.normal(next(ks), shape, f32) * scale

    def unif(shape, lo, hi):
        return jax.random.uniform(next(ks), shape, f32, lo, hi)

    dt = jnp.exp(unif((L, DN_HEADS), math.log(1e-3), math.log(1e-1)))
    return {
        'x': nrm((BATCH, SEQ, D_MODEL), 1.0),
        'norm1_g': 1.0 + nrm((L, D_MODEL), 0.02),
        'w_in': nrm((L, D_MODEL, IN_COLS), D_MODEL ** -0.5),
        'rw_mu': unif((L, RW_COLS), 0.0, 1.0),
        'rw_w0': unif((L, RW_WIDTH), -3.0, 1.0),
        'rw_w2': nrm((L, RW_DECAY_LORA, RW_WIDTH), 0.1 * RW_DECAY_LORA ** -0.5),
        'rw_a0': nrm((L, RW_WIDTH), 0.5),
        'rw_a2': nrm((L, RW_A_LORA, RW_WIDTH), 0.5 * RW_A_LORA ** -0.5),
        'rw_g2': nrm((L, RW_GATE_LORA, RW_WIDTH), RW_GATE_LORA ** -0.5),
        'rw_k_k': 0.85 + nrm((L, RW_WIDTH), 0.05),
        'rw_k_a': 1.0 + nrm((L, RW_WIDTH), 0.05),
        'rw_r_k': nrm((L, RW_HEADS, RW_HEAD), 0.1),
        'rw_ln_w': 1.0 + nrm((L, RW_WIDTH), 0.02),
        'rw_ln_b': nrm((L, RW_WIDTH), 0.02),
        'dn_conv_w': nrm((L, DN_CONV, DN_QKV), DN_CONV ** -0.5),
        'dn_a_log': jnp.log(unif((L, DN_HEADS), 1.0, 16.0)),
        'dn_dt_bias': dt + jnp.log(-jnp.expm1(-dt)),
        'dn_norm_w': 1.0 + nrm((L, DN_HEAD_V), 0.02),
        'gate_b': nrm((L, GATE_COLS), 0.1),
        'w_branch': nrm((L, RW_WIDTH + DN_VAL, D_MODEL), RW_WIDTH ** -0.5),
        'w_out': nrm((L, D_MODEL, D_MODEL), D_MODEL ** -0.5),
        'norm2_g': 1.0 + nrm((L, D_MODEL), 0.02),
        'moe_gr_w': nrm((L, D_MODEL, N_GROUPS), D_MODEL ** -0.5),
        'moe_gr_b': nrm((L, N_GROUPS), 0.01),
        'moe_er_w': nrm((L, D_MODEL, N_EXPERTS), D_MODEL ** -0.5),
        'moe_er_b': nrm((L, N_EXPERTS), 0.01),
        'moe_w1': nrm((L, N_EXPERTS, D_MODEL, D_EXPERT), D_MODEL ** -0.5),
        'moe_w3': nrm((L, N_EXPERTS, D_MODEL, D_EXPERT), D_MODEL ** -0.5),
        'moe_w2': nrm((L, N_EXPERTS, D_EXPERT, D_MODEL), D_EXPERT ** -0.5),
        'final_g': 1.0 + nrm((D_MODEL,), 0.02),
    }


def reference(x, norm1_g, w_in, rw_mu, rw_w0, rw_w2, rw_a0, rw_a2, rw_g2, rw_k_k, rw_k_a,
              rw_r_k, rw_ln_w, rw_ln_b, dn_conv_w, dn_a_log, dn_dt_bias, dn_norm_w, gate_b,
              w_branch, w_out, norm2_g, moe_gr_w, moe_gr_b, moe_er_w, moe_er_b,
              moe_w1, moe_w3, moe_w2, final_g):
    for l in range(DEPTH):
        h = _rmsnorm(x, norm1_g[l])
        proj = jnp.einsum('bsd,dc->bsc', h, w_in[l])
        p_rw, p_dn, p_gate = _split(proj, (RW_COLS, DN_COLS, GATE_COLS))
        o_rw = _rwkv7_time_mix(p_rw, rw_mu[l], rw_w0[l], rw_w2[l], rw_a0[l], rw_a2[l], rw_g2[l],
                               rw_k_k[l], rw_k_a[l], rw_r_k[l], rw_ln_w[l], rw_ln_b[l])
        o_dn = _gated_deltanet(p_dn, dn_conv_w[l], dn_a_log[l], dn_dt_bias[l], dn_norm_w[l])
        y_rw = jnp.einsum('bsc,cd->bsd', o_rw, w_branch[l, :RW_WIDTH])
        y_dn = jnp.einsum('bsc,cd->bsd', o_dn, w_branch[l, RW_WIDTH:])
        g_rw, g_dn = _split(jax.nn.sigmoid(p_gate + gate_b[l]), (D_MODEL, D_MODEL))
        x = x + jnp.einsum('bsd,de->bse', g_rw * y_rw + g_dn * y_dn, w_out[l])
        x = x + _hier_moe(_rmsnorm(x, norm2_g[l]), moe_gr_w[l], moe_gr_b[l], moe_er_w[l],
                          moe_er_b[l], moe_w1[l], moe_w3[l], moe_w2[l])
    return _rmsnorm(x, final_g)
```

```python
import os
import numpy as np
from contextlib import ExitStack
KSTOP = float(os.environ.get('KSTOP', '99'))
import concourse.bass as bass
import concourse.mybir as mybir
from concourse.bass_utils import run_bass_kernel_spmd

F32 = mybir.dt.float32
BF16 = mybir.dt.bfloat16
I32 = mybir.dt.int32
U32 = mybir.dt.uint32
AF = mybir.ActivationFunctionType
ALU = mybir.AluOpType
AX = mybir.AxisListType

CH = 64


class Cfg:
    def __init__(self, D=4096, S=4096, RWH=32, DNH=16, NG=8, DE=768, SEG=512):
        self.D, self.S, self.RWH, self.DNH, self.NG, self.DE = D, S, RWH, DNH, NG, DE
        self.KD = D // 128
        self.RW = RWH * 64
        self.DN = DNH * 128
        self.BW = self.RW + self.DN
        self.KB = self.BW // 128
        self.OWN = S // 2
        self.NE = NG * 8
        self.TG = min(1024, S)
        self.TGO = min(512, self.OWN)
        self.SEG = min(SEG, S)
        self.KP = min(8, self.KD)
        self.RW_COLS = 3 * self.RW + 448
        self.DN_COLS = 3 * self.DN + 2 * DNH + self.DN
        self.GB0 = self.RW_COLS + self.DN_COLS
        self.IN_COLS = self.GB0 + 2 * D
        rows = {}
        o = 0
        for name, n in (("r", self.RW), ("k", self.RW), ("v", self.RW), ("wd", 128), ("ad", 128), ("gd", 256),
                        ("dq", self.DN), ("dk", self.DN), ("dv", self.DN), ("ba", 128), ("z", self.DN)):
            rows[name] = o
            o += n
        self.rows = rows
        self.NA = o // 128
        self.NBLK = (2 * self.OWN + self.NE * 128) // 128
        self.FE = DE // 128

    def colsA(self):
        c = self
        RW, DN, DNH = c.RW, c.DN, c.DNH
        idx = -np.ones(c.NA * 128, np.int64)
        r = c.rows
        idx[r["r"]:r["r"] + RW] = np.arange(0, RW)
        idx[r["wd"]:r["wd"] + 96] = np.arange(RW, RW + 96)
        idx[r["k"]:r["k"] + RW] = np.arange(RW + 96, 2 * RW + 96)
        idx[r["v"]:r["v"] + RW] = np.arange(2 * RW + 96, 3 * RW + 96)
        idx[r["ad"]:r["ad"] + 96] = np.arange(3 * RW + 96, 3 * RW + 192)
        idx[r["gd"]:r["gd"] + 256] = np.arange(3 * RW + 192, 3 * RW + 448)
        b0 = c.RW_COLS
        idx[r["dq"]:r["dq"] + DN] = b0 + np.arange(0, DN)
        idx[r["dk"]:r["dk"] + DN] = b0 + np.arange(DN, 2 * DN)
        idx[r["dv"]:r["dv"] + DN] = b0 + np.arange(2 * DN, 3 * DN)
        idx[r["ba"]:r["ba"] + DNH] = b0 + 3 * DN + np.arange(0, DNH)
        idx[r["ba"] + 32:r["ba"] + 32 + DNH] = b0 + 3 * DN + DNH + np.arange(0, DNH)
        idx[r["z"]:r["z"] + DN] = b0 + 3 * DN + 2 * DNH + np.arange(0, DN)
        return idx


class Buf:
    __slots__ = ("w", "r", "x")

    def __init__(self, excl=False):
        self.w = {}
        self.r = {}
        self.x = excl


class Sched:
    def __init__(self, nc, es, n_dma_sems=24):
        self.nc = nc
        self.eng = {"pe": nc.tensor, "act": nc.scalar, "dve": nc.vector, "pool": nc.gpsimd, "sp": nc.sync}
        self.sems = []
        self.semidx = {}
        self.cnt = {}
        for k in ("pe", "act", "dve", "pool"):
            s = es.enter_context(nc.semaphore("s_" + k))
            self.semidx[k] = len(self.sems)
            self.sems.append(s)
            self.cnt[k] = 0
        self.dma_ids = []
        for i in range(n_dma_sems):
            s = es.enter_context(nc.semaphore("s_d%d" % i))
            self.dma_ids.append(len(self.sems))
            self.sems.append(s)
        self.dtotal = {i: 0 for i in self.dma_ids}
        self.dnext = 0
        self.known = {k: {} for k in self.eng}
        self.ninstr = 0
        self.tick = None

    def _wait(self, ek, need):
        kn = self.known[ek]
        e = self.eng[ek]
        own = self.semidx.get(ek)
        for s, v in need.items():
            if ek == "pe" and s == own:
                continue
            if kn.get(s, 0) < v:
                e.wait_ge(self.sems[s], v)
                kn[s] = v
                self.ninstr += 1

    def _deps(self, ek, reads, writes):
        need = {}
        for b in reads:
            if b.x:
                for s, v in b.r.items():
                    if need.get(s, 0) < v:
                        need[s] = v
            for s, v in b.w.items():
                if need.get(s, 0) < v:
                    need[s] = v
        for b in writes:
            for s, v in b.w.items():
                if need.get(s, 0) < v:
                    need[s] = v
            for s, v in b.r.items():
                if need.get(s, 0) < v:
                    need[s] = v
        self._wait(ek, need)

    def op(self, ek, fn, reads=(), writes=()):
        if self.tick is not None:
            self.tick()
        self._deps(ek, reads, writes)
        ins = fn(self.eng[ek])
        self.cnt[ek] += 1
        s = self.semidx[ek]
        ins.then_inc(self.sems[s], 1)
        v = self.cnt[ek]
        for b in reads:
            b.r[s] = v
            if b.x:
                b.w[s] = v
        for b in writes:
            b.w[s] = v
        self.ninstr += 1
        return ins

    def dma(self, qk, fn, reads=(), writes=()):
        if self.tick is not None:
            self.tick()
        self._deps(qk, reads, writes)
        s = self.dma_ids[self.dnext]
        self.dnext = (self.dnext + 1) % len(self.dma_ids)
        prev = self.dtotal[s]
        kn = self.known[qk]
        if prev and kn.get(s, 0) < prev:
            self.eng[qk].wait_ge(self.sems[s], prev)
            kn[s] = prev
        ins = fn(self.eng[qk])
        ins.then_inc(self.sems[s], 16)
        v = prev + 16
        self.dtotal[s] = v
        for b in reads:
            b.r[s] = v
        for b in writes:
            b.w[s] = v
        self.ninstr += 1
        return ins

    def barrier(self):
        need = {self.semidx[k]: self.cnt[k] for k in self.cnt if self.cnt[k]}
        for s, v in self.dtotal.items():
            if v:
                need[s] = v
        for ek in self.eng:
            self._wait(ek, dict(need))


import threading
_tl = threading.local()


def run_pair(S, f0, f1, quota=(1, 3)):
    if f0 is None:
        return f1()
    if f1 is None:
        return f0()
    st = {"turn": 0, "alive": [True, True], "used": 0, "err": None}
    cv = threading.Condition()

    def tick():
        tid = _tl.tid
        with cv:
            st["used"] += 1
            if st["used"] >= quota[tid] and st["alive"][1 - tid]:
                st["used"] = 0
                st["turn"] = 1 - tid
                cv.notify_all()
                while st["turn"] != tid:
                    cv.wait()

    def worker(tid, f):
        _tl.tid = tid
        with cv:
            while st["turn"] != tid:
                cv.wait()
        try:
            f()
        except BaseException as e:
            st["err"] = e
        finally:
            with cv:
                st["alive"][tid] = False
                st["used"] = 0
                st["turn"] = 1 - tid
                cv.notify_all()
    S.tick = tick
    ths = [threading.Thread(target=worker, args=(i, f)) for i, f in enumerate((f0, f1))]
    for t in ths:
        t.start()
    for t in ths:
        t.join()
    S.tick = None
    if st["err"] is not None:
        raise st["err"]


class K:
    def __init__(self, cfg, debug=()):
        self.cfg = cfg
        self.debug = set(debug)
        self.nc = bass.Bass("TRN2", target_bir_lowering=False)
        self.es = ExitStack()
        self.S = None
        self.din = {}

    def inp(self, name, shape, dt=F32):
        t = self.nc.dram_tensor(name, list(shape), dt, kind="ExternalInput").ap()
        self.din[name] = t
        return t

    def scratch(self, name, shape, dt=F32):
        kind = "ExternalOutput" if name in self.debug else "Internal"
        return self.nc.dram_tensor(name, list(shape), dt, kind=kind).ap()

    def sb(self, es, name, shape, dt=F32):
        return es.enter_context(self.nc.sbuf_tensor(name, list(shape), dt))

    def ps(self, es, name, shape, dt=F32):
        return es.enter_context(self.nc.psum_tensor(name, list(shape), dt))


def build_consts(kb, es):
    S, nc = kb.S, kb.nc
    c = {}
    ident = kb.sb(es, "ident", [128, 128]); b = Buf()
    S.op("pool", lambda e: e.memset(ident[:], 0.0), writes=[b])
    S.op("pool", lambda e: e.affine_select(out=ident[:], in_=ident[:], pattern=[[-1, 128]], compare_op=ALU.not_equal,
                                           fill=1.0, base=0, channel_multiplier=1), reads=[b], writes=[b])
    identb = kb.sb(es, "identb", [128, 128], BF16)
    S.op("dve", lambda e: e.tensor_copy(out=identb[:], in_=ident[:]), reads=[b], writes=[b])
    c["ident"], c["identb"], c["b_ident"] = ident, identb, b
    col = kb.sb(es, "ccol", [128, 8]); bc = Buf()
    for i, v in enumerate((1e-6, 64e-5, 1.0, 0.0, -1.0)):
        S.op("pool", lambda e: e.memset(col[:, i:i + 1], v), writes=[bc])
    c["col"], c["b_col"] = col, bc
    c["EPS6"], c["GNEPS"], c["ONE"], c["ZERO"] = col[:, 0:1], col[:, 1:2], col[:, 2:3], col[:, 3:4]
    return c


def rmsnorm_rstd(kb, c, ss, b_ss, n, nparts=128):
    S = kb.S
    S.op("dve", lambda e: e.tensor_scalar(out=ss, in0=ss, scalar1=1.0 / n, scalar2=1e-6, op0=ALU.mult, op1=ALU.add),
         reads=[b_ss], writes=[b_ss])
    S.op("act", lambda e: e.activation(out=ss, in_=ss, func=AF.Ln), reads=[b_ss], writes=[b_ss])
    S.op("act", lambda e: e.activation(out=ss, in_=ss, func=AF.Exp, scale=-0.5), reads=[b_ss], writes=[b_ss])


def phase_A(kb, c, xb, g1d, wA, PT, b_PT):
    cfg, S, nc = kb.cfg, kb.S, kb.nc
    D, KD, TG, NA = cfg.D, cfg.KD, cfg.TG, cfg.NA
    with ExitStack() as es:
        hT = kb.sb(es, "A_hT", [128, KD, TG], BF16); b_hT = Buf()
        xt = kb.sb(es, "A_xt", [128, D]); b_xt = Buf()
        hb = kb.sb(es, "A_hb", [128, D], BF16); b_hb = Buf()
        ss = kb.sb(es, "A_ss", [128, 1]); b_ss = Buf()
        g1 = kb.sb(es, "A_g1", [128, D]); b_g1 = Buf()
        slf = [kb.sb(es, "A_slf%d" % i, [128, KD, 128]) for i in range(2)]; b_slf = [Buf(), Buf()]
        slb = [kb.sb(es, "A_slb%d" % i, [128, KD, 128], BF16) for i in range(2)]; b_slb = [Buf(), Buf()]
        stg = [kb.sb(es, "A_stg%d" % i, [128, TG]) for i in range(2)]; b_stg = [Buf(), Buf()]
        psT = [kb.ps(es, "A_psT%d" % i, [128, 1024], BF16) for i in range(2)]; b_psT = [Buf(True), Buf(True)]
        psA = [kb.ps(es, "A_psA%d" % i, [128, 512]) for i in range(4)]; b_psA = [Buf(True) for _ in range(4)]
        S.dma("sp", lambda e: e.dma_start(out=g1[:], in_=g1d.partition_broadcast(128)), writes=[b_g1])
        it = 0
        pcnt = 0
        for tg in range(cfg.S // TG):
            for ti in range(TG // 128):
                t0 = tg * TG + ti * 128
                S.dma("sp", lambda e: e.dma_start(out=xt[:], in_=xb[t0:t0 + 128, :]), writes=[b_xt])
                S.op("act", lambda e: e.activation(out=hb[:], in_=xt[:], func=AF.Square, accum_out=ss[:]),
                     reads=[b_xt], writes=[b_hb, b_ss])
                rmsnorm_rstd(kb, c, ss[:], b_ss, D)
                S.op("dve", lambda e: e.scalar_tensor_tensor(out=hb[:], in0=xt[:], scalar=ss[:, 0:1], in1=g1[:],
                                                             op0=ALU.mult, op1=ALU.mult),
                     reads=[b_xt, b_ss, b_g1], writes=[b_hb])
                for k0 in range(0, KD, 8):
                    kn = min(8, KD - k0)
                    p = psT[pcnt % 2]; bp = b_psT[pcnt % 2]; pcnt += 1
                    for k in range(kn):
                        S.op("pe", lambda e: e.transpose(out=p[:, k * 128:(k + 1) * 128],
                                                         in_=hb[:, (k0 + k) * 128:(k0 + k + 1) * 128],
                                                         identity=c["identb"][:]),
                             reads=[b_hb, c["b_ident"]], writes=[bp])
                    ek = "act" if (pcnt % 2) else "dve"
                    src = p[:, 0:kn * 128].rearrange("p (a b) -> p a b", a=kn)
                    dst = hT[:, k0:k0 + kn, ti * 128:(ti + 1) * 128]
                    if ek == "act":
                        S.op("act", lambda e: e.copy(out=dst, in_=src), reads=[bp], writes=[b_hT])
                    else:
                        S.op("dve", lambda e: e.tensor_copy(out=dst, in_=src), reads=[bp], writes=[b_hT])
            for j in range(NA):
                sl = it % 2
                S.dma("sp", lambda e: e.dma_start(out=slf[sl][:], in_=wA[j]), writes=[b_slf[sl]])
                if it % 2 == 0:
                    S.op("act", lambda e: e.copy(out=slb[sl][:], in_=slf[sl][:]), reads=[b_slf[sl]], writes=[b_slb[sl]])
                else:
                    S.op("dve", lambda e: e.tensor_copy(out=slb[sl][:], in_=slf[sl][:]), reads=[b_slf[sl]], writes=[b_slb[sl]])
                for hf in range(TG // 512 if TG >= 512 else 1):
                    w = min(512, TG)
                    p = psA[pcnt % 4]; bp = b_psA[pcnt % 4]; pcnt += 1
                    for k in range(KD):
                        S.op("pe", lambda e: e.matmul(p[:, 0:w], lhsT=slb[sl][:, k, :], rhs=hT[:, k, hf * w:(hf + 1) * w],
                                                      start=(k == 0), stop=(k == KD - 1)),
                             reads=[b_slb[sl], b_hT], writes=[bp])
                    if pcnt % 2:
                        S.op("dve", lambda e: e.tensor_copy(out=stg[sl][:, hf * w:(hf + 1) * w], in_=p[:, 0:w]),
                             reads=[bp], writes=[b_stg[sl]])
                    else:
                        S.op("act", lambda e: e.copy(out=stg[sl][:, hf * w:(hf + 1) * w], in_=p[:, 0:w]),
                             reads=[bp], writes=[b_stg[sl]])
                S.dma("pool", lambda e: e.dma_start(out=PT[j * 128:(j + 1) * 128, tg * TG:(tg + 1) * TG], in_=stg[sl][:]),
                      reads=[b_stg[sl]], writes=[b_PT[j]])
                it += 1
    S.barrier()


RW_DECAY_SCALE = 0.606531


def build_masks(kb, es, c):
    S = kb.S
    m4 = kb.sb(es, "m4", [128, 128]); b = Buf()
    S.op("pool", lambda e: e.memset(m4[0:64, 0:64], 0.0), writes=[b])
    for (r0, c0, val, op) in ((0, 64, -1.0, ALU.is_ge), (64, 0, 1.0, ALU.is_gt), (64, 64, 1.0, ALU.is_ge)):
        blk = m4[r0:r0 + 64, c0:c0 + 64]
        S.op("pool", lambda e: e.memset(blk, val), writes=[b])
        S.op("pool", lambda e: e.affine_select(out=blk, in_=blk, pattern=[[1, 64]], compare_op=op, fill=0.0, base=0,
                                               channel_multiplier=-1), reads=[b], writes=[b])
    ml = kb.sb(es, "ml", [64, 64])
    S.op("pool", lambda e: e.memset(ml[:], 1.0), writes=[b])
    S.op("pool", lambda e: e.affine_select(out=ml[:], in_=ml[:], pattern=[[-1, 64]], compare_op=ALU.is_gt, fill=0.0, base=0,
                                           channel_multiplier=1), reads=[b], writes=[b])
    mup = kb.sb(es, "mup", [64, 64])
    S.op("pool", lambda e: e.memset(mup[:], 1.0), writes=[b])
    S.op("pool", lambda e: e.affine_select(out=mup[:], in_=mup[:], pattern=[[1, 64]], compare_op=ALU.is_gt, fill=0.0, base=0,
                                           channel_multiplier=-1), reads=[b], writes=[b])
    ones = kb.sb(es, "ones", [128, 128])
    S.op("pool", lambda e: e.memset(ones[:], 1.0), writes=[b])
    mneg = kb.sb(es, "mneg", [128, 128])
    S.op("pool", lambda e: e.memset(mneg[:], 0.0), writes=[b])
    for (r0, c0, op) in ((0, 0, ALU.is_gt), (0, 64, ALU.is_ge), (64, 0, ALU.is_gt), (64, 64, ALU.is_ge)):
        blk = mneg[r0:r0 + 64, c0:c0 + 64]
        S.op("pool", lambda e: e.affine_select(out=blk, in_=blk, pattern=[[1, 64]], compare_op=op, fill=-30000.0, base=0,
                                               channel_multiplier=-1), reads=[b], writes=[b])
    sgn = kb.sb(es, "sgn", [128, 1])
    S.op("pool", lambda e: e.memset(sgn[0:64, :], -1.0), writes=[b])
    S.op("pool", lambda e: e.memset(sgn[64:128, :], 1.0), writes=[b])
    c.update(m4=m4, ml=ml, mup=mup, ones=ones, sgn=sgn, mneg=mneg, b_mask=b)


class T:
    def __init__(self, ap, excl=False, b=None):
        self.ap = ap
        self.b = b if b is not None else Buf(excl)

    def __getitem__(self, k):
        return self.ap[k]


def dplr_segment(kb, c, u, Kd, NCH, P):
    S = kb.S
    V = Kd
    QR, BK, BKe, VV, wC, H, OT = (u[k] for k in ("QR", "BK", "BKe", "VV", "wC", "H", "OT"))
    Hb = u["Hb"]
    G4, A, AT, Y, YT, TT, Xp, Up, WmT, KKt, KBe, UV = (P[k] for k in ("G4", "A", "AT", "Y", "YT", "TT", "Xp", "Up", "WmT", "KKt", "KBe", "UV"))
    pg, pinv, pm, pseq = P["pg"], P["pinv"], P["pm"], P["pseq"]
    m4, ml, ident, sgn, bm, bi = c["m4"], c["ml"], c["identb"], c["sgn"], c["b_mask"], c["b_ident"]
    pmb = T(pm.ap.bitcast(BF16), b=pm.b)
    pAb = T(pinv[0].ap.bitcast(BF16), b=pinv[0].b)
    safe = "QRg" in u
    QRg = u["QRg"] if safe else QR
    for c0 in range(0, NCH, 4):
        n = min(4, NCH - c0)
        for i in range(n):
            S.op("pe", lambda e: e.matmul(pg[:, i * 128:(i + 1) * 128], lhsT=BK[0:Kd, c0 + i, :], rhs=QRg[0:Kd, c0 + i, :],
                                          start=True, stop=True), reads=[BK.b, QRg.b], writes=[pg.b])
        S.op("dve", lambda e: e.tensor_tensor(out=G4[:, c0:c0 + n, :], in0=pg[:, 0:n * 128].rearrange("p (a b) -> p a b", a=n),
                                              in1=m4[:].unsqueeze(1).to_broadcast([128, n, 128]), op=ALU.mult),
             reads=[pg.b, bm], writes=[G4.b])
        S.op("dve", lambda e: e.tensor_tensor(out=AT[0:64, c0:c0 + n, :], in0=pg[0:64, 0:n * 128].rearrange("p (a b) -> p a b", a=n)[:, :, 0:64],
                                              in1=c["mup"][:].unsqueeze(1).to_broadcast([64, n, 64]), op=ALU.mult),
             reads=[pg.b, bm], writes=[AT.b])
        if safe:
            pe_ = pinv[1]
            NA, E, nones = u["NA"], u["E"], u["nones"]
            ones = c["ones"]
            for i in range(n):
                cc_ = c0 + i
                for hf in range(2):
                    o_ = i * 128 + hf * 64
                    S.op("pe", lambda e: e.matmul(pe_[:, o_:o_ + 64], lhsT=ones[0:1, 0:128], rhs=NA[0:1, cc_, hf * 64:(hf + 1) * 64],
                                                  start=True, stop=False), reads=[NA.b, bm], writes=[pe_.b])
                    S.op("pe", lambda e: e.matmul(pe_[:, o_:o_ + 64], lhsT=NA[0:1, cc_, :], rhs=nones[0:1, 0:64],
                                                  start=False, stop=True), reads=[NA.b, nones.b], writes=[pe_.b])
            S.op("dve", lambda e: e.tensor_tensor(out=E[:, c0:c0 + n, :], in0=pe_[:, 0:n * 128].rearrange("p (a b) -> p a b", a=n),
                                                  in1=c["mneg"][:].unsqueeze(1).to_broadcast([128, n, 128]), op=ALU.add),
                 reads=[pe_.b, bm], writes=[E.b])
            S.op("act", lambda e: e.activation(out=E[:, c0:c0 + n, :], in_=E[:, c0:c0 + n, :], func=AF.Exp), reads=[E.b], writes=[E.b])
            S.op("pool", lambda e: e.tensor_tensor(out=G4[:, c0:c0 + n, :], in0=G4[:, c0:c0 + n, :], in1=E[:, c0:c0 + n, :], op=ALU.mult),
                 reads=[G4.b, E.b], writes=[G4.b])
            S.op("pool", lambda e: e.tensor_tensor(out=AT[0:64, c0:c0 + n, :], in0=AT[0:64, c0:c0 + n, :], in1=E[0:64, c0:c0 + n, 0:64], op=ALU.mult),
                 reads=[AT.b, E.b], writes=[AT.b])
    pA = pinv[0]
    if safe:
        for i in range(NCH):
            S.op("pe", lambda e: e.transpose(out=pAb[0:64, i * 64:(i + 1) * 64], in_=AT[0:64, i, :], identity=ident[0:64, 0:64]),
                 reads=[AT.b, bi], writes=[pA.b])
        S.op("dve", lambda e: e.tensor_copy(out=A[0:64, :, :], in_=pAb[0:64, 0:NCH * 64].rearrange("p (a b) -> p a b", a=NCH)),
             reads=[pA.b], writes=[A.b])
    else:
        for i in range(NCH):
            S.op("pe", lambda e: e.matmul(pA[0:64, i * 64:(i + 1) * 64], lhsT=QR[0:Kd, i, 0:64], rhs=BK[0:Kd, i, 0:64],
                                          start=True, stop=True), reads=[QR.b, BK.b], writes=[pA.b])
        S.op("dve", lambda e: e.tensor_tensor(out=A[0:64, :, :], in0=pA[0:64, 0:NCH * 64].rearrange("p (a b) -> p a b", a=NCH),
                                              in1=ml[:].unsqueeze(1).to_broadcast([64, NCH, 64]), op=ALU.mult),
             reads=[pA.b, bm], writes=[A.b])
    S.op("dve", lambda e: e.scalar_tensor_tensor(out=TT[0:64, :, :], in0=AT[0:64, :, :], scalar=-1.0,
                                                 in1=c["ident"][0:64, 0:64].unsqueeze(1).to_broadcast([64, NCH, 64]),
                                                 op0=ALU.mult, op1=ALU.add), reads=[AT.b, bi], writes=[TT.b])
    Yc, YTc = A, AT
    for lvl in range(5):
        Yn, YTn = Y[lvl % 2], YT[lvl % 2]
        pY, pYT, pT_ = pinv[0], pinv[1], pinv[2]
        for i in range(NCH):
            S.op("pe", lambda e: e.matmul(pY[0:64, i * 64:(i + 1) * 64], lhsT=YTc[0:64, i, :], rhs=Yc[0:64, i, :],
                                          start=True, stop=True), reads=[YTc.b, Yc.b], writes=[pY.b])
        S.op("act", lambda e: e.copy(out=Yn[0:64, :, :], in_=pY[0:64, 0:NCH * 64].rearrange("p (a b) -> p a b", a=NCH)),
             reads=[pY.b], writes=[Yn.b])
        if lvl < 4:
            for i in range(NCH):
                S.op("pe", lambda e: e.matmul(pYT[0:64, i * 64:(i + 1) * 64], lhsT=Yc[0:64, i, :], rhs=YTc[0:64, i, :],
                                              start=True, stop=True), reads=[YTc.b, Yc.b], writes=[pYT.b])
            S.op("dve", lambda e: e.tensor_copy(out=YTn[0:64, :, :], in_=pYT[0:64, 0:NCH * 64].rearrange("p (a b) -> p a b", a=NCH)),
                 reads=[pYT.b], writes=[YTn.b])
        for i in range(NCH):
            S.op("pe", lambda e: e.matmul(pT_[0:64, i * 64:(i + 1) * 64], lhsT=Yn[0:64, i, :], rhs=TT[0:64, i, :],
                                          start=True, stop=True), reads=[Yn.b, TT.b], writes=[pT_.b])
        S.op("dve", lambda e: e.tensor_tensor(out=TT[0:64, :, :], in0=TT[0:64, :, :],
                                              in1=pT_[0:64, 0:NCH * 64].rearrange("p (a b) -> p a b", a=NCH), op=ALU.add),
             reads=[pT_.b, TT.b], writes=[TT.b])
        Yc, YTc = Yn, YTn
    if KSTOP < 4:
        return
    nb = 512 // V
    for c0 in range(0, NCH, nb):
        n = min(nb, NCH - c0)
        for i in range(n):
            S.op("pe", lambda e: e.transpose(out=pmb[:, i * V:(i + 1) * V], in_=VV[0:Kd, c0 + i, :], identity=ident[0:Kd, 0:Kd]),
                 reads=[VV.b, bi], writes=[pm.b])
        S.op("act", lambda e: e.copy(out=UV[64:128, c0:c0 + n, 0:V], in_=pmb[64:128, 0:n * V].rearrange("p (a b) -> p a b", a=n)),
             reads=[pm.b], writes=[UV.b])
        if KSTOP < 4.15:
            continue
        for i in range(n):
            S.op("pe", lambda e: e.transpose(out=pmb[:, i * V:(i + 1) * V], in_=BKe[0:Kd, c0 + i, :], identity=ident[0:Kd, 0:Kd]),
                 reads=[BKe.b, bi], writes=[pm.b])
        S.op("dve", lambda e: e.tensor_scalar(out=KBe[:, c0:c0 + n, 0:Kd], in0=pmb[:, 0:n * V].rearrange("p (a b) -> p a b", a=n),
                                              scalar1=sgn[:, 0:1], scalar2=None, op0=ALU.mult), reads=[pm.b, bm], writes=[KBe.b])
        if KSTOP < 4.25:
            continue
        for i in range(n):
            S.op("pe", lambda e: e.transpose(out=pmb[0:64, i * V:(i + 1) * V], in_=QR[0:Kd, c0 + i, 0:64], identity=ident[0:Kd, 0:Kd]),
                 reads=[QR.b, bi], writes=[pm.b])
        S.op("act", lambda e: e.copy(out=KKt[0:64, c0:c0 + n, 0:Kd], in_=pmb[0:64, 0:n * V].rearrange("p (a b) -> p a b", a=n)),
             reads=[pm.b], writes=[KKt.b])
        if KSTOP < 4.35:
            continue
        for i in range(n):
            S.op("pe", lambda e: e.matmul(pm[0:64, i * V:(i + 1) * V], lhsT=G4[:, c0 + i, 0:64], rhs=UV[:, c0 + i, 0:V],
                                          start=True, stop=True), reads=[G4.b, UV.b], writes=[pm.b])
        S.op("dve", lambda e: e.tensor_copy(out=Xp[0:64, c0:c0 + n, 0:V], in_=pm[0:64, 0:n * V].rearrange("p (a b) -> p a b", a=n)),
             reads=[pm.b], writes=[Xp.b])
        if KSTOP < 4.45:
            continue
        for i in range(n):
            S.op("pe", lambda e: e.matmul(pm[0:64, i * V:(i + 1) * V], lhsT=TT[0:64, c0 + i, :], rhs=Xp[0:64, c0 + i, 0:V],
                                          start=True, stop=True), reads=[TT.b, Xp.b], writes=[pm.b])
        S.op("act", lambda e: e.copy(out=Up[0:64, c0:c0 + n, 0:V], in_=pm[0:64, 0:n * V].rearrange("p (a b) -> p a b", a=n)),
             reads=[pm.b], writes=[Up.b])
    if KSTOP < 4.55:
        return
    for i in range(NCH):
        S.op("pe", lambda e: e.matmul(pm[0:Kd, i * 64:(i + 1) * 64], lhsT=KKt[0:64, i, 0:Kd], rhs=TT[0:64, i, :],
                                      start=True, stop=True), reads=[KKt.b, TT.b], writes=[pm.b])
    S.op("dve", lambda e: e.tensor_copy(out=WmT[0:Kd, :, :], in_=pm[0:Kd, 0:NCH * 64].rearrange("p (a b) -> p a b", a=NCH)),
         reads=[pm.b], writes=[WmT.b])
    if KSTOP < 5:
        return
    pU, pO, pH = pseq
    for i in range(NCH):
        S.op("pe", lambda e: e.matmul(pU[0:64, 0:V], lhsT=WmT[0:Kd, i, :], rhs=Hb[0:Kd, :], start=True, stop=True),
             reads=[WmT.b, Hb.b], writes=[pU.b])
        S.op("dve", lambda e: e.tensor_tensor(out=UV[0:64, i, 0:V], in0=pU[0:64, 0:V], in1=Up[0:64, i, 0:V], op=ALU.add),
             reads=[pU.b, Up.b], writes=[UV.b])
        S.op("pe", lambda e: e.matmul(pO[0:V, 0:64], lhsT=Hb[0:Kd, :], rhs=QR[0:Kd, i, 64:128], start=True, stop=False),
             reads=[Hb.b, QR.b], writes=[pO.b])
        S.op("pe", lambda e: e.matmul(pO[0:V, 0:64], lhsT=UV[:, i, 0:V], rhs=G4[:, i, 64:128], start=False, stop=True),
             reads=[UV.b, G4.b], writes=[pO.b])
        S.op("act", lambda e: e.copy(out=OT[0:V, i * 64:(i + 1) * 64], in_=pO[0:V, 0:64]), reads=[pO.b], writes=[OT.b])
        S.op("pe", lambda e: e.matmul(pH[0:Kd, 0:V], lhsT=KBe[:, i, 0:Kd], rhs=UV[:, i, 0:V], start=True, stop=True),
             reads=[KBe.b, UV.b], writes=[pH.b])
        S.op("dve", lambda e: e.scalar_tensor_tensor(out=Hb[0:Kd, :], in0=H[0:Kd, :], scalar=wC[0:Kd, i:i + 1], in1=pH[0:Kd, 0:V],
                                                     op0=ALU.mult, op1=ALU.add), reads=[H.b, wC.b, pH.b], writes=[Hb.b])
        S.op("dve", lambda e: e.scalar_tensor_tensor(out=H[0:Kd, :], in0=H[0:Kd, :], scalar=wC[0:Kd, i:i + 1], in1=pH[0:Kd, 0:V],
                                                     op0=ALU.mult, op1=ALU.add), reads=[H.b, wC.b, pH.b], writes=[H.b])


def phase_B(kb, c, PT, b_PT, prm, OMIX, b_OMIX):
    cfg, S, nc = kb.cfg, kb.S, kb.nc
    W = cfg.SEG
    NCH = W // 64
    RWH, DNH, RW, DN = cfg.RWH, cfg.DNH, cfg.RW, cfg.DN
    rows = cfg.rows
    c0 = RW_DECAY_SCALE
    with ExitStack() as es:
        build_masks(kb, es, c)
        ones, ident = c["ones"], c["ident"]
        bm, bi, bcol = c["b_mask"], c["b_ident"], c["b_col"]
        mk = lambda name, shape, dt=F32: T(kb.sb(es, "B_" + name, shape, dt))
        rwp = mk("rwp", [64, 13, RWH])
        lmu = mk("lmu", [128, 8])
        w2 = mk("w2", [96, RW]); a2 = mk("a2", [96, RW]); g2 = mk("g2", [128, 2, RW])
        dnp = mk("dnp", [128, 12, DNH]); dnw = mk("dnw", [128, 1]); dnh = mk("dnh", [16, 3])
        sel = mk("sel", [16, 16, 128])
        omka = mk("omka", [64, RWH])
        S.dma("sp", lambda e: e.dma_start(out=rwp[:, 0:10, :], in_=prm["rwp"]), writes=[rwp.b])
        S.dma("sp", lambda e: e.dma_start(out=lmu[:, 0:4], in_=prm["lmu"]), writes=[lmu.b])
        S.dma("sp", lambda e: e.dma_start(out=w2[:], in_=prm["rw_w2"]), writes=[w2.b])
        S.dma("sp", lambda e: e.dma_start(out=a2[:], in_=prm["rw_a2"]), writes=[a2.b])
        S.dma("sp", lambda e: e.dma_start(out=g2[:], in_=prm["rw_g2"].rearrange("(a p) n -> p a n", p=128)), writes=[g2.b])
        S.dma("sp", lambda e: e.dma_start(out=dnp[:], in_=prm["dnp"]), writes=[dnp.b])
        S.dma("sp", lambda e: e.dma_start(out=dnw[:], in_=prm["dnw"]), writes=[dnw.b])
        S.dma("sp", lambda e: e.dma_start(out=dnh[:, 0:2], in_=prm["dnh"]), writes=[dnh.b])
        S.op("dve", lambda e: e.tensor_scalar(out=rwp[:, 10:13, :], in0=rwp[:, 0:3, :], scalar1=-1.0, scalar2=1.0, op0=ALU.mult, op1=ALU.add),
             reads=[rwp.b], writes=[rwp.b])
        S.op("dve", lambda e: e.tensor_scalar(out=lmu[:, 4:8], in0=lmu[:, 0:4], scalar1=-1.0, scalar2=1.0, op0=ALU.mult, op1=ALU.add),
             reads=[lmu.b], writes=[lmu.b])
        S.op("dve", lambda e: e.tensor_scalar(out=omka[:], in0=rwp[:, 6, :], scalar1=-1.0, scalar2=1.0, op0=ALU.mult, op1=ALU.add),
             reads=[rwp.b], writes=[omka.b])
        S.op("act", lambda e: e.activation(out=dnh[:, 2:3], in_=dnh[:, 0:1], func=AF.Exp), reads=[dnh.b], writes=[dnh.b])
        S.op("dve", lambda e: e.tensor_scalar(out=dnh[:, 2:3], in0=dnh[:, 2:3], scalar1=-1.0, scalar2=None, op0=ALU.mult),
             reads=[dnh.b], writes=[dnh.b])
        S.op("pool", lambda e: e.memset(sel[:], 1.0), writes=[sel.b])
        S.op("pool", lambda e: e.affine_select(out=sel[:], in_=sel[:], pattern=[[1, 16], [0, 128]], compare_op=ALU.is_equal, fill=0.0,
                                               base=0, channel_multiplier=-1), reads=[sel.b], writes=[sel.b])
        rmask = mk("rmask", [128, W])
        S.op("pool", lambda e: e.memset(rmask[:], 1.0), writes=[rmask.b])
        S.op("pool", lambda e: e.memset(rmask[:].rearrange("p (a b) -> p a b", b=64)[:, :, 0:1], 0.0), writes=[rmask.b])
        Hrw = [mk("Hrw%d" % h, [64, 64]) for h in range(RWH)]
        Hdn = [mk("Hdn%d" % h, [128, 128]) for h in range(DNH)]
        Hrwb = [mk("Hrwb%d" % h, [64, 64], BF16) for h in range(RWH)]
        Hdnb = [mk("Hdnb%d" % h, [128, 128], BF16) for h in range(DNH)]
        for Ht in Hrw + Hdn + Hrwb + Hdnb:
            S.op("pool", lambda e: e.memset(Ht[:], 0.0), writes=[Ht.b])
        def mku(par):
            d = {k: mk("%s_%d" % (k, par), [128, NCH, 128], BF16) for k in ("QR", "BK", "BKe", "VV", "QRg")}
            d["wC"] = mk("wC_%d" % par, [128, NCH])
            d["NA"] = mk("NA_%d" % par, [1, NCH, 128])
            d["HO1"] = mk("HO1_%d" % par, [128, W]); d["HO2"] = mk("HO2_%d" % par, [128, W])
            S.op("pool", lambda e: e.memset(d["VV"][:], 0.0), writes=[d["VV"].b])
            return d
        usets = [mku(0), mku(1)]
        OT = mk("OT", [128, W])
        nones = mk("nones", [1, 64])
        S.op("pool", lambda e: e.memset(nones[:], -1.0), writes=[nones.b])
        P = {k: mk(k, [128, NCH, 128], BF16) for k in ("G4", "Xp", "KKt", "KBe", "UV")}
        P["Up"] = mk("Up", [128, NCH, 128])
        P["E"] = mk("E", [128, NCH, 128])
        S.op("pool", lambda e: e.memset(P["UV"][:], 0.0), writes=[P["UV"].b])
        P["WmT"] = mk("WmT", [128, NCH, 64], BF16)
        for k in ("A", "AT", "TT"):
            P[k] = mk(k, [64, NCH, 64], BF16)
        P["Y"] = [mk("Y%d" % i, [64, NCH, 64], BF16) for i in range(2)]
        P["YT"] = [mk("YT%d" % i, [64, NCH, 64], BF16) for i in range(2)]
        pst = lambda name: T(kb.ps(es, "Bp_" + name, [128, 512]), excl=True)
        P["pg"] = pst("g"); P["pinv"] = [pst("i0"), pst("i1"), pst("i2")]; P["pm"] = pst("m")
        pseq = kb.ps(es, "Bp_seq", [128, 512])
        bseq = Buf(True)
        P["pseq"] = [T(pseq[:, 0:128], b=bseq), T(pseq[:, 128:256], b=bseq), T(pseq[:, 256:384], b=bseq)]
        pp = [pst("p0"), pst("p1")]
        ppc = [0]

        def nextp():
            ppc[0] += 1
            return pp[ppc[0] % 2]
        ft = {}

        def F(name, dt=F32):
            if name not in ft:
                ft[name] = mk("f_" + name, [128, W], dt)
            return ft[name]
        SC = min(512, RW, DN)
        stage = mk("stage", [128, W // 128, SC], BF16)
        LW = mk("LW", [128, 1, W + 1]); LWs = mk("LWs", [128, 4, W])
        BETA = T(LWs[0:16, 0, :], b=LWs.b); GG = T(LWs[0:16, 1, :], b=LWs.b)
        PIN = [mk("PIN%d" % i, [128, W + 4]) for i in range(3)]

        def v3(t, Kd):
            return t[0:Kd, :].rearrange("p (a b) -> p a b", b=64)

        def dve_tt(out, a, b_, op, reads, writes, eng="dve"):
            S.op(eng, lambda e: e.tensor_tensor(out=out, in0=a, in1=b_, op=op), reads=reads, writes=writes)

        def load_shift(dst, row0, nrows, t0, lead):
            bsrc = [b_PT[j] for j in range(row0 // 128, (row0 + nrows - 1) // 128 + 1)]
            if t0 == 0:
                S.op("pool", lambda e: e.memset(dst[0:nrows, 0:lead], 0.0), writes=[dst.b])
                S.dma("sp", lambda e: e.dma_start(out=dst[0:nrows, lead:lead + W], in_=PT[row0:row0 + nrows, 0:W]), reads=bsrc, writes=[dst.b])
            else:
                S.dma("sp", lambda e: e.dma_start(out=dst[0:nrows, 0:lead + W], in_=PT[row0:row0 + nrows, t0 - lead:t0 + W]), reads=bsrc, writes=[dst.b])

        def chunk_common(u, Kd, Gsrc, Gsrc_reads, scale, r_, kk_, b_, kh_, v_, b0_=None):
            G, GP, EG, ENG, EGP, EGC = F("G"), F("GP"), F("A"), F("SQ"), F("RS"), F("SIG")
            S.op("dve", lambda e: e.tensor_tensor_scan(out=G[0:Kd, :], data0=rmask[0:Kd, :], data1=Gsrc, initial=0.0, op0=ALU.mult, op1=ALU.add),
                 reads=[rmask.b] + Gsrc_reads, writes=[G.b])
            dve_tt(GP[0:Kd, :], G[0:Kd, :], Gsrc, ALU.subtract, [G.b] + Gsrc_reads, [GP.b])
            S.op("act", lambda e: e.activation(out=EG[0:Kd, :], in_=G[0:Kd, :], func=AF.Exp, scale=scale), reads=[G.b], writes=[EG.b])
            if b0_ is None:
                S.op("act", lambda e: e.activation(out=ENG[0:Kd, :], in_=G[0:Kd, :], func=AF.Exp, scale=-scale), reads=[G.b], writes=[ENG.b])
            S.op("act", lambda e: e.activation(out=EGP[0:Kd, :], in_=GP[0:Kd, :], func=AF.Exp, scale=scale), reads=[GP.b], writes=[EGP.b])
            gend = v3(G, Kd)[:, :, 63:64]
            dve_tt(v3(EGC, Kd), gend.to_broadcast([Kd, NCH, 64]), v3(G, Kd), ALU.subtract, [G.b], [EGC.b])
            S.op("act", lambda e: e.activation(out=EGC[0:Kd, :], in_=EGC[0:Kd, :], func=AF.Exp, scale=scale), reads=[EGC.b], writes=[EGC.b])
            S.op("act", lambda e: e.activation(out=u["wC"][0:Kd, :], in_=gend.rearrange("p a b -> p (a b)"), func=AF.Exp, scale=scale),
                 reads=[G.b], writes=[u["wC"].b])
            QR, BK, BKe, VV = u["QR"], u["BK"], u["BKe"], u["VV"]
            dve_tt(QR[0:Kd, :, 0:64], v3(kk_, Kd), v3(EGP, Kd), ALU.mult, [kk_.b, EGP.b], [QR.b])
            dve_tt(QR[0:Kd, :, 64:128], v3(r_, Kd), v3(EG, Kd), ALU.mult, [r_.b, EG.b], [QR.b], eng="pool")
            if b0_ is None:
                dve_tt(BK[0:Kd, :, 0:64], v3(b_, Kd), v3(ENG, Kd), ALU.mult, [b_.b, ENG.b], [BK.b])
                dve_tt(BK[0:Kd, :, 64:128], v3(kh_, Kd), v3(ENG, Kd), ALU.mult, [kh_.b, ENG.b], [BK.b], eng="pool")
            else:
                QRg, NA = u["QRg"], u["NA"]
                S.op("pool", lambda e: e.tensor_copy(out=BK[0:Kd, :, 0:64], in_=v3(b0_, Kd)), reads=[b0_.b], writes=[BK.b])
                S.op("pool", lambda e: e.tensor_copy(out=BK[0:Kd, :, 64:128], in_=v3(kh_, Kd)), reads=[kh_.b], writes=[BK.b])
                S.op("pool", lambda e: e.tensor_copy(out=QRg[0:Kd, :, 0:64], in_=v3(kk_, Kd)), reads=[kk_.b], writes=[QRg.b])
                S.op("pool", lambda e: e.tensor_copy(out=QRg[0:Kd, :, 64:128], in_=v3(r_, Kd)), reads=[r_.b], writes=[QRg.b])
                S.op("dve", lambda e: e.tensor_copy(out=NA[0:1, :, 0:64], in_=v3(GP, 1)), reads=[GP.b], writes=[NA.b])
                S.op("dve", lambda e: e.tensor_copy(out=NA[0:1, :, 64:128], in_=v3(G, 1)), reads=[G.b], writes=[NA.b])
            dve_tt(BKe[0:Kd, :, 0:64], v3(b_, Kd), v3(EGC, Kd), ALU.mult, [b_.b, EGC.b], [BKe.b])
            dve_tt(BKe[0:Kd, :, 64:128], v3(kh_, Kd), v3(EGC, Kd), ALU.mult, [kh_.b, EGC.b], [BKe.b], eng="pool")
            S.op("pool", lambda e: e.tensor_copy(out=VV[0:Kd, :, 64:128], in_=v3(v_, Kd)), reads=[v_.b], writes=[VV.b])

        def l2_rstd(Kd, src, dst):
            SQ = F("SQ")
            S.op("act", lambda e: e.activation(out=SQ[0:Kd, :], in_=src[0:Kd, :], func=AF.Square), reads=[src.b], writes=[SQ.b])
            p = nextp()
            S.op("pe", lambda e: e.matmul(p[0:Kd, 0:W], lhsT=ones[0:Kd, 0:Kd], rhs=SQ[0:Kd, :], start=True, stop=True), reads=[SQ.b, bm], writes=[p.b])
            S.op("act", lambda e: e.activation(out=dst[0:Kd, :], in_=p[0:Kd, 0:W], func=AF.Ln, bias=c["EPS6"][0:Kd, :]), reads=[p.b, bcol], writes=[dst.b])
            S.op("act", lambda e: e.activation(out=dst[0:Kd, :], in_=dst[0:Kd, :], func=AF.Exp, scale=-0.5), reads=[dst.b], writes=[dst.b])

        def to_stage(Kd, res, col0):
            p = P["pg"]
            nb_ = W // 128
            for i in range(nb_):
                S.op("pe", lambda e: e.transpose(out=p[:, i * Kd:(i + 1) * Kd], in_=res[0:Kd, i * 128:(i + 1) * 128], identity=ident[0:Kd, 0:Kd]),
                     reads=[res.b, bi], writes=[p.b])
            S.op("act", lambda e: e.copy(out=stage[:, :, col0:col0 + Kd], in_=p[:, 0:nb_ * Kd].rearrange("p (a b) -> p a b", a=nb_)),
                 reads=[p.b], writes=[stage.b])

        def prep_rw_shared(t0):
            for i, nm in enumerate(("wd", "ad", "gd", "gd")):
                r0 = rows[nm] + (128 if i == 3 else 0)
                bsrc = [b_PT[r0 // 128]]
                if t0 == 0:
                    S.op("pool", lambda e: e.memset(LW[:, 0, 0:1], 0.0), writes=[LW.b])
                    S.dma("sp", lambda e: e.dma_start(out=LW[:, 0, 1:1 + W], in_=PT[r0:r0 + 128, 0:W]), reads=bsrc, writes=[LW.b])
                else:
                    S.dma("sp", lambda e: e.dma_start(out=LW[:, 0, :], in_=PT[r0:r0 + 128, t0 - 1:t0 + W]), reads=bsrc, writes=[LW.b])
                S.op("dve", lambda e: e.tensor_scalar(out=LWs[:, i, :], in0=LW[:, 0, 0:W], scalar1=lmu[:, i:i + 1], scalar2=None, op0=ALU.mult),
                     reads=[LW.b, lmu.b], writes=[LWs.b])
                S.op("dve", lambda e: e.scalar_tensor_tensor(out=LWs[:, i, :], in0=LW[:, 0, 1:1 + W], scalar=lmu[:, 4 + i:5 + i], in1=LWs[:, i, :],
                                                             op0=ALU.mult, op1=ALU.add), reads=[LW.b, lmu.b, LWs.b], writes=[LWs.b])
                if i != 1:
                    fn = AF.Tanh if i == 0 else AF.Sigmoid
                    S.op("act", lambda e: e.activation(out=LWs[:, i, :], in_=LWs[:, i, :], func=fn), reads=[LWs.b], writes=[LWs.b])

        def prep_dn_shared(t0):
            rb = rows["ba"]
            S.dma("sp", lambda e: e.dma_start(out=BETA[:], in_=PT[rb:rb + 16, t0:t0 + W]), reads=[b_PT[rb // 128]], writes=[BETA.b])
            S.dma("sp", lambda e: e.dma_start(out=GG[:], in_=PT[rb + 32:rb + 48, t0:t0 + W]), reads=[b_PT[rb // 128]], writes=[GG.b])
            S.op("act", lambda e: e.activation(out=BETA[:], in_=BETA[:], func=AF.Sigmoid), reads=[BETA.b], writes=[BETA.b])
            S.op("act", lambda e: e.activation(out=GG[:], in_=GG[:], func=AF.Exp, bias=dnh[:, 1:2]), reads=[GG.b, dnh.b], writes=[GG.b])
            S.op("act", lambda e: e.activation(out=GG[:], in_=GG[:], func=AF.Ln, bias=c["ONE"][0:16, :]), reads=[GG.b, bcol], writes=[GG.b])
            S.op("dve", lambda e: e.tensor_scalar(out=GG[:], in0=GG[:], scalar1=dnh[:, 2:3], scalar2=None, op0=ALU.mult), reads=[GG.b, dnh.b], writes=[GG.b])

        def prep_rw(u, h, t0):
            if h == 0:
                prep_rw_shared(t0)
            src = {}
            for i, nm in enumerate(("r", "k", "v")):
                load_shift(PIN[i], rows[nm] + h * 64, 64, t0, 1)
                d = F("x_" + nm)
                S.op("dve", lambda e: e.tensor_scalar(out=d[0:64, :], in0=PIN[i][0:64, 0:W], scalar1=rwp[:, i, h:h + 1], scalar2=None, op0=ALU.mult),
                     reads=[PIN[i].b, rwp.b], writes=[d.b])
                S.op("dve", lambda e: e.scalar_tensor_tensor(out=d[0:64, :], in0=PIN[i][0:64, 1:1 + W], scalar=rwp[:, 10 + i, h:h + 1], in1=d[0:64, :],
                                                             op0=ALU.mult, op1=ALU.add), reads=[PIN[i].b, rwp.b, d.b], writes=[d.b])
                src[nm] = d
            r_, k_, v_ = src["r"], src["k"], src["v"]
            hs = slice(h * 64, (h + 1) * 64)
            SIG, A_, GT = F("SIG"), F("A"), u["HO2"]
            p = nextp()
            S.op("pe", lambda e: e.matmul(p[0:64, 0:W], lhsT=w2[0:96, hs], rhs=LWs[0:96, 0, :], start=True, stop=True), reads=[w2.b, LWs.b], writes=[p.b])
            S.op("act", lambda e: e.activation(out=SIG[0:64, :], in_=p[0:64, 0:W], func=AF.Sigmoid, bias=rwp[:, 3, h:h + 1]), reads=[p.b, rwp.b], writes=[SIG.b])
            p = nextp()
            S.op("pe", lambda e: e.matmul(p[0:64, 0:W], lhsT=a2[0:96, hs], rhs=LWs[0:96, 1, :], start=True, stop=True), reads=[a2.b, LWs.b], writes=[p.b])
            S.op("act", lambda e: e.activation(out=A_[0:64, :], in_=p[0:64, 0:W], func=AF.Sigmoid, bias=rwp[:, 4, h:h + 1]), reads=[p.b, rwp.b], writes=[A_.b])
            p = nextp()
            for a in range(2):
                S.op("pe", lambda e: e.matmul(p[0:64, 0:W], lhsT=g2[:, a, hs], rhs=LWs[:, 2 + a, :], start=(a == 0), stop=(a == 1)), reads=[g2.b, LWs.b], writes=[p.b])
            S.op("act", lambda e: e.copy(out=GT[0:64, :], in_=p[0:64, 0:W]), reads=[p.b], writes=[GT.b])
            KX, RS, KK, T1, KH, B_ = F("KX"), F("RS"), F("KK"), F("T1"), F("KH"), F("B")
            S.op("dve", lambda e: e.tensor_scalar(out=KX[0:64, :], in0=k_[0:64, :], scalar1=rwp[:, 5, h:h + 1], scalar2=None, op0=ALU.mult),
                 reads=[k_.b, rwp.b], writes=[KX.b])
            l2_rstd(64, KX, RS)
            dve_tt(KK[0:64, :], KX[0:64, :], RS[0:64, :], ALU.mult, [KX.b, RS.b], [KK.b])
            S.op("dve", lambda e: e.tensor_scalar(out=T1[0:64, :], in0=A_[0:64, :], scalar1=rwp[:, 6, h:h + 1], scalar2=omka[:, h:h + 1], op0=ALU.mult, op1=ALU.add),
                 reads=[A_.b, rwp.b, omka.b], writes=[T1.b])
            dve_tt(KH[0:64, :], k_[0:64, :], T1[0:64, :], ALU.mult, [k_.b, T1.b], [KH.b], eng="pool")
            dve_tt(B_[0:64, :], KK[0:64, :], A_[0:64, :], ALU.mult, [KK.b, A_.b], [B_.b], eng="pool")
            RK, BON = F("KX"), u["HO1"]
            S.op("dve", lambda e: e.scalar_tensor_tensor(out=RK[0:64, :], in0=r_[0:64, :], scalar=rwp[:, 7, h:h + 1], in1=KH[0:64, :], op0=ALU.mult, op1=ALU.mult),
                 reads=[r_.b, rwp.b, KH.b], writes=[RK.b])
            p = nextp()
            S.op("pe", lambda e: e.matmul(p[0:64, 0:W], lhsT=ones[0:64, 0:64], rhs=RK[0:64, :], start=True, stop=True), reads=[RK.b, bm], writes=[p.b])
            dve_tt(BON[0:64, :], p[0:64, 0:W], v_[0:64, :], ALU.mult, [p.b, v_.b], [BON.b])
            chunk_common(u, 64, SIG[0:64, :], [SIG.b], -c0, r_, KK, B_, KH, v_)

        def prep_dn(u, h, t0):
            if h == 0:
                prep_dn_shared(t0)
            src = {}
            for i, nm in enumerate(("dq", "dk", "dv")):
                load_shift(PIN[i], rows[nm] + h * 128, 128, t0, 3)
                d = F("x_" + ("r", "k", "v")[i])
                S.op("dve", lambda e: e.tensor_scalar(out=d[:, :], in0=PIN[i][:, 0:W], scalar1=dnp[:, i, h:h + 1], scalar2=None, op0=ALU.mult),
                     reads=[PIN[i].b, dnp.b], writes=[d.b])
                for j in range(1, 4):
                    S.op("dve", lambda e: e.scalar_tensor_tensor(out=d[:, :], in0=PIN[i][:, j:j + W], scalar=dnp[:, 3 * j + i, h:h + 1], in1=d[:, :],
                                                                 op0=ALU.mult, op1=ALU.add), reads=[PIN[i].b, dnp.b, d.b], writes=[d.b])
                S.op("act", lambda e: e.activation(out=d[:, :], in_=d[:, :], func=AF.Silu), reads=[d.b], writes=[d.b])
                src[nm] = d
            q_, k_, v_ = src["dq"], src["dk"], src["dv"]
            RS, QN, KN, B_, EGB, VB = F("RS"), F("KX"), F("KK"), F("B"), F("T1"), F("KH")
            l2_rstd(128, q_, RS)
            S.op("dve", lambda e: e.scalar_tensor_tensor(out=QN[:, :], in0=q_[:, :], scalar=128.0 ** -0.5, in1=RS[:, :], op0=ALU.mult, op1=ALU.mult),
                 reads=[q_.b, RS.b], writes=[QN.b])
            l2_rstd(128, k_, RS)
            dve_tt(KN[:, :], k_[:, :], RS[:, :], ALU.mult, [k_.b, RS.b], [KN.b])
            pbeta = nextp()
            S.op("pe", lambda e: e.matmul(pbeta[:, 0:W], lhsT=sel[:, h, :], rhs=BETA[:], start=True, stop=True), reads=[sel.b, BETA.b], writes=[pbeta.b])
            B0 = F("x_k")
            dve_tt(B0[:, :], KN[:, :], pbeta[:, 0:W], ALU.mult, [KN.b, pbeta.b], [B0.b])
            dve_tt(VB[:, :], v_[:, :], pbeta[:, 0:W], ALU.mult, [v_.b, pbeta.b], [VB.b])
            pg_ = nextp()
            S.op("pe", lambda e: e.matmul(pg_[:, 0:W], lhsT=sel[:, h, :], rhs=GG[:], start=True, stop=True), reads=[sel.b, GG.b], writes=[pg_.b])
            GB = F("SIG")
            S.op("act", lambda e: e.copy(out=GB[:, :], in_=pg_[:, 0:W]), reads=[pg_.b], writes=[GB.b])
            S.op("act", lambda e: e.activation(out=EGB[:, :], in_=pg_[:, 0:W], func=AF.Exp), reads=[pg_.b], writes=[EGB.b])
            dve_tt(B_[:, :], B0[:, :], EGB[:, :], ALU.mult, [B0.b, EGB.b], [B_.b])
            ZT = u["HO1"]
            rz = rows["z"] + h * 128
            S.dma("sp", lambda e: e.dma_start(out=ZT[:, :], in_=PT[rz:rz + 128, t0:t0 + W]), reads=[b_PT[rz // 128]], writes=[ZT.b])
            S.op("act", lambda e: e.activation(out=ZT[:, :], in_=ZT[:, :], func=AF.Silu), reads=[ZT.b], writes=[ZT.b])
            chunk_common(u, 128, GB[:, :], [GB.b], 1.0, QN, KN, B_, KN, VB, b0_=B0)

        e_X, e_DD = mk("e_X", [128, W]), mk("e_DD", [128, W])

        def flush_stage(h, Kd, base, t0):
            if (h * Kd + Kd) % SC == 0:
                cb = base + (h * Kd + Kd) - SC
                for i in range(W // 128):
                    S.dma("pool", lambda e: e.dma_start(out=OMIX[t0 + i * 128:t0 + (i + 1) * 128, cb:cb + SC], in_=stage[:, i, :]), reads=[stage.b], writes=[b_OMIX])

        def run_rw(u, h, t0):
            uu = dict(u); uu["H"] = Hrw[h]; uu["Hb"] = Hrwb[h]; uu["OT"] = OT
            uu.pop("QRg", None)
            dplr_segment(kb, c, uu, 64, NCH, P)
            BON, GT = u["HO1"], u["HO2"]
            SQ, MEAN, DD, RES = e_X, e_X, e_DD, e_X
            p = P["pm"]
            S.op("pe", lambda e: e.matmul(p[0:64, 0:W], lhsT=ones[0:64, 0:64], rhs=OT[0:64, :], start=True, stop=True), reads=[OT.b, bm], writes=[p.b])
            S.op("act", lambda e: e.mul(out=MEAN[0:64, :], in_=p[0:64, 0:W], mul=1.0 / 64), reads=[p.b], writes=[MEAN.b])
            dve_tt(DD[0:64, :], OT[0:64, :], MEAN[0:64, :], ALU.subtract, [OT.b, MEAN.b], [DD.b])
            S.op("act", lambda e: e.activation(out=SQ[0:64, :], in_=DD[0:64, :], func=AF.Square), reads=[DD.b], writes=[SQ.b])
            S.op("pe", lambda e: e.matmul(p[0:64, 0:W], lhsT=ones[0:64, 0:64], rhs=SQ[0:64, :], start=True, stop=True), reads=[SQ.b, bm], writes=[p.b])
            S.op("act", lambda e: e.activation(out=RES[0:64, :], in_=p[0:64, 0:W], func=AF.Ln, scale=1.0 / 64, bias=c["GNEPS"][0:64, :]), reads=[p.b, bcol], writes=[RES.b])
            S.op("act", lambda e: e.activation(out=RES[0:64, :], in_=RES[0:64, :], func=AF.Exp, scale=-0.5), reads=[RES.b], writes=[RES.b])
            dve_tt(DD[0:64, :], DD[0:64, :], RES[0:64, :], ALU.mult, [DD.b, RES.b], [DD.b])
            S.op("dve", lambda e: e.tensor_scalar(out=DD[0:64, :], in0=DD[0:64, :], scalar1=rwp[:, 8, h:h + 1], scalar2=rwp[:, 9, h:h + 1], op0=ALU.mult, op1=ALU.add),
                 reads=[DD.b, rwp.b], writes=[DD.b])
            dve_tt(DD[0:64, :], DD[0:64, :], BON[0:64, :], ALU.add, [DD.b, BON.b], [DD.b], eng="pool")
            dve_tt(RES[0:64, :], DD[0:64, :], GT[0:64, :], ALU.mult, [DD.b, GT.b], [RES.b])
            to_stage(64, RES, (h * 64) % SC)
            flush_stage(h, 64, 0, t0)

        def run_dn(u, h, t0):
            uu = dict(u); uu["H"] = Hdn[h]; uu["Hb"] = Hdnb[h]; uu["OT"] = OT; uu["E"] = P["E"]; uu["nones"] = nones
            dplr_segment(kb, c, uu, 128, NCH, P)
            ZT = u["HO1"]
            SQ, l2n, RES = e_X, e_X, e_DD
            p = P["pm"]
            S.op("act", lambda e: e.activation(out=SQ[:, :], in_=OT[:, :], func=AF.Square), reads=[OT.b], writes=[SQ.b])
            S.op("pe", lambda e: e.matmul(p[:, 0:W], lhsT=ones[:, :], rhs=SQ[:, :], start=True, stop=True), reads=[SQ.b, bm], writes=[p.b])
            S.op("act", lambda e: e.activation(out=l2n[:, :], in_=p[:, 0:W], func=AF.Ln, scale=1.0 / 128, bias=c["EPS6"]), reads=[p.b, bcol], writes=[l2n.b])
            S.op("act", lambda e: e.activation(out=l2n[:, :], in_=l2n[:, :], func=AF.Exp, scale=-0.5), reads=[l2n.b], writes=[l2n.b])
            S.op("dve", lambda e: e.scalar_tensor_tensor(out=RES[:, :], in0=OT[:, :], scalar=dnw[:, 0:1], in1=l2n[:, :], op0=ALU.mult, op1=ALU.mult),
                 reads=[OT.b, dnw.b, l2n.b], writes=[RES.b])
            dve_tt(RES[:, :], RES[:, :], ZT[:, :], ALU.mult, [RES.b, ZT.b], [RES.b])
            to_stage(128, RES, (h * 128) % SC)
            flush_stage(h, 128, RW, t0)

        units = []
        for seg in range(cfg.S // W):
            units += [("rw", h, seg * W) for h in range(RWH)] + [("dn", h, seg * W) for h in range(DNH)]

        def mkprep(i):
            kind, h, t0 = units[i]
            return (lambda: prep_rw(usets[i % 2], h, t0)) if kind == "rw" else (lambda: prep_dn(usets[i % 2], h, t0))

        def mkrun(i):
            kind, h, t0 = units[i]
            return (lambda: run_rw(usets[i % 2], h, t0)) if kind == "rw" else (lambda: run_dn(usets[i % 2], h, t0))
        mkprep(0)()
        for i in range(len(units)):
            nxt = mkprep(i + 1) if i + 1 < len(units) else None
            run_pair(S, nxt, mkrun(i), quota=(1, 3))
    S.barrier()


def build_hT(kb, c, S, src, row0, ntiles, D, KD, xt, hb, ss, gt, hT, psT, pcnt, nrm=True):
    for ti in range(ntiles):
        t0 = row0 + ti * 128
        S.dma("sp", lambda e: e.dma_start(out=xt[:], in_=src[t0:t0 + 128, :]), writes=[xt.b])
        S.op("act", lambda e: e.activation(out=hb[:], in_=xt[:], func=AF.Square, accum_out=ss[:]), reads=[xt.b], writes=[hb.b, ss.b])
        rmsnorm_rstd(kb, c, ss[:], ss.b, D)
        S.op("dve", lambda e: e.scalar_tensor_tensor(out=hb[:], in0=xt[:], scalar=ss[:, 0:1], in1=gt[:], op0=ALU.mult, op1=ALU.mult),
             reads=[xt.b, ss.b, gt.b], writes=[hb.b])
        transpose_bf(kb, c, S, hb, KD, hT, ti, psT, pcnt)


def transpose_bf(kb, c, S, src, KD, dstT, ti, psT, pcnt):
    for k0 in range(0, KD, 8):
        kn = min(8, KD - k0)
        p = psT[pcnt[0] % len(psT)]; pcnt[0] += 1
        for k in range(kn):
            S.op("pe", lambda e: e.transpose(out=p[:, k * 128:(k + 1) * 128], in_=src[:, (k0 + k) * 128:(k0 + k + 1) * 128], identity=c["identb"][:]),
                 reads=[src.b, c["b_ident"]], writes=[p.b])
        sv = p[:, 0:kn * 128].rearrange("p (a b) -> p a b", a=kn)
        dv = dstT[:, k0:k0 + kn, ti * 128:(ti + 1) * 128]
        if pcnt[0] % 2:
            S.op("act", lambda e: e.copy(out=dv, in_=sv), reads=[p.b], writes=[dstT.b])
        else:
            S.op("dve", lambda e: e.tensor_copy(out=dv, in_=sv), reads=[p.b], writes=[dstT.b])


def phase_A1(kb, c, xo, g1d, wG, gbd, GATES, b_G):
    cfg, S = kb.cfg, kb.S
    D, KD, TGO, KP = cfg.D, cfg.KD, cfg.TGO, cfg.KP
    NT = TGO // 128
    NPG = KD // KP
    with ExitStack() as es:
        mk = lambda name, shape, dt=F32: T(kb.sb(es, "G_" + name, shape, dt))
        hT = mk("hT", [128, KD, TGO], BF16); xt = mk("xt", [128, D]); hb = mk("hb", [128, D], BF16); ss = mk("ss", [128, 1]); g1 = mk("g1", [128, D])
        slf = [mk("slf%d" % i, [128, KP, 512]) for i in range(2)]; slb = [mk("slb%d" % i, [128, KP, 512], BF16) for i in range(2)]
        gb = [mk("gb%d" % i, [128, 512]) for i in range(2)]
        stg = [mk("stg%d" % i, [128, 512]) for i in range(2)]
        psT = [T(kb.ps(es, "G_psT%d" % i, [128, 1024], BF16), excl=True) for i in range(2)]
        psG = [T(kb.ps(es, "G_ps%d" % i, [128, 512]), excl=True) for i in range(NT)]
        S.dma("sp", lambda e: e.dma_start(out=g1[:], in_=g1d.partition_broadcast(128)), writes=[g1.b])
        pcnt = [0]
        it = 0
        for og in range(cfg.OWN // TGO):
            build_hT(kb, c, S, xo, og * TGO, NT, D, KD, xt, hb, ss, g1, hT, psT, pcnt)
            for gc in range(2 * D // 512):
                gbt = gb[gc % 2]
                S.dma("sp", lambda e: e.dma_start(out=gbt[:], in_=gbd[0:1, gc * 512:(gc + 1) * 512].partition_broadcast(128)), writes=[gbt.b])
                for q in range(NPG):
                    sl = it % 2; it += 1
                    S.dma("sp", lambda e: e.dma_start(out=slf[sl][:], in_=wG[gc, q]), writes=[slf[sl].b])
                    if it % 2:
                        S.op("act", lambda e: e.copy(out=slb[sl][:], in_=slf[sl][:]), reads=[slf[sl].b], writes=[slb[sl].b])
                    else:
                        S.op("dve", lambda e: e.tensor_copy(out=slb[sl][:], in_=slf[sl][:]), reads=[slf[sl].b], writes=[slb[sl].b])
                    for t in range(NT):
                        for k in range(KP):
                            kk = q * KP + k
                            S.op("pe", lambda e: e.matmul(psG[t][:, :], lhsT=hT[:, kk, t * 128:(t + 1) * 128], rhs=slb[sl][:, k, :],
                                                          start=(kk == 0), stop=(kk == KD - 1)), reads=[hT.b, slb[sl].b], writes=[psG[t].b])
                for t in range(NT):
                    st = stg[t % 2]
                    S.op("dve", lambda e: e.tensor_tensor(out=st[:], in0=psG[t][:, :], in1=gbt[:], op=ALU.add), reads=[psG[t].b, gbt.b], writes=[st.b])
                    S.op("act", lambda e: e.activation(out=st[:], in_=st[:], func=AF.Sigmoid), reads=[st.b], writes=[st.b])
                    r0 = og * TGO + t * 128
                    S.dma("pool", lambda e: e.dma_start(out=GATES[r0:r0 + 128, gc * 512:(gc + 1) * 512], in_=st[:]), reads=[st.b], writes=[b_G])
    S.barrier()


def phase_C1(kb, c, xo, own_idx, OMIX, b_OMIX, GATES, b_G, wBR, wO, XMID, b_XMID):
    cfg, S = kb.cfg, kb.S
    D, KD, KB, TGO, BW = cfg.D, cfg.KD, cfg.KB, cfg.TGO, cfg.BW
    NT = TGO // 128
    KBr = cfg.RW // 128
    KH = max(KD // 2, 1)
    with ExitStack() as es:
        mk = lambda name, shape, dt=F32: T(kb.sb(es, "C_" + name, shape, dt))
        KM = max(KD, KB)
        oT = mk("oT", [128, KM, TGO], BF16)
        mg = mk("mg", [128, NT, D], BF16)
        og_ = [mk("og%d" % i, [128, BW], BF16) for i in range(2)]
        idx = mk("idx", [128, cfg.OWN // 128], I32)
        SLW = min(8, KD, KBr)
        slf = [mk("slf%d" % i, [128, SLW, 512]) for i in range(2)]

        def pieces(k0, k1):
            return [(a, min(a + SLW, k1)) for a in range(k0, k1, SLW)]
        slb = [mk("slb%d" % i, [128, KM, 512], BF16) for i in range(2)]
        gt = [mk("gt%d" % i, [128, 2, 512]) for i in range(2)]
        tm = [mk("tm%d" % i, [128, 512]) for i in range(2)]
        xs = [mk("xs%d" % i, [128, 512]) for i in range(2)]
        psT = [T(kb.ps(es, "C_psT%d" % i, [128, 1024], BF16), excl=True) for i in range(2)]
        psY = [T(kb.ps(es, "C_psY%d" % i, [128, 512]), excl=True) for i in range(4)]
        S.dma("sp", lambda e: e.dma_start(out=idx[:], in_=own_idx), writes=[idx.b])
        pcnt = [0]
        it = 0
        yc = 0
        for g in range(cfg.OWN // TGO):
            for t in range(NT):
                o = og_[t % 2]
                col = g * NT + t
                S.dma("pool", lambda e: e.indirect_dma_start(out=o[:], out_offset=None, in_=OMIX,
                                                             in_offset=bass.IndirectOffsetOnAxis(ap=idx[:, col:col + 1], axis=0)),
                      reads=[idx.b, b_OMIX], writes=[o.b])
                transpose_bf(kb, c, S, o, KB, oT, t, psT, pcnt)
            for cc in range(D // 512):
                sb_ = slb[cc % 2]
                for br, (k0, k1) in enumerate(pieces(0, KBr) + pieces(KBr, KB)):
                    sl = it % 2; it += 1
                    S.dma("sp", lambda e: e.dma_start(out=slf[sl][:, 0:k1 - k0, :], in_=wBR[cc, :, k0:k1, :]), writes=[slf[sl].b])
                    if br % 2 == 0:
                        S.op("act", lambda e: e.copy(out=sb_[:, k0:k1, :], in_=slf[sl][:, 0:k1 - k0, :]), reads=[slf[sl].b], writes=[sb_.b])
                    else:
                        S.op("dve", lambda e: e.tensor_copy(out=sb_[:, k0:k1, :], in_=slf[sl][:, 0:k1 - k0, :]), reads=[slf[sl].b], writes=[sb_.b])
                for t in range(NT):
                    r0 = g * TGO + t * 128
                    gtt = gt[t % 2]
                    S.dma("sp", lambda e: e.dma_start(out=gtt[:, 0, :], in_=GATES[r0:r0 + 128, cc * 512:(cc + 1) * 512]), reads=[b_G], writes=[gtt.b])
                    S.dma("sp", lambda e: e.dma_start(out=gtt[:, 1, :], in_=GATES[r0:r0 + 128, D + cc * 512:D + (cc + 1) * 512]), reads=[b_G], writes=[gtt.b])
                    pr = psY[yc % 4]; pd = psY[(yc + 1) % 4]; yc += 2
                    for k in range(0, KBr):
                        S.op("pe", lambda e: e.matmul(pr[:, :], lhsT=oT[:, k, t * 128:(t + 1) * 128], rhs=sb_[:, k, :], start=(k == 0), stop=(k == KBr - 1)),
                             reads=[oT.b, sb_.b], writes=[pr.b])
                    for k in range(KBr, KB):
                        S.op("pe", lambda e: e.matmul(pd[:, :], lhsT=oT[:, k, t * 128:(t + 1) * 128], rhs=sb_[:, k, :], start=(k == KBr), stop=(k == KB - 1)),
                             reads=[oT.b, sb_.b], writes=[pd.b])
                    t1 = tm[t % 2]
                    S.op("dve", lambda e: e.tensor_tensor(out=t1[:], in0=pr[:, :], in1=gtt[:, 0, :], op=ALU.mult), reads=[pr.b, gtt.b], writes=[t1.b])
                    S.op("dve", lambda e: e.tensor_tensor(out=gtt[:, 1, :], in0=pd[:, :], in1=gtt[:, 1, :], op=ALU.mult), reads=[pd.b, gtt.b], writes=[gtt.b])
                    S.op("dve", lambda e: e.tensor_tensor(out=mg[:, t, cc * 512:(cc + 1) * 512], in0=t1[:], in1=gtt[:, 1, :], op=ALU.add),
                         reads=[t1.b, gtt.b], writes=[mg.b])
            for t in range(NT):
                mt = T(mg[:, t, :], b=mg.b)
                transpose_bf(kb, c, S, mt, KD, oT, t, psT, pcnt)
            for cc in range(D // 512):
                sb_ = slb[cc % 2]
                for hf, (k0, k1) in enumerate(pieces(0, KD)):
                    sl = it % 2; it += 1
                    S.dma("sp", lambda e: e.dma_start(out=slf[sl][:, 0:k1 - k0, :], in_=wO[cc, :, k0:k1, :]), writes=[slf[sl].b])
                    if hf % 2 == 0:
                        S.op("act", lambda e: e.copy(out=sb_[:, k0:k1, :], in_=slf[sl][:, 0:k1 - k0, :]), reads=[slf[sl].b], writes=[sb_.b])
                    else:
                        S.op("dve", lambda e: e.tensor_copy(out=sb_[:, k0:k1, :], in_=slf[sl][:, 0:k1 - k0, :]), reads=[slf[sl].b], writes=[sb_.b])
                for t in range(NT):
                    r0 = g * TGO + t * 128
                    x_ = xs[t % 2]
                    S.dma("sp", lambda e: e.dma_start(out=x_[:], in_=xo[r0:r0 + 128, cc * 512:(cc + 1) * 512]), writes=[x_.b])
                    pz = psY[yc % 4]; yc += 1
                    for k in range(KD):
                        S.op("pe", lambda e: e.matmul(pz[:, :], lhsT=oT[:, k, t * 128:(t + 1) * 128], rhs=sb_[:, k, :], start=(k == 0), stop=(k == KD - 1)),
                             reads=[oT.b, sb_.b], writes=[pz.b])
                    S.op("dve", lambda e: e.tensor_tensor(out=x_[:], in0=pz[:, :], in1=x_[:], op=ALU.add), reads=[pz.b, x_.b], writes=[x_.b])
                    S.dma("pool", lambda e: e.dma_start(out=XMID[r0:r0 + 128, cc * 512:(cc + 1) * 512], in_=x_[:]), reads=[x_.b], writes=[b_XMID])
    S.barrier()


def phase_C2(kb, c, XMID, b_XMID, g2d, wRd, bRd, X2, b_X2, R):
    cfg, S = kb.cfg, kb.S
    D, KD, NG, NE = cfg.D, cfg.KD, cfg.NG, cfg.NE
    NR = NG + NE
    NTO = cfg.OWN // 128
    with ExitStack() as es:
        mk = lambda name, shape, dt=F32: T(kb.sb(es, "R_" + name, shape, dt))
        xm = mk("xm", [128, D]); x2 = mk("x2", [128, D]); x2b = mk("x2b", [128, D], BF16); ss = mk("ss", [128, 1]); g2 = mk("g2", [128, D])
        x2T = mk("x2T", [128, KD, 128]); wR = mk("wR", [128, KD, NR]); bR = mk("bR", [128, NR])
        zt = mk("zt", [128, D], BF16)
        LG = mk("LG", [128, NR]); sm = mk("sm", [128, 16]); t3 = mk("t3", [128, NG, 8]); og = mk("og", [128, NG]); se = mk("se", [128, 8]); ee = mk("ee", [128, 8])
        m8 = mk("m8", [128, 8]); i8 = mk("i8", [128, 8], U32); i8f = mk("i8f", [128, 2]); tg = mk("tg", [128, NG]); oh = mk("oh", [128, NE])
        iog = mk("iog", [128, NG]); ioe = R["ioe"]
        psT = [T(kb.ps(es, "R_psT%d" % i, [128, 512]), excl=True) for i in range(2)]
        psL = T(kb.ps(es, "R_psL", [128, 512]), excl=True)
        S.dma("sp", lambda e: e.dma_start(out=g2[:], in_=g2d.partition_broadcast(128)), writes=[g2.b])
        S.dma("sp", lambda e: e.dma_start(out=wR[:], in_=wRd), writes=[wR.b])
        S.dma("sp", lambda e: e.dma_start(out=bR[:], in_=bRd.partition_broadcast(128)), writes=[bR.b])
        S.op("pool", lambda e: e.iota(iog[:], pattern=[[1, NG]], base=0, channel_multiplier=0, allow_small_or_imprecise_dtypes=True), writes=[iog.b])
        S.op("pool", lambda e: e.memset(zt[:], 0.0), writes=[zt.b])
        S.dma("pool", lambda e: e.dma_start(out=X2[cfg.OWN:cfg.OWN + 128, :], in_=zt[:]), reads=[zt.b], writes=[b_X2])
        M, RT = R["M"], R["RT"]
        pc = 0
        for i in range(NTO):
            r0 = i * 128
            S.dma("sp", lambda e: e.dma_start(out=xm[:], in_=XMID[r0:r0 + 128, :]), reads=[b_XMID], writes=[xm.b])
            S.op("act", lambda e: e.activation(out=x2[:], in_=xm[:], func=AF.Square, accum_out=ss[:]), reads=[xm.b], writes=[x2.b, ss.b])
            rmsnorm_rstd(kb, c, ss[:], ss.b, D)
            S.op("dve", lambda e: e.scalar_tensor_tensor(out=x2[:], in0=xm[:], scalar=ss[:, 0:1], in1=g2[:], op0=ALU.mult, op1=ALU.mult),
                 reads=[xm.b, ss.b, g2.b], writes=[x2.b])
            S.op("pool", lambda e: e.tensor_copy(out=x2b[:], in_=x2[:]), reads=[x2.b], writes=[x2b.b])
            S.dma("pool", lambda e: e.dma_start(out=X2[r0:r0 + 128, :], in_=x2b[:]), reads=[x2b.b], writes=[b_X2])
            for k0 in range(0, KD, 4):
                kn = min(4, KD - k0)
                p = psT[pc % 2]; pc += 1
                for k in range(kn):
                    S.op("pe", lambda e: e.transpose(out=p[:, k * 128:(k + 1) * 128], in_=x2[:, (k0 + k) * 128:(k0 + k + 1) * 128], identity=c["ident"][:]),
                         reads=[x2.b, c["b_ident"]], writes=[p.b])
                S.op("act", lambda e: e.copy(out=x2T[:, k0:k0 + kn, :], in_=p[:, 0:kn * 128].rearrange("p (a b) -> p a b", a=kn)), reads=[p.b], writes=[x2T.b])
            for k in range(KD):
                S.op("pe", lambda e: e.matmul(psL[:, 0:NR], lhsT=x2T[:, k, :], rhs=wR[:, k, :], start=(k == 0), stop=(k == KD - 1)),
                     reads=[x2T.b, wR.b], writes=[psL.b])
            D_ = lambda fn, reads, writes: S.op("dve", fn, reads=reads, writes=writes)
            D_(lambda e: e.tensor_tensor(out=LG[:], in0=psL[:, 0:NR], in1=bR[:], op=ALU.add), [psL.b, bR.b], [LG.b])
            D_(lambda e: e.tensor_reduce(out=sm[:, 0:1], in_=LG[:, 0:NG], axis=AX.X, op=ALU.max), [LG.b], [sm.b])
            D_(lambda e: e.tensor_scalar(out=og[:], in0=LG[:, 0:NG], scalar1=sm[:, 0:1], scalar2=None, op0=ALU.is_equal), [LG.b, sm.b], [og.b])
            D_(lambda e: e.tensor_scalar(out=sm[:, 1:2], in0=sm[:, 0:1], scalar1=-1.0, scalar2=None, op0=ALU.mult), [sm.b], [sm.b])
            S.op("act", lambda e: e.activation(out=tg[:], in_=LG[:, 0:NG], func=AF.Exp, bias=sm[:, 1:2], accum_out=sm[:, 2:3]), reads=[LG.b, sm.b], writes=[tg.b, sm.b])
            D_(lambda e: e.reciprocal(out=sm[:, 3:4], in_=sm[:, 2:3]), [sm.b], [sm.b])
            D_(lambda e: e.tensor_tensor(out=tg[:], in0=og[:], in1=iog[:], op=ALU.mult), [og.b, iog.b, tg.b], [tg.b])
            D_(lambda e: e.tensor_reduce(out=sm[:, 4:5], in_=tg[:], axis=AX.X, op=ALU.add), [tg.b], [sm.b])
            D_(lambda e: e.tensor_tensor(out=t3[:], in0=LG[:, NG:NR].rearrange("p (g e) -> p g e", e=8), in1=og[:].unsqueeze(2).to_broadcast([128, NG, 8]), op=ALU.mult),
               [LG.b, og.b], [t3.b])
            D_(lambda e: e.tensor_reduce(out=se[:], in_=t3[:].rearrange("p g e -> p e g"), axis=AX.X, op=ALU.add), [t3.b], [se.b])
            D_(lambda e: e.tensor_reduce(out=sm[:, 5:6], in_=se[:], axis=AX.X, op=ALU.max), [se.b], [sm.b])
            D_(lambda e: e.tensor_scalar(out=sm[:, 5:6], in0=sm[:, 5:6], scalar1=-1.0, scalar2=None, op0=ALU.mult), [sm.b], [sm.b])
            S.op("act", lambda e: e.activation(out=ee[:], in_=se[:], func=AF.Exp, bias=sm[:, 5:6]), reads=[se.b, sm.b], writes=[ee.b])
            D_(lambda e: e.max(out=m8[:], in_=ee[:]), [ee.b], [m8.b])
            D_(lambda e: e.max_index(out=i8[:], in_max=m8[:], in_values=ee[:]), [m8.b, ee.b], [i8.b])
            D_(lambda e: e.tensor_copy(out=i8f[:], in_=i8[:, 0:2]), [i8.b], [i8f.b])
            D_(lambda e: e.tensor_tensor(out=sm[:, 6:7], in0=m8[:, 0:1], in1=m8[:, 1:2], op=ALU.add), [m8.b], [sm.b])
            D_(lambda e: e.reciprocal(out=sm[:, 6:7], in_=sm[:, 6:7]), [sm.b], [sm.b])
            D_(lambda e: e.tensor_tensor(out=sm[:, 6:7], in0=sm[:, 6:7], in1=sm[:, 3:4], op=ALU.mult), [sm.b], [sm.b])
            D_(lambda e: e.tensor_scalar(out=RT[:, i, 2:4], in0=m8[:, 0:2], scalar1=sm[:, 6:7], scalar2=None, op0=ALU.mult), [m8.b, sm.b], [RT.b])
            D_(lambda e: e.scalar_tensor_tensor(out=RT[:, i, 0:2], in0=sm[:, 4:5].to_broadcast([128, 2]), scalar=8.0, in1=i8f[:], op0=ALU.mult, op1=ALU.add),
               [sm.b, i8f.b], [RT.b])
            D_(lambda e: e.tensor_scalar(out=M[:, i, :], in0=ioe[:], scalar1=RT[:, i, 0:1], scalar2=None, op0=ALU.is_equal), [ioe.b, RT.b], [M.b])
            D_(lambda e: e.tensor_scalar(out=oh[:], in0=ioe[:], scalar1=RT[:, i, 1:2], scalar2=None, op0=ALU.is_equal), [ioe.b, RT.b], [oh.b])
            D_(lambda e: e.tensor_tensor(out=M[:, i, :], in0=M[:, i, :], in1=oh[:], op=ALU.add), [M.b, oh.b], [M.b])
    S.barrier()


def phase_D0(kb, c, R, SLOT, b_SLOT):
    cfg, S = kb.cfg, kb.S
    NE, NBLK = cfg.NE, cfg.NBLK
    NTO = cfg.OWN // 128
    NQ1 = cfg.KD // min(4, cfg.KD)
    FE = cfg.FE
    with ExitStack() as es:
        mk = lambda name, shape, dt=F32: T(kb.sb(es, "Z_" + name, shape, dt))
        M, RT, ioe = R["M"], R["RT"], R["ioe"]
        triu = mk("triu", [128, 128]); ones = mk("ones", [128, 128])
        msum = mk("msum", [128, NE]); mcum = mk("mcum", [128, NE]); cnt = mk("cnt", [128, NE]); pcn = mk("pcn", [128, NE]); pend = mk("pend", [128, NE])
        poff = mk("poff", [128, NE]); ci = mk("ci", [128, NE], I32)
        j128 = mk("j128", [128, NBLK]); cmp = mk("cmp", [128, NBLK, NE]); be = mk("be", [128, NBLK])
        qp1 = mk("qp1", [128, NQ1]); qp2 = mk("qp2", [128, FE * 2]); f1 = mk("f1", [128, NBLK, NQ1]); f2 = mk("f2", [128, NBLK, FE * 2])
        dest = mk("dest", [128, NE]); oh = mk("oh", [128, NE]); dd = mk("dd", [128, NTO, 2])
        toki = mk("toki", [128, NTO, 16], I32); sinit = mk("sinit", [128, NBLK, 16], I32)
        ps = [T(kb.ps(es, "Z_ps%d" % i, [128, 512]), excl=True) for i in range(2)]
        P_ = lambda fn, reads, writes: S.op("pool", fn, reads=reads, writes=writes)
        D_ = lambda fn, reads, writes: S.op("dve", fn, reads=reads, writes=writes)
        P_(lambda e: e.memset(triu[:], 1.0), [], [triu.b])
        P_(lambda e: e.affine_select(out=triu[:], in_=triu[:], pattern=[[1, 128]], compare_op=ALU.is_gt, fill=0.0, base=0, channel_multiplier=-1), [triu.b], [triu.b])
        P_(lambda e: e.memset(ones[:], 1.0), [], [ones.b])
        P_(lambda e: e.iota(j128[:], pattern=[[128, NBLK]], base=0, channel_multiplier=0, allow_small_or_imprecise_dtypes=True), [], [j128.b])
        P_(lambda e: e.iota(qp1[:], pattern=[[128, NQ1]], base=0, channel_multiplier=1, allow_small_or_imprecise_dtypes=True), [], [qp1.b])
        P_(lambda e: e.iota(qp2[:], pattern=[[128, FE * 2]], base=0, channel_multiplier=1, allow_small_or_imprecise_dtypes=True), [], [qp2.b])
        P_(lambda e: e.iota(toki[:], pattern=[[128, NTO], [0, 16]], base=0, channel_multiplier=1), [], [toki.b])
        P_(lambda e: e.iota(sinit[:], pattern=[[0, NBLK], [0, 16]], base=cfg.OWN, channel_multiplier=0), [], [sinit.b])
        S.dma("sp", lambda e: e.dma_start(out=SLOT.rearrange("(j p) c -> p j c", p=128), in_=sinit[:]), reads=[sinit.b], writes=[b_SLOT])
        D_(lambda e: e.tensor_reduce(out=msum[:], in_=M[:].rearrange("p i e -> p e i"), axis=AX.X, op=ALU.add), [M.b], [msum.b])
        S.op("pe", lambda e: e.matmul(ps[0][:, 0:NE], lhsT=ones[:], rhs=msum[:], start=True, stop=True), reads=[ones.b, msum.b], writes=[ps[0].b])
        D_(lambda e: e.tensor_scalar(out=ci[:], in0=ps[0][:, 0:NE], scalar1=127.0, scalar2=None, op0=ALU.add), [ps[0].b], [ci.b])
        D_(lambda e: e.tensor_single_scalar(out=ci[:], in_=ci[:], scalar=7, op=ALU.arith_shift_right), [ci.b], [ci.b])
        D_(lambda e: e.tensor_single_scalar(out=ci[:], in_=ci[:], scalar=7, op=ALU.logical_shift_left), [ci.b], [ci.b])
        D_(lambda e: e.tensor_copy(out=pcn[:], in_=ci[:]), [ci.b], [pcn.b])
        D_(lambda e: e.tensor_tensor_scan(out=pend[:], data0=ones[:, 0:NE], data1=pcn[:], initial=0.0, op0=ALU.mult, op1=ALU.add), [ones.b, pcn.b], [pend.b])
        D_(lambda e: e.tensor_tensor(out=poff[:], in0=pend[:], in1=pcn[:], op=ALU.subtract), [pend.b, pcn.b], [poff.b])
        D_(lambda e: e.tensor_tensor(out=cmp[:], in0=pend[:].unsqueeze(1).to_broadcast([128, NBLK, NE]), in1=j128[:].unsqueeze(2).to_broadcast([128, NBLK, NE]), op=ALU.is_le),
           [pend.b, j128.b], [cmp.b])
        D_(lambda e: e.tensor_reduce(out=be[:], in_=cmp[:], axis=AX.X, op=ALU.add), [cmp.b], [be.b])
        big = mk("big", [128, NBLK])
        D_(lambda e: e.tensor_scalar(out=big[:], in0=be[:], scalar1=float(NE) - 0.5, scalar2=1.0e7, op0=ALU.is_ge, op1=ALU.mult), [be.b], [big.b])
        D_(lambda e: e.tensor_scalar(out=be[:], in0=be[:], scalar1=float(NE - 1), scalar2=None, op0=ALU.min), [be.b], [be.b])
        D_(lambda e: e.scalar_tensor_tensor(out=f1[:], in0=be[:].unsqueeze(2).to_broadcast([128, NBLK, NQ1]), scalar=float(NQ1 * 128),
                                            in1=qp1[:].unsqueeze(1).to_broadcast([128, NBLK, NQ1]), op0=ALU.mult, op1=ALU.add), [be.b, qp1.b], [f1.b])
        D_(lambda e: e.tensor_tensor(out=f1[:], in0=f1[:], in1=big[:].unsqueeze(2).to_broadcast([128, NBLK, NQ1]), op=ALU.add), [f1.b, big.b], [f1.b])
        D_(lambda e: e.tensor_copy(out=R["IDX1"][:], in_=f1[:]), [f1.b], [R["IDX1"].b])
        D_(lambda e: e.scalar_tensor_tensor(out=f2[:], in0=be[:].unsqueeze(2).to_broadcast([128, NBLK, FE * 2]), scalar=float(FE * 2 * 128),
                                            in1=qp2[:].unsqueeze(1).to_broadcast([128, NBLK, FE * 2]), op0=ALU.mult, op1=ALU.add), [be.b, qp2.b], [f2.b])
        D_(lambda e: e.tensor_tensor(out=f2[:], in0=f2[:], in1=big[:].unsqueeze(2).to_broadcast([128, NBLK, FE * 2]), op=ALU.add), [f2.b, big.b], [f2.b])
        D_(lambda e: e.tensor_copy(out=R["IDX2"][:], in_=f2[:]), [f2.b], [R["IDX2"].b])
        P_(lambda e: e.memset(mcum[:], 0.0), [], [mcum.b])
        for i in range(NTO):
            p = ps[i % 2]
            S.op("pe", lambda e: e.matmul(p[:, 0:NE], lhsT=triu[:], rhs=M[:, i, :], start=True, stop=False), reads=[triu.b, M.b], writes=[p.b])
            S.op("pe", lambda e: e.matmul(p[:, 0:NE], lhsT=ones[:], rhs=mcum[:], start=False, stop=True), reads=[ones.b, mcum.b], writes=[p.b])
            D_(lambda e: e.tensor_tensor(out=dest[:], in0=p[:, 0:NE], in1=poff[:], op=ALU.add), [p.b, poff.b], [dest.b])
            D_(lambda e: e.tensor_tensor(out=mcum[:], in0=mcum[:], in1=M[:, i, :], op=ALU.add), [mcum.b, M.b], [mcum.b])
            for a in range(2):
                D_(lambda e: e.tensor_scalar(out=oh[:], in0=ioe[:], scalar1=RT[:, i, a:a + 1], scalar2=None, op0=ALU.is_equal), [ioe.b, RT.b], [oh.b])
                D_(lambda e: e.tensor_tensor(out=oh[:], in0=oh[:], in1=dest[:], op=ALU.mult), [oh.b, dest.b], [oh.b])
                D_(lambda e: e.tensor_reduce(out=dd[:, i, a:a + 1], in_=oh[:], axis=AX.X, op=ALU.add), [oh.b], [dd.b])
        D_(lambda e: e.tensor_copy(out=R["DEST"][:], in_=dd[:]), [dd.b], [R["DEST"].b])
        for i in range(NTO):
            for a in range(2):
                S.dma("pool", lambda e: e.indirect_dma_start(out=SLOT, out_offset=bass.IndirectOffsetOnAxis(ap=R["DEST"][:, i, a:a + 1], axis=0),
                                                             in_=toki[:, i, :], in_offset=None), reads=[R["DEST"].b, toki.b], writes=[b_SLOT])
    S.barrier()


def phase_D(kb, c, R, SLOT, b_SLOT, X2, b_X2, w1L, w3L, w2L, Y, b_Y):
    cfg, S = kb.cfg, kb.S
    D, KD, DE, FE, NBLK = cfg.D, cfg.KD, cfg.DE, cfg.FE, cfg.NBLK
    KQ = min(4, KD)
    NQ1 = KD // KQ
    DH = D // 2
    CW = min(512, DH)
    ech = [(o, min(512, DE - o)) for o in range(0, DE, 512)]
    with ExitStack() as es:
        mk = lambda name, shape, dt=F32: T(kb.sb(es, "M_" + name, shape, dt))
        tok = [mk("tok%d" % i, [128, 16], I32) for i in range(2)]
        xg = [mk("xg%d" % i, [128, D], BF16) for i in range(2)]
        xbT = mk("xbT", [128, KD, 128], BF16)
        NWB = 4
        wb = [[mk("wb%d_%d" % (a, i), [128, KQ, DE], BF16) for i in range(NWB)] for a in range(2)]
        w2b = [mk("w2b%d" % i, [128, FE, DH], BF16) for i in range(3)]
        actf = mk("actf", [128, DE]); actb = mk("actb", [128, DE], BF16); actT = mk("actT", [128, FE, 128], BF16)
        yst = mk("yst", [128, D])
        psT = [T(kb.ps(es, "M_psT", [128, 1024], BF16), excl=True)]
        ph = [[T(kb.ps(es, "M_ph%d_%d" % (a, i), [128, 512]), excl=True) for i in range(len(ech))] for a in range(2)]
        py = [T(kb.ps(es, "M_py%d" % i, [128, 512]), excl=True) for i in range(2)]
        IDX1, IDX2 = R["IDX1"], R["IDX2"]
        bc1 = kb.nc.gpsimd.to_reg(cfg.NE * NQ1 * 128 - 1)
        bc2 = kb.nc.gpsimd.to_reg(cfg.NE * FE * 2 * 128 - 1)
        for t_ in wb[0] + wb[1] + w2b:
            S.op("pool", lambda e: e.memset(t_[:], 0.0), writes=[t_.b])
        w2c = 0
        pcnt = [0]
        cst = 0
        yc = 0
        for j in range(NBLK):
            tk = tok[j % 2]; x_ = xg[j % 2]
            S.dma("sp", lambda e: e.dma_start(out=tk[:], in_=SLOT[j * 128:(j + 1) * 128, :]), reads=[b_SLOT], writes=[tk.b])
            S.dma("pool", lambda e: e.indirect_dma_start(out=x_[:], out_offset=None, in_=X2, in_offset=bass.IndirectOffsetOnAxis(ap=tk[:, 0:1], axis=0)),
                  reads=[tk.b, b_X2], writes=[x_.b])
            for ti in range(1):
                transpose_bf(kb, c, S, x_, KD, xbT, 0, psT, pcnt)
            for q in range(NQ1):
                wq = (j * NQ1 + q) % NWB
                for a, wl in enumerate((w1L, w3L)):
                    b_ = wb[a][wq]
                    S.dma("pool", lambda e: e.indirect_dma_start(out=b_[:].rearrange("p a b -> p (a b)"), out_offset=None, in_=wl,
                                                                 in_offset=bass.IndirectOffsetOnAxis(ap=IDX1[:, j, q:q + 1], axis=0),
                                                                 bounds_check=bc1, oob_is_err=False),
                          reads=[IDX1.b], writes=[b_.b])
                for kk in range(KQ):
                    k = q * KQ + kk
                    for a in range(2):
                        for ci_, (o, wdt) in enumerate(ech):
                            S.op("pe", lambda e: e.matmul(ph[a][ci_][:, 0:wdt], lhsT=xbT[:, k, :], rhs=wb[a][wq][:, kk, o:o + wdt], start=(k == 0), stop=(k == KD - 1)),
                                 reads=[xbT.b, wb[a][wq].b], writes=[ph[a][ci_].b])
            for ci_, (o, wdt) in enumerate(ech):
                S.op("act", lambda e: e.activation(out=actf[:, o:o + wdt], in_=ph[0][ci_][:, 0:wdt], func=AF.Silu), reads=[ph[0][ci_].b], writes=[actf.b])
                S.op("dve", lambda e: e.tensor_tensor(out=actb[:, o:o + wdt], in0=actf[:, o:o + wdt], in1=ph[1][ci_][:, 0:wdt], op=ALU.mult),
                     reads=[actf.b, ph[1][ci_].b], writes=[actb.b])
            transpose_bf(kb, c, S, actb, FE, actT, 0, psT, pcnt)
            for hf in range(2):
                w2b_ = w2b[w2c % 3]; w2c += 1
                for f in range(FE):
                    col = f * 2 + hf
                    S.dma("pool", lambda e: e.indirect_dma_start(out=w2b_[:, f, :], out_offset=None, in_=w2L, in_offset=bass.IndirectOffsetOnAxis(ap=IDX2[:, j, col:col + 1], axis=0),
                                                                 bounds_check=bc2, oob_is_err=False),
                          reads=[IDX2.b], writes=[w2b_.b])
                for cc in range(DH // CW):
                    p = py[yc % 2]; yc += 1
                    for f in range(FE):
                        S.op("pe", lambda e: e.matmul(p[:, 0:CW], lhsT=actT[:, f, :], rhs=w2b_[:, f, cc * CW:(cc + 1) * CW], start=(f == 0), stop=(f == FE - 1)),
                             reads=[actT.b, w2b_.b], writes=[p.b])
                    o0 = hf * DH + cc * CW
                    if yc % 2:
                        S.op("act", lambda e: e.copy(out=yst[:, o0:o0 + CW], in_=p[:, 0:CW]), reads=[p.b], writes=[yst.b])
                    else:
                        S.op("dve", lambda e: e.tensor_copy(out=yst[:, o0:o0 + CW], in_=p[:, 0:CW]), reads=[p.b], writes=[yst.b])
            S.dma("sp", lambda e: e.dma_start(out=Y[j * 128:(j + 1) * 128, :], in_=yst[:]), reads=[yst.b], writes=[b_Y])
    S.barrier()


def phase_E(kb, c, R, XMID, b_XMID, Y, b_Y, fgd, out, b_out):
    cfg, S = kb.cfg, kb.S
    D = cfg.D
    NTO = cfg.OWN // 128
    with ExitStack() as es:
        mk = lambda name, shape, dt=F32: T(kb.sb(es, "E_" + name, shape, dt))
        xm = [mk("xm%d" % i, [128, D]) for i in range(2)]
        ya = [mk("ya%d" % i, [128, D]) for i in range(2)]
        yb = [mk("yb%d" % i, [128, D]) for i in range(2)]
        sq = mk("sq", [128, D]); ss = mk("ss", [128, 1]); fg = mk("fg", [128, D])
        S.dma("sp", lambda e: e.dma_start(out=fg[:], in_=fgd.partition_broadcast(128)), writes=[fg.b])
        RT, DEST = R["RT"], R["DEST"]
        for i in range(NTO):
            r0 = i * 128
            x_, a_, b_ = xm[i % 2], ya[i % 2], yb[i % 2]
            S.dma("sp", lambda e: e.dma_start(out=x_[:], in_=XMID[r0:r0 + 128, :]), reads=[b_XMID], writes=[x_.b])
            S.dma("pool", lambda e: e.indirect_dma_start(out=a_[:], out_offset=None, in_=Y, in_offset=bass.IndirectOffsetOnAxis(ap=DEST[:, i, 0:1], axis=0)),
                  reads=[DEST.b, b_Y], writes=[a_.b])
            S.dma("pool", lambda e: e.indirect_dma_start(out=b_[:], out_offset=None, in_=Y, in_offset=bass.IndirectOffsetOnAxis(ap=DEST[:, i, 1:2], axis=0)),
                  reads=[DEST.b, b_Y], writes=[b_.b])
            S.op("dve", lambda e: e.scalar_tensor_tensor(out=x_[:], in0=a_[:], scalar=RT[:, i, 2:3], in1=x_[:], op0=ALU.mult, op1=ALU.add),
                 reads=[a_.b, RT.b, x_.b], writes=[x_.b])
            S.op("dve", lambda e: e.scalar_tensor_tensor(out=x_[:], in0=b_[:], scalar=RT[:, i, 3:4], in1=x_[:], op0=ALU.mult, op1=ALU.add),
                 reads=[b_.b, RT.b, x_.b], writes=[x_.b])
            S.op("act", lambda e: e.activation(out=sq[:], in_=x_[:], func=AF.Square, accum_out=ss[:]), reads=[x_.b], writes=[sq.b, ss.b])
            rmsnorm_rstd(kb, c, ss[:], ss.b, D)
            S.op("dve", lambda e: e.scalar_tensor_tensor(out=a_[:], in0=x_[:], scalar=ss[:, 0:1], in1=fg[:], op0=ALU.mult, op1=ALU.mult),
                 reads=[x_.b, ss.b, fg.b, a_.b], writes=[a_.b])
            S.dma("sp", lambda e: e.dma_start(out=out[r0:r0 + 128, :], in_=a_[:]), reads=[a_.b], writes=[b_out])
    S.barrier()


def build_program(cfg, debug=(), phases="ABCDE"):
    kb = K(cfg, debug)
    nc = kb.nc
    D, S_, KD, OWN, NE, NG = cfg.D, cfg.S, cfg.KD, cfg.OWN, cfg.NE, cfg.NG
    NTO = OWN // 128
    KQ = min(4, KD); NQ1 = KD // KQ; FE = cfg.FE
    xb = kb.inp("xb", [S_, D])
    xo = kb.inp("xo", [OWN, D])
    own_idx = kb.inp("own_idx", [128, NTO], I32)
    g1d = kb.inp("norm1_g", [1, D])
    wA = kb.inp("wA", [cfg.NA, 128, KD, 128])
    wG = kb.inp("wG", [2 * D // 512, KD // cfg.KP, 128, cfg.KP, 512])
    gbd = kb.inp("gate_b", [1, 2 * D])
    prm = {}
    for name, shape in (("rwp", [64, 10, cfg.RWH]), ("lmu", [128, 4]), ("rw_w2", [96, cfg.RW]), ("rw_a2", [96, cfg.RW]),
                        ("rw_g2", [256, cfg.RW]), ("dnp", [128, 12, cfg.DNH]), ("dnw", [128, 1]), ("dnh", [16, 2])):
        prm[name] = kb.inp(name, shape)
    wBR = kb.inp("wBR", [D // 512, 128, cfg.KB, 512])
    wO = kb.inp("wO", [D // 512, 128, KD, 512])
    g2d = kb.inp("norm2_g", [1, D])
    fgd = kb.inp("final_g", [1, D])
    wRd = kb.inp("wR", [128, KD, NG + NE])
    bRd = kb.inp("bR", [1, NG + NE])
    w1L = kb.inp("w1L", [NE * NQ1 * 128, KQ * cfg.DE])
    w3L = kb.inp("w3L", [NE * NQ1 * 128, KQ * cfg.DE])
    w2L = kb.inp("w2L", [NE * FE * 2 * 128, D // 2])
    PT = kb.scratch("PT", [cfg.NA * 128, S_]); b_PT = [Buf() for _ in range(cfg.NA)]
    OMIX = kb.scratch("OMIX", [S_, cfg.BW], BF16); b_OMIX = Buf()
    GATES = kb.scratch("GATES", [OWN, 2 * D]); b_G = Buf()
    XMID = kb.scratch("XMID", [OWN, D]); b_XMID = Buf()
    X2 = kb.scratch("X2", [OWN + 128, D], BF16); b_X2 = Buf()
    SLOT = kb.scratch("SLOT", [cfg.NBLK * 128, 16], I32); b_SLOT = Buf()
    Y = kb.scratch("Y", [cfg.NBLK * 128, D]); b_Y = Buf()
    out = nc.dram_tensor("out", [OWN, D], F32, kind="ExternalOutput").ap(); b_out = Buf()
    with ExitStack() as es:
        kb.S = Sched(nc, es)
        S = kb.S
        c = build_consts(kb, es)
        R = {}
        R["M"] = T(kb.sb(es, "r_M", [128, NTO, NE])); R["RT"] = T(kb.sb(es, "r_RT", [128, NTO, 4])); R["ioe"] = T(kb.sb(es, "r_ioe", [128, NE]))
        R["IDX1"] = T(kb.sb(es, "r_IDX1", [128, cfg.NBLK, NQ1], I32)); R["IDX2"] = T(kb.sb(es, "r_IDX2", [128, cfg.NBLK, FE * 2], I32))
        R["DEST"] = T(kb.sb(es, "r_DEST", [128, NTO, 2], I32))
        S.op("pool", lambda e: e.iota(R["ioe"][:], pattern=[[1, NE]], base=0, channel_multiplier=0, allow_small_or_imprecise_dtypes=True), writes=[R["ioe"].b])
        if "A" in phases:
            phase_A(kb, c, xb, g1d, wA, PT, b_PT)
            phase_A1(kb, c, xo, g1d, wG, gbd, GATES, b_G)
        if "B" in phases:
            phase_B(kb, c, PT, b_PT, prm, OMIX, b_OMIX)
        if "C" in phases:
            phase_C1(kb, c, xo, own_idx, OMIX, b_OMIX, GATES, b_G, wBR, wO, XMID, b_XMID)
            phase_C2(kb, c, XMID, b_XMID, g2d, wRd, bRd, X2, b_X2, R)
        if "D" in phases:
            phase_D0(kb, c, R, SLOT, b_SLOT)
            phase_D(kb, c, R, SLOT, b_SLOT, X2, b_X2, w1L, w3L, w2L, Y, b_Y)
        if "E" in phases:
            phase_E(kb, c, R, XMID, b_XMID, Y, b_Y, fgd, out, b_out)
        S.barrier()
        print("ninstr", S.ninstr)
    return kb


def prep_shared(cfg, inp):
    sh = {}
    D, KD, RW, DN, RWH, DNH, NE, NG, DE, FE = cfg.D, cfg.KD, cfg.RW, cfg.DN, cfg.RWH, cfg.DNH, cfg.NE, cfg.NG, cfg.DE, cfg.FE
    w_in = inp["w_in"][0]
    ca = cfg.colsA()
    wa = np.zeros((D, cfg.NA * 128), np.float32)
    m = ca >= 0
    wa[:, m] = w_in[:, ca[m]]
    sh["wA"] = np.ascontiguousarray(wa.reshape(KD, 128, cfg.NA, 128).transpose(2, 1, 0, 3))
    del wa
    wg = w_in[:, cfg.GB0:cfg.GB0 + 2 * D]
    KP = cfg.KP
    sh["wG"] = np.ascontiguousarray(wg.reshape(KD // KP, KP, 128, 2 * D // 512, 512).transpose(3, 0, 2, 1, 4))
    sh["gate_b"] = np.ascontiguousarray(inp["gate_b"].reshape(1, 2 * D))
    sh["norm1_g"] = np.ascontiguousarray(inp["norm1_g"].reshape(1, D))
    sh["norm2_g"] = np.ascontiguousarray(inp["norm2_g"].reshape(1, D))
    sh["final_g"] = np.ascontiguousarray(inp["final_g"].reshape(1, D))
    mu = inp["rw_mu"][0]
    vecs = [mu[0:RW], mu[RW + 96:2 * RW + 96], mu[2 * RW + 96:3 * RW + 96], inp["rw_w0"][0], inp["rw_a0"][0], inp["rw_k_k"][0],
            inp["rw_k_a"][0], inp["rw_r_k"][0].reshape(-1), inp["rw_ln_w"][0], inp["rw_ln_b"][0]]
    sh["rwp"] = np.ascontiguousarray(np.stack([v.reshape(RWH, 64).T for v in vecs], axis=1).astype(np.float32))
    lmu = np.zeros((128, 4), np.float32)
    lmu[0:96, 0] = mu[RW:RW + 96]
    lmu[0:96, 1] = mu[3 * RW + 96:3 * RW + 192]
    lmu[:, 2] = mu[3 * RW + 192:3 * RW + 320]
    lmu[:, 3] = mu[3 * RW + 320:3 * RW + 448]
    sh["lmu"] = lmu
    sh["rw_w2"] = np.ascontiguousarray(inp["rw_w2"][0])
    sh["rw_a2"] = np.ascontiguousarray(inp["rw_a2"][0])
    sh["rw_g2"] = np.ascontiguousarray(inp["rw_g2"][0])
    cw = inp["dn_conv_w"][0]
    sh["dnp"] = np.ascontiguousarray(cw.reshape(4, 3, DNH, 128).transpose(3, 0, 1, 2).reshape(128, 12, DNH))
    sh["dnw"] = np.ascontiguousarray(inp["dn_norm_w"][0].reshape(128, 1))
    dnh = np.zeros((16, 2), np.float32)
    dnh[:DNH, 0] = inp["dn_a_log"][0]
    dnh[:DNH, 1] = inp["dn_dt_bias"][0]
    sh["dnh"] = dnh
    wb = inp["w_branch"][0]
    sh["wBR"] = np.ascontiguousarray(wb.reshape(cfg.KB, 128, D // 512, 512).transpose(2, 1, 0, 3))
    wo = inp["w_out"][0]
    sh["wO"] = np.ascontiguousarray(wo.reshape(KD, 128, D // 512, 512).transpose(2, 1, 0, 3))
    wr = np.concatenate([inp["moe_gr_w"][0], inp["moe_er_w"][0]], axis=1)
    sh["wR"] = np.ascontiguousarray(wr.reshape(KD, 128, NG + NE).transpose(1, 0, 2))
    sh["bR"] = np.ascontiguousarray(np.concatenate([inp["moe_gr_b"][0], inp["moe_er_b"][0]]).reshape(1, NG + NE))
    KQ = min(4, KD); NQ1 = KD // KQ
    for nm, key in (("w1L", "moe_w1"), ("w3L", "moe_w3")):
        w = inp[key][0]
        sh[nm] = np.ascontiguousarray(w.reshape(NE, NQ1, KQ, 128, DE).transpose(0, 1, 3, 2, 4)).reshape(NE * NQ1 * 128, KQ * DE)
    w = inp["moe_w2"][0]
    sh["w2L"] = np.ascontiguousarray(w.reshape(NE, FE, 128, 2, D // 2).transpose(0, 1, 3, 2, 4)).reshape(NE * FE * 2 * 128, D // 2)
    return sh


def core_inputs(cfg, inp, sh, b, s):
    d = dict(sh)
    OWN = cfg.OWN
    d["xb"] = np.ascontiguousarray(inp["x"][b])
    d["xo"] = np.ascontiguousarray(inp["x"][b, s * OWN:(s + 1) * OWN])
    d["own_idx"] = np.ascontiguousarray((s * OWN + np.arange(OWN, dtype=np.int32)).reshape(OWN // 128, 128).T)
    return d


_CACHE = {}


def kernel(**inputs):
    cfg = Cfg()
    inp = {k: np.asarray(v) for k, v in inputs.items()}
    if "kb" not in _CACHE:
        _CACHE["kb"] = build_program(cfg)
    kb = _CACHE["kb"]
    sh = prep_shared(cfg, inp)
    B = inp["x"].shape[0]
    ins = []
    for cid in range(8):
        b, s = cid // 2, cid % 2
        d = core_inputs(cfg, inp, sh, b % B, s)
        ins.append({k: v for k, v in d.items() if k in kb.din})
    res = run_bass_kernel_spmd(kb.nc, ins, core_ids=list(range(8)))
    out = np.zeros((B, cfg.S, cfg.D), np.float32)
    for cid in range(8):
        b, s = cid // 2, cid % 2
        out[b, s * cfg.OWN:(s + 1) * cfg.OWN] = res.results[cid]["out"]
    return out
```

```python
import os
import numpy as np
from contextlib import ExitStack
KSTOP = float(os.environ.get('KSTOP', '99'))
BQ = tuple(int(v) for v in os.environ.get('BQ', '1,6').split(','))
FILL = int(os.environ.get('FILL', '1'))
import concourse.bass as bass
import concourse.mybir as mybir
from concourse.bass_utils import run_bass_kernel_spmd

F32 = mybir.dt.float32
BF16 = mybir.dt.bfloat16
I32 = mybir.dt.int32
U32 = mybir.dt.uint32
AF = mybir.ActivationFunctionType
ALU = mybir.AluOpType
AX = mybir.AxisListType

CH = 64


class Cfg:
    def __init__(self, D=4096, S=4096, RWH=32, DNH=16, NG=8, DE=768, SEG=512):
        self.D, self.S, self.RWH, self.DNH, self.NG, self.DE = D, S, RWH, DNH, NG, DE
        self.KD = D // 128
        self.RW = RWH * 64
        self.DN = DNH * 128
        self.BW = self.RW + self.DN
        self.KB = self.BW // 128
        self.OWN = S // 2
        self.NE = NG * 8
        self.TG = min(1024, S)
        self.TGO = min(512, self.OWN)
        self.SEG = min(SEG, S)
        self.KP = min(8, self.KD)
        self.RW_COLS = 3 * self.RW + 448
        self.DN_COLS = 3 * self.DN + 2 * DNH + self.DN
        self.GB0 = self.RW_COLS + self.DN_COLS
        self.IN_COLS = self.GB0 + 2 * D
        rows = {}
        o = 0
        for name, n in (("r", self.RW), ("k", self.RW), ("v", self.RW), ("wd", 128), ("ad", 128), ("gd", 256),
                        ("dq", self.DN), ("dk", self.DN), ("dv", self.DN), ("ba", 128), ("z", self.DN)):
            rows[name] = o
            o += n
        self.rows = rows
        self.NA = o // 128
        self.NBLK = (2 * self.OWN + self.NE * 128) // 128
        self.FE = DE // 128

    def colsA(self):
        c = self
        RW, DN, DNH = c.RW, c.DN, c.DNH
        idx = -np.ones(c.NA * 128, np.int64)
        r = c.rows
        idx[r["r"]:r["r"] + RW] = np.arange(0, RW)
        idx[r["wd"]:r["wd"] + 96] = np.arange(RW, RW + 96)
        idx[r["k"]:r["k"] + RW] = np.arange(RW + 96, 2 * RW + 96)
        idx[r["v"]:r["v"] + RW] = np.arange(2 * RW + 96, 3 * RW + 96)
        idx[r["ad"]:r["ad"] + 96] = np.arange(3 * RW + 96, 3 * RW + 192)
        idx[r["gd"]:r["gd"] + 256] = np.arange(3 * RW + 192, 3 * RW + 448)
        b0 = c.RW_COLS
        idx[r["dq"]:r["dq"] + DN] = b0 + np.arange(0, DN)
        idx[r["dk"]:r["dk"] + DN] = b0 + np.arange(DN, 2 * DN)
        idx[r["dv"]:r["dv"] + DN] = b0 + np.arange(2 * DN, 3 * DN)
        idx[r["ba"]:r["ba"] + DNH] = b0 + 3 * DN + np.arange(0, DNH)
        idx[r["ba"] + 32:r["ba"] + 32 + DNH] = b0 + 3 * DN + DNH + np.arange(0, DNH)
        idx[r["z"]:r["z"] + DN] = b0 + 3 * DN + 2 * DNH + np.arange(0, DN)
        return idx


class Buf:
    __slots__ = ("w", "r", "x")

    def __init__(self, excl=False):
        self.w = {}
        self.r = {}
        self.x = excl


class Sched:
    def __init__(self, nc, es, n_dma_sems=24):
        self.nc = nc
        self.eng = {"pe": nc.tensor, "act": nc.scalar, "dve": nc.vector, "pool": nc.gpsimd, "sp": nc.sync}
        self.sems = []
        self.semidx = {}
        self.cnt = {}
        for k in ("pe", "act", "dve", "pool"):
            s = es.enter_context(nc.semaphore("s_" + k))
            self.semidx[k] = len(self.sems)
            self.sems.append(s)
            self.cnt[k] = 0
        self.dma_ids = []
        for i in range(n_dma_sems):
            s = es.enter_context(nc.semaphore("s_d%d" % i))
            self.dma_ids.append(len(self.sems))
            self.sems.append(s)
        self.dtotal = {i: 0 for i in self.dma_ids}
        self.dnext = 0
        self.known = {k: {} for k in self.eng}
        self.ninstr = 0
        self.tick = None

    def _wait(self, ek, need):
        kn = self.known[ek]
        e = self.eng[ek]
        own = self.semidx.get(ek)
        for s, v in need.items():
            if ek == "pe" and s == own:
                continue
            if kn.get(s, 0) < v:
                e.wait_ge(self.sems[s], v)
                kn[s] = v
                self.ninstr += 1

    def _deps(self, ek, reads, writes):
        need = {}
        for b in reads:
            if b.x:
                for s, v in b.r.items():
                    if need.get(s, 0) < v:
                        need[s] = v
            for s, v in b.w.items():
                if need.get(s, 0) < v:
                    need[s] = v
        for b in writes:
            for s, v in b.w.items():
                if need.get(s, 0) < v:
                    need[s] = v
            for s, v in b.r.items():
                if need.get(s, 0) < v:
                    need[s] = v
        self._wait(ek, need)

    def op(self, ek, fn, reads=(), writes=()):
        if self.tick is not None:
            self.tick()
        self._deps(ek, reads, writes)
        ins = fn(self.eng[ek])
        self.cnt[ek] += 1
        s = self.semidx[ek]
        ins.then_inc(self.sems[s], 1)
        v = self.cnt[ek]
        for b in reads:
            b.r[s] = v
            if b.x:
                b.w[s] = v
        for b in writes:
            b.w[s] = v
        self.ninstr += 1
        return ins

    def dma(self, qk, fn, reads=(), writes=()):
        if self.tick is not None:
            self.tick()
        self._deps(qk, reads, writes)
        s = self.dma_ids[self.dnext]
        self.dnext = (self.dnext + 1) % len(self.dma_ids)
        prev = self.dtotal[s]
        kn = self.known[qk]
        if prev and kn.get(s, 0) < prev:
            self.eng[qk].wait_ge(self.sems[s], prev)
            kn[s] = prev
        ins = fn(self.eng[qk])
        ins.then_inc(self.sems[s], 16)
        v = prev + 16
        self.dtotal[s] = v
        for b in reads:
            b.r[s] = v
        for b in writes:
            b.w[s] = v
        self.ninstr += 1
        return ins

    def barrier(self):
        need = {self.semidx[k]: self.cnt[k] for k in self.cnt if self.cnt[k]}
        for s, v in self.dtotal.items():
            if v:
                need[s] = v
        for ek in self.eng:
            self._wait(ek, dict(need))


import threading
_tl = threading.local()


def run_pair(S, f0, f1, quota=(1, 3)):
    if f0 is None:
        return f1()
    if f1 is None:
        return f0()
    st = {"turn": 0, "alive": [True, True], "used": 0, "err": None}
    cv = threading.Condition()

    def tick():
        tid = _tl.tid
        with cv:
            st["used"] += 1
            if st["used"] >= quota[tid] and st["alive"][1 - tid]:
                st["used"] = 0
                st["turn"] = 1 - tid
                cv.notify_all()
                while st["turn"] != tid:
                    cv.wait()

    def worker(tid, f):
        _tl.tid = tid
        with cv:
            while st["turn"] != tid:
                cv.wait()
        try:
            f()
        except BaseException as e:
            st["err"] = e
        finally:
            with cv:
                st["alive"][tid] = False
                st["used"] = 0
                st["turn"] = 1 - tid
                cv.notify_all()
    S.tick = tick
    ths = [threading.Thread(target=worker, args=(i, f)) for i, f in enumerate((f0, f1))]
    for t in ths:
        t.start()
    for t in ths:
        t.join()
    S.tick = None
    if st["err"] is not None:
        raise st["err"]


class K:
    def __init__(self, cfg, debug=()):
        self.cfg = cfg
        self.debug = set(debug)
        self.nc = bass.Bass("TRN2", target_bir_lowering=False)
        self.es = ExitStack()
        self.S = None
        self.din = {}

    def inp(self, name, shape, dt=F32):
        t = self.nc.dram_tensor(name, list(shape), dt, kind="ExternalInput").ap()
        self.din[name] = t
        return t

    def scratch(self, name, shape, dt=F32):
        kind = "ExternalOutput" if name in self.debug else "Internal"
        return self.nc.dram_tensor(name, list(shape), dt, kind=kind).ap()

    def sb(self, es, name, shape, dt=F32):
        return es.enter_context(self.nc.sbuf_tensor(name, list(shape), dt))

    def ps(self, es, name, shape, dt=F32):
        return es.enter_context(self.nc.psum_tensor(name, list(shape), dt))


def build_consts(kb, es):
    S, nc = kb.S, kb.nc
    c = {}
    ident = kb.sb(es, "ident", [128, 128]); b = Buf()
    S.op("pool", lambda e: e.memset(ident[:], 0.0), writes=[b])
    S.op("pool", lambda e: e.affine_select(out=ident[:], in_=ident[:], pattern=[[-1, 128]], compare_op=ALU.not_equal,
                                           fill=1.0, base=0, channel_multiplier=1), reads=[b], writes=[b])
    identb = kb.sb(es, "identb", [128, 128], BF16)
    S.op("dve", lambda e: e.tensor_copy(out=identb[:], in_=ident[:]), reads=[b], writes=[b])
    c["ident"], c["identb"], c["b_ident"] = ident, identb, b
    col = kb.sb(es, "ccol", [128, 8]); bc = Buf()
    for i, v in enumerate((1e-6, 64e-5, 1.0, 0.0, -1.0)):
        S.op("pool", lambda e: e.memset(col[:, i:i + 1], v), writes=[bc])
    c["col"], c["b_col"] = col, bc
    c["EPS6"], c["GNEPS"], c["ONE"], c["ZERO"] = col[:, 0:1], col[:, 1:2], col[:, 2:3], col[:, 3:4]
    return c


def rmsnorm_rstd(kb, c, ss, b_ss, n, nparts=128):
    S = kb.S
    S.op("dve", lambda e: e.tensor_scalar(out=ss, in0=ss, scalar1=1.0 / n, scalar2=1e-6, op0=ALU.mult, op1=ALU.add),
         reads=[b_ss], writes=[b_ss])
    S.op("act", lambda e: e.activation(out=ss, in_=ss, func=AF.Ln), reads=[b_ss], writes=[b_ss])
    S.op("act", lambda e: e.activation(out=ss, in_=ss, func=AF.Exp, scale=-0.5), reads=[b_ss], writes=[b_ss])


def phase_A(kb, c, xb, g1d, wA, PT, b_PT):
    cfg, S, nc = kb.cfg, kb.S, kb.nc
    D, KD, TG, NA = cfg.D, cfg.KD, cfg.TG, cfg.NA
    with ExitStack() as es:
        hT = kb.sb(es, "A_hT", [128, KD, TG], BF16); b_hT = Buf()
        xt = kb.sb(es, "A_xt", [128, D]); b_xt = Buf()
        hb = kb.sb(es, "A_hb", [128, D], BF16); b_hb = Buf()
        ss = kb.sb(es, "A_ss", [128, 1]); b_ss = Buf()
        g1 = kb.sb(es, "A_g1", [128, D]); b_g1 = Buf()
        slf = [kb.sb(es, "A_slf%d" % i, [128, KD, 128]) for i in range(2)]; b_slf = [Buf(), Buf()]
        slb = [kb.sb(es, "A_slb%d" % i, [128, KD, 128], BF16) for i in range(2)]; b_slb = [Buf(), Buf()]
        stg = [kb.sb(es, "A_stg%d" % i, [128, TG]) for i in range(2)]; b_stg = [Buf(), Buf()]
        psT = [kb.ps(es, "A_psT%d" % i, [128, 1024], BF16) for i in range(2)]; b_psT = [Buf(True), Buf(True)]
        psA = [kb.ps(es, "A_psA%d" % i, [128, 512]) for i in range(4)]; b_psA = [Buf(True) for _ in range(4)]
        S.dma("sp", lambda e: e.dma_start(out=g1[:], in_=g1d.partition_broadcast(128)), writes=[b_g1])
        it = 0
        pcnt = 0
        for tg in range(cfg.S // TG):
            for ti in range(TG // 128):
                t0 = tg * TG + ti * 128
                S.dma("sp", lambda e: e.dma_start(out=xt[:], in_=xb[t0:t0 + 128, :]), writes=[b_xt])
                S.op("act", lambda e: e.activation(out=hb[:], in_=xt[:], func=AF.Square, accum_out=ss[:]),
                     reads=[b_xt], writes=[b_hb, b_ss])
                rmsnorm_rstd(kb, c, ss[:], b_ss, D)
                S.op("dve", lambda e: e.scalar_tensor_tensor(out=hb[:], in0=xt[:], scalar=ss[:, 0:1], in1=g1[:],
                                                             op0=ALU.mult, op1=ALU.mult),
                     reads=[b_xt, b_ss, b_g1], writes=[b_hb])
                for k0 in range(0, KD, 8):
                    kn = min(8, KD - k0)
                    p = psT[pcnt % 2]; bp = b_psT[pcnt % 2]; pcnt += 1
                    for k in range(kn):
                        S.op("pe", lambda e: e.transpose(out=p[:, k * 128:(k + 1) * 128],
                                                         in_=hb[:, (k0 + k) * 128:(k0 + k + 1) * 128],
                                                         identity=c["identb"][:]),
                             reads=[b_hb, c["b_ident"]], writes=[bp])
                    ek = "act" if (pcnt % 2) else "dve"
                    src = p[:, 0:kn * 128].rearrange("p (a b) -> p a b", a=kn)
                    dst = hT[:, k0:k0 + kn, ti * 128:(ti + 1) * 128]
                    if ek == "act":
                        S.op("act", lambda e: e.copy(out=dst, in_=src), reads=[bp], writes=[b_hT])
                    else:
                        S.op("dve", lambda e: e.tensor_copy(out=dst, in_=src), reads=[bp], writes=[b_hT])
            for j in range(NA):
                sl = it % 2
                S.dma("sp", lambda e: e.dma_start(out=slf[sl][:], in_=wA[j]), writes=[b_slf[sl]])
                if it % 2 == 0:
                    S.op("act", lambda e: e.copy(out=slb[sl][:], in_=slf[sl][:]), reads=[b_slf[sl]], writes=[b_slb[sl]])
                else:
                    S.op("dve", lambda e: e.tensor_copy(out=slb[sl][:], in_=slf[sl][:]), reads=[b_slf[sl]], writes=[b_slb[sl]])
                for hf in range(TG // 512 if TG >= 512 else 1):
                    w = min(512, TG)
                    p = psA[pcnt % 4]; bp = b_psA[pcnt % 4]; pcnt += 1
                    for k in range(KD):
                        S.op("pe", lambda e: e.matmul(p[:, 0:w], lhsT=slb[sl][:, k, :], rhs=hT[:, k, hf * w:(hf + 1) * w],
                                                      start=(k == 0), stop=(k == KD - 1)),
                             reads=[b_slb[sl], b_hT], writes=[bp])
                    if pcnt % 2:
                        S.op("dve", lambda e: e.tensor_copy(out=stg[sl][:, hf * w:(hf + 1) * w], in_=p[:, 0:w]),
                             reads=[bp], writes=[b_stg[sl]])
                    else:
                        S.op("act", lambda e: e.copy(out=stg[sl][:, hf * w:(hf + 1) * w], in_=p[:, 0:w]),
                             reads=[bp], writes=[b_stg[sl]])
                S.dma("pool", lambda e: e.dma_start(out=PT[j * 128:(j + 1) * 128, tg * TG:(tg + 1) * TG], in_=stg[sl][:]),
                      reads=[b_stg[sl]], writes=[b_PT[j]])
                it += 1
    S.barrier()


RW_DECAY_SCALE = 0.606531


def build_masks(kb, es, c):
    S = kb.S
    m4 = kb.sb(es, "m4", [128, 128]); b = Buf()
    S.op("pool", lambda e: e.memset(m4[0:64, 0:64], 0.0), writes=[b])
    for (r0, c0, val, op) in ((0, 64, -1.0, ALU.is_ge), (64, 0, 1.0, ALU.is_gt), (64, 64, 1.0, ALU.is_ge)):
        blk = m4[r0:r0 + 64, c0:c0 + 64]
        S.op("pool", lambda e: e.memset(blk, val), writes=[b])
        S.op("pool", lambda e: e.affine_select(out=blk, in_=blk, pattern=[[1, 64]], compare_op=op, fill=0.0, base=0,
                                               channel_multiplier=-1), reads=[b], writes=[b])
    ml = kb.sb(es, "ml", [64, 64])
    S.op("pool", lambda e: e.memset(ml[:], 1.0), writes=[b])
    S.op("pool", lambda e: e.affine_select(out=ml[:], in_=ml[:], pattern=[[-1, 64]], compare_op=ALU.is_gt, fill=0.0, base=0,
                                           channel_multiplier=1), reads=[b], writes=[b])
    mup = kb.sb(es, "mup", [64, 64])
    S.op("pool", lambda e: e.memset(mup[:], 1.0), writes=[b])
    S.op("pool", lambda e: e.affine_select(out=mup[:], in_=mup[:], pattern=[[1, 64]], compare_op=ALU.is_gt, fill=0.0, base=0,
                                           channel_multiplier=-1), reads=[b], writes=[b])
    ones = kb.sb(es, "ones", [128, 128])
    S.op("pool", lambda e: e.memset(ones[:], 1.0), writes=[b])
    mneg = kb.sb(es, "mneg", [128, 128])
    S.op("pool", lambda e: e.memset(mneg[:], 0.0), writes=[b])
    for (r0, c0, op) in ((0, 0, ALU.is_gt), (0, 64, ALU.is_ge), (64, 0, ALU.is_gt), (64, 64, ALU.is_ge)):
        blk = mneg[r0:r0 + 64, c0:c0 + 64]
        S.op("pool", lambda e: e.affine_select(out=blk, in_=blk, pattern=[[1, 64]], compare_op=op, fill=-30000.0, base=0,
                                               channel_multiplier=-1), reads=[b], writes=[b])
    sgn = kb.sb(es, "sgn", [128, 1])
    S.op("pool", lambda e: e.memset(sgn[0:64, :], -1.0), writes=[b])
    S.op("pool", lambda e: e.memset(sgn[64:128, :], 1.0), writes=[b])
    c.update(m4=m4, ml=ml, mup=mup, ones=ones, sgn=sgn, mneg=mneg, b_mask=b)


class T:
    def __init__(self, ap, excl=False, b=None):
        self.ap = ap
        self.b = b if b is not None else Buf(excl)

    def __getitem__(self, k):
        return self.ap[k]


def dplr_segment(kb, c, u, Kd, NCH, P):
    S = kb.S
    V = Kd
    QR, BK, BKe, VV, wC, H, OT = (u[k] for k in ("QR", "BK", "BKe", "VV", "wC", "H", "OT"))
    Hb = u["Hb"]
    G4, A, AT, Y, YT, TT, Xp, Up, WmT, KKt, KBe, UV = (P[k] for k in ("G4", "A", "AT", "Y", "YT", "TT", "Xp", "Up", "WmT", "KKt", "KBe", "UV"))
    pg, pinv, pm, pseq = P["pg"], P["pinv"], P["pm"], P["pseq"]
    m4, ml, ident, sgn, bm, bi = c["m4"], c["ml"], c["identb"], c["sgn"], c["b_mask"], c["b_ident"]
    pmb = T(pm.ap.bitcast(BF16), b=pm.b)
    pAb = T(pinv[0].ap.bitcast(BF16), b=pinv[0].b)
    safe = "QRg" in u
    QRg = u["QRg"] if safe else QR
    for c0 in range(0, NCH, 4):
        n = min(4, NCH - c0)
        for i in range(n):
            S.op("pe", lambda e: e.matmul(pg[:, i * 128:(i + 1) * 128], lhsT=BK[0:Kd, c0 + i, :], rhs=QRg[0:Kd, c0 + i, :],
                                          start=True, stop=True), reads=[BK.b, QRg.b], writes=[pg.b])
        S.op("dve", lambda e: e.tensor_tensor(out=G4[:, c0:c0 + n, :], in0=pg[:, 0:n * 128].rearrange("p (a b) -> p a b", a=n),
                                              in1=m4[:].unsqueeze(1).to_broadcast([128, n, 128]), op=ALU.mult),
             reads=[pg.b, bm], writes=[G4.b])
        S.op("dve", lambda e: e.tensor_tensor(out=AT[0:64, c0:c0 + n, :], in0=pg[0:64, 0:n * 128].rearrange("p (a b) -> p a b", a=n)[:, :, 0:64],
                                              in1=c["mup"][:].unsqueeze(1).to_broadcast([64, n, 64]), op=ALU.mult),
             reads=[pg.b, bm], writes=[AT.b])
        if safe:
            pe_ = pinv[1]
            NA, E, nones = u["NA"], u["E"], u["nones"]
            ones = c["ones"]
            for i in range(n):
                cc_ = c0 + i
                for hf in range(2):
                    o_ = i * 128 + hf * 64
                    S.op("pe", lambda e: e.matmul(pe_[:, o_:o_ + 64], lhsT=ones[0:1, 0:128], rhs=NA[0:1, cc_, hf * 64:(hf + 1) * 64],
                                                  start=True, stop=False), reads=[NA.b, bm], writes=[pe_.b])
                    S.op("pe", lambda e: e.matmul(pe_[:, o_:o_ + 64], lhsT=NA[0:1, cc_, :], rhs=nones[0:1, 0:64],
                                                  start=False, stop=True), reads=[NA.b, nones.b], writes=[pe_.b])
            S.op("dve", lambda e: e.tensor_tensor(out=E[:, c0:c0 + n, :], in0=pe_[:, 0:n * 128].rearrange("p (a b) -> p a b", a=n),
                                                  in1=c["mneg"][:].unsqueeze(1).to_broadcast([128, n, 128]), op=ALU.add),
                 reads=[pe_.b, bm], writes=[E.b])
            S.op("act", lambda e: e.activation(out=E[:, c0:c0 + n, :], in_=E[:, c0:c0 + n, :], func=AF.Exp), reads=[E.b], writes=[E.b])
            S.op("pool", lambda e: e.tensor_tensor(out=G4[:, c0:c0 + n, :], in0=G4[:, c0:c0 + n, :], in1=E[:, c0:c0 + n, :], op=ALU.mult),
                 reads=[G4.b, E.b], writes=[G4.b])
            S.op("pool", lambda e: e.tensor_tensor(out=AT[0:64, c0:c0 + n, :], in0=AT[0:64, c0:c0 + n, :], in1=E[0:64, c0:c0 + n, 0:64], op=ALU.mult),
                 reads=[AT.b, E.b], writes=[AT.b])
    pA = pinv[0]
    if safe:
        for i in range(NCH):
            S.op("pe", lambda e: e.transpose(out=pAb[0:64, i * 64:(i + 1) * 64], in_=AT[0:64, i, :], identity=ident[0:64, 0:64]),
                 reads=[AT.b, bi], writes=[pA.b])
        S.op("dve", lambda e: e.tensor_copy(out=A[0:64, :, :], in_=pAb[0:64, 0:NCH * 64].rearrange("p (a b) -> p a b", a=NCH)),
             reads=[pA.b], writes=[A.b])
    else:
        for i in range(NCH):
            S.op("pe", lambda e: e.matmul(pA[0:64, i * 64:(i + 1) * 64], lhsT=QR[0:Kd, i, 0:64], rhs=BK[0:Kd, i, 0:64],
                                          start=True, stop=True), reads=[QR.b, BK.b], writes=[pA.b])
        S.op("dve", lambda e: e.tensor_tensor(out=A[0:64, :, :], in0=pA[0:64, 0:NCH * 64].rearrange("p (a b) -> p a b", a=NCH),
                                              in1=ml[:].unsqueeze(1).to_broadcast([64, NCH, 64]), op=ALU.mult),
             reads=[pA.b, bm], writes=[A.b])
    S.op("dve", lambda e: e.scalar_tensor_tensor(out=TT[0:64, :, :], in0=AT[0:64, :, :], scalar=-1.0,
                                                 in1=c["ident"][0:64, 0:64].unsqueeze(1).to_broadcast([64, NCH, 64]),
                                                 op0=ALU.mult, op1=ALU.add), reads=[AT.b, bi], writes=[TT.b])
    nb = 512 // V
    fillers = []

    def f_p8(c0, n):
        for i in range(n):
            S.op("pe", lambda e: e.transpose(out=pmb[:, i * V:(i + 1) * V], in_=VV[0:Kd, c0 + i, :], identity=ident[0:Kd, 0:Kd]),
                 reads=[VV.b, bi], writes=[pm.b])
        S.op("act", lambda e: e.copy(out=UV[64:128, c0:c0 + n, 0:V], in_=pmb[64:128, 0:n * V].rearrange("p (a b) -> p a b", a=n)),
             reads=[pm.b], writes=[UV.b])

    def f_p6(c0, n):
        for i in range(n):
            S.op("pe", lambda e: e.transpose(out=pmb[:, i * V:(i + 1) * V], in_=BKe[0:Kd, c0 + i, :], identity=ident[0:Kd, 0:Kd]),
                 reads=[BKe.b, bi], writes=[pm.b])
        S.op("dve", lambda e: e.tensor_scalar(out=KBe[:, c0:c0 + n, 0:Kd], in0=pmb[:, 0:n * V].rearrange("p (a b) -> p a b", a=n),
                                              scalar1=sgn[:, 0:1], scalar2=None, op0=ALU.mult), reads=[pm.b, bm], writes=[KBe.b])

    def f_p7(c0, n):
        for i in range(n):
            S.op("pe", lambda e: e.transpose(out=pmb[0:64, i * V:(i + 1) * V], in_=QR[0:Kd, c0 + i, 0:64], identity=ident[0:Kd, 0:Kd]),
                 reads=[QR.b, bi], writes=[pm.b])
        S.op("act", lambda e: e.copy(out=KKt[0:64, c0:c0 + n, 0:Kd], in_=pmb[0:64, 0:n * V].rearrange("p (a b) -> p a b", a=n)),
             reads=[pm.b], writes=[KKt.b])

    def f_p4(c0, n):
        for i in range(n):
            S.op("pe", lambda e: e.matmul(pm[0:64, i * V:(i + 1) * V], lhsT=G4[:, c0 + i, 0:64], rhs=UV[:, c0 + i, 0:V],
                                          start=True, stop=True), reads=[G4.b, UV.b], writes=[pm.b])
        S.op("dve", lambda e: e.tensor_copy(out=Xp[0:64, c0:c0 + n, 0:V], in_=pm[0:64, 0:n * V].rearrange("p (a b) -> p a b", a=n)),
             reads=[pm.b], writes=[Xp.b])
    for c0 in range(0, NCH, nb):
        n = min(nb, NCH - c0)
        for fn in (f_p8, f_p6, f_p7, f_p4):
            fillers.append((fn, c0, n))
    fillers.reverse()
    Yc, YTc = A, AT
    for lvl in range(5):
        Yn, YTn = Y[lvl % 2], YT[lvl % 2]
        pY, pYT, pT_ = pinv[0], pinv[1], pinv[2]
        for i in range(NCH):
            S.op("pe", lambda e: e.matmul(pY[0:64, i * 64:(i + 1) * 64], lhsT=YTc[0:64, i, :], rhs=Yc[0:64, i, :],
                                          start=True, stop=True), reads=[YTc.b, Yc.b], writes=[pY.b])
        S.op("act", lambda e: e.copy(out=Yn[0:64, :, :], in_=pY[0:64, 0:NCH * 64].rearrange("p (a b) -> p a b", a=NCH)),
             reads=[pY.b], writes=[Yn.b])
        if lvl < 4:
            for i in range(NCH):
                S.op("pe", lambda e: e.matmul(pYT[0:64, i * 64:(i + 1) * 64], lhsT=Yc[0:64, i, :], rhs=YTc[0:64, i, :],
                                              start=True, stop=True), reads=[YTc.b, Yc.b], writes=[pYT.b])
            S.op("dve", lambda e: e.tensor_copy(out=YTn[0:64, :, :], in_=pYT[0:64, 0:NCH * 64].rearrange("p (a b) -> p a b", a=NCH)),
                 reads=[pYT.b], writes=[YTn.b])
        for k_ in range(FILL):
            if fillers:
                fn, c0, n = fillers.pop()
                fn(c0, n)
        for i in range(NCH):
            S.op("pe", lambda e: e.matmul(pT_[0:64, i * 64:(i + 1) * 64], lhsT=Yn[0:64, i, :], rhs=TT[0:64, i, :],
                                          start=True, stop=True), reads=[Yn.b, TT.b], writes=[pT_.b])
        S.op("dve", lambda e: e.tensor_tensor(out=TT[0:64, :, :], in0=TT[0:64, :, :],
                                              in1=pT_[0:64, 0:NCH * 64].rearrange("p (a b) -> p a b", a=NCH), op=ALU.add),
             reads=[pT_.b, TT.b], writes=[TT.b])
        Yc, YTc = Yn, YTn
    while fillers:
        fn, c0, n = fillers.pop()
        fn(c0, n)
    for c0 in range(0, NCH, nb):
        n = min(nb, NCH - c0)
        for i in range(n):
            S.op("pe", lambda e: e.matmul(pm[0:64, i * V:(i + 1) * V], lhsT=TT[0:64, c0 + i, :], rhs=Xp[0:64, c0 + i, 0:V],
                                          start=True, stop=True), reads=[TT.b, Xp.b], writes=[pm.b])
        S.op("act", lambda e: e.copy(out=Up[0:64, c0:c0 + n, 0:V], in_=pm[0:64, 0:n * V].rearrange("p (a b) -> p a b", a=n)),
             reads=[pm.b], writes=[Up.b])
    if KSTOP < 4.55:
        return
    for i in range(NCH):
        S.op("pe", lambda e: e.matmul(pm[0:Kd, i * 64:(i + 1) * 64], lhsT=KKt[0:64, i, 0:Kd], rhs=TT[0:64, i, :],
                                      start=True, stop=True), reads=[KKt.b, TT.b], writes=[pm.b])
    S.op("dve", lambda e: e.tensor_copy(out=WmT[0:Kd, :, :], in_=pm[0:Kd, 0:NCH * 64].rearrange("p (a b) -> p a b", a=NCH)),
         reads=[pm.b], writes=[WmT.b])
    if KSTOP < 5:
        return
    pU, pO, pH = pseq
    for i in range(NCH):
        S.op("pe", lambda e: e.matmul(pU[0:64, 0:V], lhsT=WmT[0:Kd, i, :], rhs=Hb[0:Kd, :], start=True, stop=True),
             reads=[WmT.b, Hb.b], writes=[pU.b])
        S.op("dve", lambda e: e.tensor_tensor(out=UV[0:64, i, 0:V], in0=pU[0:64, 0:V], in1=Up[0:64, i, 0:V], op=ALU.add),
             reads=[pU.b, Up.b], writes=[UV.b])
        S.op("pe", lambda e: e.matmul(pO[0:V, 0:64], lhsT=Hb[0:Kd, :], rhs=QR[0:Kd, i, 64:128], start=True, stop=False),
             reads=[Hb.b, QR.b], writes=[pO.b])
        S.op("pe", lambda e: e.matmul(pO[0:V, 0:64], lhsT=UV[:, i, 0:V], rhs=G4[:, i, 64:128], start=False, stop=True),
             reads=[UV.b, G4.b], writes=[pO.b])
        S.op("act", lambda e: e.copy(out=OT[0:V, i * 64:(i + 1) * 64], in_=pO[0:V, 0:64]), reads=[pO.b], writes=[OT.b])
        S.op("pe", lambda e: e.matmul(pH[0:Kd, 0:V], lhsT=KBe[:, i, 0:Kd], rhs=UV[:, i, 0:V], start=True, stop=True),
             reads=[KBe.b, UV.b], writes=[pH.b])
        S.op("dve", lambda e: e.scalar_tensor_tensor(out=Hb[0:Kd, :], in0=H[0:Kd, :], scalar=wC[0:Kd, i:i + 1], in1=pH[0:Kd, 0:V],
                                                     op0=ALU.mult, op1=ALU.add), reads=[H.b, wC.b, pH.b], writes=[Hb.b])
        S.op("dve", lambda e: e.scalar_tensor_tensor(out=H[0:Kd, :], in0=H[0:Kd, :], scalar=wC[0:Kd, i:i + 1], in1=pH[0:Kd, 0:V],
                                                     op0=ALU.mult, op1=ALU.add), reads=[H.b, wC.b, pH.b], writes=[H.b])


def phase_B(kb, c, PT, b_PT, prm, OMIX, b_OMIX):
    cfg, S, nc = kb.cfg, kb.S, kb.nc
    W = cfg.SEG
    NCH = W // 64
    RWH, DNH, RW, DN = cfg.RWH, cfg.DNH, cfg.RW, cfg.DN
    rows = cfg.rows
    c0 = RW_DECAY_SCALE
    with ExitStack() as es:
        build_masks(kb, es, c)
        ones, ident = c["ones"], c["ident"]
        bm, bi, bcol = c["b_mask"], c["b_ident"], c["b_col"]
        mk = lambda name, shape, dt=F32: T(kb.sb(es, "B_" + name, shape, dt))
        rwp = mk("rwp", [64, 13, RWH])
        lmu = mk("lmu", [128, 8])
        w2 = mk("w2", [96, RW]); a2 = mk("a2", [96, RW]); g2 = mk("g2", [128, 2, RW])
        dnp = mk("dnp", [128, 12, DNH]); dnw = mk("dnw", [128, 1]); dnh = mk("dnh", [16, 3])
        sel = mk("sel", [16, 16, 128])
        omka = mk("omka", [64, RWH])
        S.dma("sp", lambda e: e.dma_start(out=rwp[:, 0:10, :], in_=prm["rwp"]), writes=[rwp.b])
        S.dma("sp", lambda e: e.dma_start(out=lmu[:, 0:4], in_=prm["lmu"]), writes=[lmu.b])
        S.dma("sp", lambda e: e.dma_start(out=w2[:], in_=prm["rw_w2"]), writes=[w2.b])
        S.dma("sp", lambda e: e.dma_start(out=a2[:], in_=prm["rw_a2"]), writes=[a2.b])
        S.dma("sp", lambda e: e.dma_start(out=g2[:], in_=prm["rw_g2"].rearrange("(a p) n -> p a n", p=128)), writes=[g2.b])
        S.dma("sp", lambda e: e.dma_start(out=dnp[:], in_=prm["dnp"]), writes=[dnp.b])
        S.dma("sp", lambda e: e.dma_start(out=dnw[:], in_=prm["dnw"]), writes=[dnw.b])
        S.dma("sp", lambda e: e.dma_start(out=dnh[:, 0:2], in_=prm["dnh"]), writes=[dnh.b])
        S.op("dve", lambda e: e.tensor_scalar(out=rwp[:, 10:13, :], in0=rwp[:, 0:3, :], scalar1=-1.0, scalar2=1.0, op0=ALU.mult, op1=ALU.add),
             reads=[rwp.b], writes=[rwp.b])
        S.op("dve", lambda e: e.tensor_scalar(out=lmu[:, 4:8], in0=lmu[:, 0:4], scalar1=-1.0, scalar2=1.0, op0=ALU.mult, op1=ALU.add),
             reads=[lmu.b], writes=[lmu.b])
        S.op("dve", lambda e: e.tensor_scalar(out=omka[:], in0=rwp[:, 6, :], scalar1=-1.0, scalar2=1.0, op0=ALU.mult, op1=ALU.add),
             reads=[rwp.b], writes=[omka.b])
        S.op("act", lambda e: e.activation(out=dnh[:, 2:3], in_=dnh[:, 0:1], func=AF.Exp), reads=[dnh.b], writes=[dnh.b])
        S.op("dve", lambda e: e.tensor_scalar(out=dnh[:, 2:3], in0=dnh[:, 2:3], scalar1=-1.0, scalar2=None, op0=ALU.mult),
             reads=[dnh.b], writes=[dnh.b])
        S.op("pool", lambda e: e.memset(sel[:], 1.0), writes=[sel.b])
        S.op("pool", lambda e: e.affine_select(out=sel[:], in_=sel[:], pattern=[[1, 16], [0, 128]], compare_op=ALU.is_equal, fill=0.0,
                                               base=0, channel_multiplier=-1), reads=[sel.b], writes=[sel.b])
        rmask = mk("rmask", [128, W])
        S.op("pool", lambda e: e.memset(rmask[:], 1.0), writes=[rmask.b])
        S.op("pool", lambda e: e.memset(rmask[:].rearrange("p (a b) -> p a b", b=64)[:, :, 0:1], 0.0), writes=[rmask.b])
        Hrw = [mk("Hrw%d" % h, [64, 64]) for h in range(RWH)]
        Hdn = [mk("Hdn%d" % h, [128, 128]) for h in range(DNH)]
        Hrwb = [mk("Hrwb%d" % h, [64, 64], BF16) for h in range(RWH)]
        Hdnb = [mk("Hdnb%d" % h, [128, 128], BF16) for h in range(DNH)]
        for Ht in Hrw + Hdn + Hrwb + Hdnb:
            S.op("pool", lambda e: e.memset(Ht[:], 0.0), writes=[Ht.b])
        def mku(par):
            d = {k: mk("%s_%d" % (k, par), [128, NCH, 128], BF16) for k in ("QR", "BK", "BKe", "VV", "QRg")}
            d["wC"] = mk("wC_%d" % par, [128, NCH])
            d["NA"] = mk("NA_%d" % par, [1, NCH, 128])
            d["HO1"] = mk("HO1_%d" % par, [128, W]); d["HO2"] = mk("HO2_%d" % par, [128, W])
            S.op("pool", lambda e: e.memset(d["VV"][:], 0.0), writes=[d["VV"].b])
            return d
        usets = [mku(0), mku(1)]
        OT = mk("OT", [128, W])
        nones = mk("nones", [1, 64])
        S.op("pool", lambda e: e.memset(nones[:], -1.0), writes=[nones.b])
        P = {k: mk(k, [128, NCH, 128], BF16) for k in ("G4", "Xp", "KKt", "KBe", "UV")}
        P["Up"] = mk("Up", [128, NCH, 128])
        P["E"] = mk("E", [128, NCH, 128])
        S.op("pool", lambda e: e.memset(P["UV"][:], 0.0), writes=[P["UV"].b])
        P["WmT"] = mk("WmT", [128, NCH, 64], BF16)
        for k in ("A", "AT", "TT"):
            P[k] = mk(k, [64, NCH, 64], BF16)
        P["Y"] = [mk("Y%d" % i, [64, NCH, 64], BF16) for i in range(2)]
        P["YT"] = [mk("YT%d" % i, [64, NCH, 64], BF16) for i in range(2)]
        pst = lambda name: T(kb.ps(es, "Bp_" + name, [128, 512]), excl=True)
        P["pg"] = pst("g"); P["pinv"] = [pst("i0"), pst("i1"), pst("i2")]; P["pm"] = pst("m")
        pseq = kb.ps(es, "Bp_seq", [128, 512])
        bseq = Buf(True)
        P["pseq"] = [T(pseq[:, 0:128], b=bseq), T(pseq[:, 128:256], b=bseq), T(pseq[:, 256:384], b=bseq)]
        pp = [pst("p0"), pst("p1")]
        ppc = [0]

        def nextp():
            ppc[0] += 1
            return pp[ppc[0] % 2]
        ft = {}

        def F(name, dt=F32):
            if name not in ft:
                ft[name] = mk("f_" + name, [128, W], dt)
            return ft[name]
        SC = min(512, RW, DN)
        stage = mk("stage", [128, W // 128, SC], BF16)
        LW = mk("LW", [128, 1, W + 1]); LWs = mk("LWs", [128, 4, W])
        BETA = T(LWs[0:16, 0, :], b=LWs.b); GG = T(LWs[0:16, 1, :], b=LWs.b)
        PIN = [mk("PIN%d" % i, [128, W + 4]) for i in range(3)]

        def v3(t, Kd):
            return t[0:Kd, :].rearrange("p (a b) -> p a b", b=64)

        def dve_tt(out, a, b_, op, reads, writes, eng="dve"):
            S.op(eng, lambda e: e.tensor_tensor(out=out, in0=a, in1=b_, op=op), reads=reads, writes=writes)

        def load_shift(dst, row0, nrows, t0, lead):
            bsrc = [b_PT[j] for j in range(row0 // 128, (row0 + nrows - 1) // 128 + 1)]
            if t0 == 0:
                S.op("pool", lambda e: e.memset(dst[0:nrows, 0:lead], 0.0), writes=[dst.b])
                S.dma("sp", lambda e: e.dma_start(out=dst[0:nrows, lead:lead + W], in_=PT[row0:row0 + nrows, 0:W]), reads=bsrc, writes=[dst.b])
            else:
                S.dma("sp", lambda e: e.dma_start(out=dst[0:nrows, 0:lead + W], in_=PT[row0:row0 + nrows, t0 - lead:t0 + W]), reads=bsrc, writes=[dst.b])

        def chunk_common(u, Kd, Gsrc, Gsrc_reads, scale, r_, kk_, b_, kh_, v_, b0_=None):
            G, GP, EG, ENG, EGP, EGC = F("G"), F("GP"), F("A"), F("SQ"), F("RS"), F("SIG")
            S.op("dve", lambda e: e.tensor_tensor_scan(out=G[0:Kd, :], data0=rmask[0:Kd, :], data1=Gsrc, initial=0.0, op0=ALU.mult, op1=ALU.add),
                 reads=[rmask.b] + Gsrc_reads, writes=[G.b])
            dve_tt(GP[0:Kd, :], G[0:Kd, :], Gsrc, ALU.subtract, [G.b] + Gsrc_reads, [GP.b])
            S.op("act", lambda e: e.activation(out=EG[0:Kd, :], in_=G[0:Kd, :], func=AF.Exp, scale=scale), reads=[G.b], writes=[EG.b])
            if b0_ is None:
                S.op("act", lambda e: e.activation(out=ENG[0:Kd, :], in_=G[0:Kd, :], func=AF.Exp, scale=-scale), reads=[G.b], writes=[ENG.b])
            S.op("act", lambda e: e.activation(out=EGP[0:Kd, :], in_=GP[0:Kd, :], func=AF.Exp, scale=scale), reads=[GP.b], writes=[EGP.b])
            gend = v3(G, Kd)[:, :, 63:64]
            dve_tt(v3(EGC, Kd), gend.to_broadcast([Kd, NCH, 64]), v3(G, Kd), ALU.subtract, [G.b], [EGC.b])
            S.op("act", lambda e: e.activation(out=EGC[0:Kd, :], in_=EGC[0:Kd, :], func=AF.Exp, scale=scale), reads=[EGC.b], writes=[EGC.b])
            S.op("act", lambda e: e.activation(out=u["wC"][0:Kd, :], in_=gend.rearrange("p a b -> p (a b)"), func=AF.Exp, scale=scale),
                 reads=[G.b], writes=[u["wC"].b])
            QR, BK, BKe, VV = u["QR"], u["BK"], u["BKe"], u["VV"]
            dve_tt(QR[0:Kd, :, 0:64], v3(kk_, Kd), v3(EGP, Kd), ALU.mult, [kk_.b, EGP.b], [QR.b])
            dve_tt(QR[0:Kd, :, 64:128], v3(r_, Kd), v3(EG, Kd), ALU.mult, [r_.b, EG.b], [QR.b], eng="pool")
            if b0_ is None:
                dve_tt(BK[0:Kd, :, 0:64], v3(b_, Kd), v3(ENG, Kd), ALU.mult, [b_.b, ENG.b], [BK.b])
                dve_tt(BK[0:Kd, :, 64:128], v3(kh_, Kd), v3(ENG, Kd), ALU.mult, [kh_.b, ENG.b], [BK.b], eng="pool")
            else:
                QRg, NA = u["QRg"], u["NA"]
                S.op("pool", lambda e: e.tensor_copy(out=BK[0:Kd, :, 0:64], in_=v3(b0_, Kd)), reads=[b0_.b], writes=[BK.b])
                S.op("pool", lambda e: e.tensor_copy(out=BK[0:Kd, :, 64:128], in_=v3(kh_, Kd)), reads=[kh_.b], writes=[BK.b])
                S.op("pool", lambda e: e.tensor_copy(out=QRg[0:Kd, :, 0:64], in_=v3(kk_, Kd)), reads=[kk_.b], writes=[QRg.b])
                S.op("pool", lambda e: e.tensor_copy(out=QRg[0:Kd, :, 64:128], in_=v3(r_, Kd)), reads=[r_.b], writes=[QRg.b])
                S.op("dve", lambda e: e.tensor_copy(out=NA[0:1, :, 0:64], in_=v3(GP, 1)), reads=[GP.b], writes=[NA.b])
                S.op("dve", lambda e: e.tensor_copy(out=NA[0:1, :, 64:128], in_=v3(G, 1)), reads=[G.b], writes=[NA.b])
            dve_tt(BKe[0:Kd, :, 0:64], v3(b_, Kd), v3(EGC, Kd), ALU.mult, [b_.b, EGC.b], [BKe.b])
            dve_tt(BKe[0:Kd, :, 64:128], v3(kh_, Kd), v3(EGC, Kd), ALU.mult, [kh_.b, EGC.b], [BKe.b], eng="pool")
            S.op("pool", lambda e: e.tensor_copy(out=VV[0:Kd, :, 64:128], in_=v3(v_, Kd)), reads=[v_.b], writes=[VV.b])

        def l2_rstd(Kd, src, dst):
            SQ = F("SQ")
            S.op("act", lambda e: e.activation(out=SQ[0:Kd, :], in_=src[0:Kd, :], func=AF.Square), reads=[src.b], writes=[SQ.b])
            p = nextp()
            S.op("pe", lambda e: e.matmul(p[0:Kd, 0:W], lhsT=ones[0:Kd, 0:Kd], rhs=SQ[0:Kd, :], start=True, stop=True), reads=[SQ.b, bm], writes=[p.b])
            S.op("act", lambda e: e.activation(out=dst[0:Kd, :], in_=p[0:Kd, 0:W], func=AF.Ln, bias=c["EPS6"][0:Kd, :]), reads=[p.b, bcol], writes=[dst.b])
            S.op("act", lambda e: e.activation(out=dst[0:Kd, :], in_=dst[0:Kd, :], func=AF.Exp, scale=-0.5), reads=[dst.b], writes=[dst.b])

        def to_stage(Kd, res, col0):
            p = P["pg"]
            nb_ = W // 128
            for i in range(nb_):
                S.op("pe", lambda e: e.transpose(out=p[:, i * Kd:(i + 1) * Kd], in_=res[0:Kd, i * 128:(i + 1) * 128], identity=ident[0:Kd, 0:Kd]),
                     reads=[res.b, bi], writes=[p.b])
            S.op("act", lambda e: e.copy(out=stage[:, :, col0:col0 + Kd], in_=p[:, 0:nb_ * Kd].rearrange("p (a b) -> p a b", a=nb_)),
                 reads=[p.b], writes=[stage.b])

        def prep_rw_shared(t0):
            for i, nm in enumerate(("wd", "ad", "gd", "gd")):
                r0 = rows[nm] + (128 if i == 3 else 0)
                bsrc = [b_PT[r0 // 128]]
                if t0 == 0:
                    S.op("pool", lambda e: e.memset(LW[:, 0, 0:1], 0.0), writes=[LW.b])
                    S.dma("sp", lambda e: e.dma_start(out=LW[:, 0, 1:1 + W], in_=PT[r0:r0 + 128, 0:W]), reads=bsrc, writes=[LW.b])
                else:
                    S.dma("sp", lambda e: e.dma_start(out=LW[:, 0, :], in_=PT[r0:r0 + 128, t0 - 1:t0 + W]), reads=bsrc, writes=[LW.b])
                S.op("dve", lambda e: e.tensor_scalar(out=LWs[:, i, :], in0=LW[:, 0, 0:W], scalar1=lmu[:, i:i + 1], scalar2=None, op0=ALU.mult),
                     reads=[LW.b, lmu.b], writes=[LWs.b])
                S.op("dve", lambda e: e.scalar_tensor_tensor(out=LWs[:, i, :], in0=LW[:, 0, 1:1 + W], scalar=lmu[:, 4 + i:5 + i], in1=LWs[:, i, :],
                                                             op0=ALU.mult, op1=ALU.add), reads=[LW.b, lmu.b, LWs.b], writes=[LWs.b])
                if i != 1:
                    fn = AF.Tanh if i == 0 else AF.Sigmoid
                    S.op("act", lambda e: e.activation(out=LWs[:, i, :], in_=LWs[:, i, :], func=fn), reads=[LWs.b], writes=[LWs.b])

        def prep_dn_shared(t0):
            rb = rows["ba"]
            S.dma("sp", lambda e: e.dma_start(out=BETA[:], in_=PT[rb:rb + 16, t0:t0 + W]), reads=[b_PT[rb // 128]], writes=[BETA.b])
            S.dma("sp", lambda e: e.dma_start(out=GG[:], in_=PT[rb + 32:rb + 48, t0:t0 + W]), reads=[b_PT[rb // 128]], writes=[GG.b])
            S.op("act", lambda e: e.activation(out=BETA[:], in_=BETA[:], func=AF.Sigmoid), reads=[BETA.b], writes=[BETA.b])
            S.op("act", lambda e: e.activation(out=GG[:], in_=GG[:], func=AF.Exp, bias=dnh[:, 1:2]), reads=[GG.b, dnh.b], writes=[GG.b])
            S.op("act", lambda e: e.activation(out=GG[:], in_=GG[:], func=AF.Ln, bias=c["ONE"][0:16, :]), reads=[GG.b, bcol], writes=[GG.b])
            S.op("dve", lambda e: e.tensor_scalar(out=GG[:], in0=GG[:], scalar1=dnh[:, 2:3], scalar2=None, op0=ALU.mult), reads=[GG.b, dnh.b], writes=[GG.b])

        def prep_rw(u, h, t0):
            if h == 0:
                prep_rw_shared(t0)
            src = {}
            for i, nm in enumerate(("r", "k", "v")):
                load_shift(PIN[i], rows[nm] + h * 64, 64, t0, 1)
                d = F("x_" + nm)
                S.op("dve", lambda e: e.tensor_scalar(out=d[0:64, :], in0=PIN[i][0:64, 0:W], scalar1=rwp[:, i, h:h + 1], scalar2=None, op0=ALU.mult),
                     reads=[PIN[i].b, rwp.b], writes=[d.b])
                S.op("dve", lambda e: e.scalar_tensor_tensor(out=d[0:64, :], in0=PIN[i][0:64, 1:1 + W], scalar=rwp[:, 10 + i, h:h + 1], in1=d[0:64, :],
                                                             op0=ALU.mult, op1=ALU.add), reads=[PIN[i].b, rwp.b, d.b], writes=[d.b])
                src[nm] = d
            r_, k_, v_ = src["r"], src["k"], src["v"]
            hs = slice(h * 64, (h + 1) * 64)
            SIG, A_, GT = F("SIG"), F("A"), u["HO2"]
            p = nextp()
            S.op("pe", lambda e: e.matmul(p[0:64, 0:W], lhsT=w2[0:96, hs], rhs=LWs[0:96, 0, :], start=True, stop=True), reads=[w2.b, LWs.b], writes=[p.b])
            S.op("act", lambda e: e.activation(out=SIG[0:64, :], in_=p[0:64, 0:W], func=AF.Sigmoid, bias=rwp[:, 3, h:h + 1]), reads=[p.b, rwp.b], writes=[SIG.b])
            p = nextp()
            S.op("pe", lambda e: e.matmul(p[0:64, 0:W], lhsT=a2[0:96, hs], rhs=LWs[0:96, 1, :], start=True, stop=True), reads=[a2.b, LWs.b], writes=[p.b])
            S.op("act", lambda e: e.activation(out=A_[0:64, :], in_=p[0:64, 0:W], func=AF.Sigmoid, bias=rwp[:, 4, h:h + 1]), reads=[p.b, rwp.b], writes=[A_.b])
            p = nextp()
            for a in range(2):
                S.op("pe", lambda e: e.matmul(p[0:64, 0:W], lhsT=g2[:, a, hs], rhs=LWs[:, 2 + a, :], start=(a == 0), stop=(a == 1)), reads=[g2.b, LWs.b], writes=[p.b])
            S.op("act", lambda e: e.copy(out=GT[0:64, :], in_=p[0:64, 0:W]), reads=[p.b], writes=[GT.b])
            KX, RS, KK, T1, KH, B_ = F("KX"), F("RS"), F("KK"), F("T1"), F("KH"), F("B")
            S.op("dve", lambda e: e.tensor_scalar(out=KX[0:64, :], in0=k_[0:64, :], scalar1=rwp[:, 5, h:h + 1], scalar2=None, op0=ALU.mult),
                 reads=[k_.b, rwp.b], writes=[KX.b])
            l2_rstd(64, KX, RS)
            dve_tt(KK[0:64, :], KX[0:64, :], RS[0:64, :], ALU.mult, [KX.b, RS.b], [KK.b])
            S.op("dve", lambda e: e.tensor_scalar(out=T1[0:64, :], in0=A_[0:64, :], scalar1=rwp[:, 6, h:h + 1], scalar2=omka[:, h:h + 1], op0=ALU.mult, op1=ALU.add),
                 reads=[A_.b, rwp.b, omka.b], writes=[T1.b])
            dve_tt(KH[0:64, :], k_[0:64, :], T1[0:64, :], ALU.mult, [k_.b, T1.b], [KH.b], eng="pool")
            dve_tt(B_[0:64, :], KK[0:64, :], A_[0:64, :], ALU.mult, [KK.b, A_.b], [B_.b], eng="pool")
            RK, BON = F("KX"), u["HO1"]
            S.op("dve", lambda e: e.scalar_tensor_tensor(out=RK[0:64, :], in0=r_[0:64, :], scalar=rwp[:, 7, h:h + 1], in1=KH[0:64, :], op0=ALU.mult, op1=ALU.mult),
                 reads=[r_.b, rwp.b, KH.b], writes=[RK.b])
            p = nextp()
            S.op("pe", lambda e: e.matmul(p[0:64, 0:W], lhsT=ones[0:64, 0:64], rhs=RK[0:64, :], start=True, stop=True), reads=[RK.b, bm], writes=[p.b])
            dve_tt(BON[0:64, :], p[0:64, 0:W], v_[0:64, :], ALU.mult, [p.b, v_.b], [BON.b])
            chunk_common(u, 64, SIG[0:64, :], [SIG.b], -c0, r_, KK, B_, KH, v_)

        def prep_dn(u, h, t0):
            if h == 0:
                prep_dn_shared(t0)
            src = {}
            for i, nm in enumerate(("dq", "dk", "dv")):
                load_shift(PIN[i], rows[nm] + h * 128, 128, t0, 3)
                d = F("x_" + ("r", "k", "v")[i])
                S.op("dve", lambda e: e.tensor_scalar(out=d[:, :], in0=PIN[i][:, 0:W], scalar1=dnp[:, i, h:h + 1], scalar2=None, op0=ALU.mult),
                     reads=[PIN[i].b, dnp.b], writes=[d.b])
                for j in range(1, 4):
                    S.op("dve", lambda e: e.scalar_tensor_tensor(out=d[:, :], in0=PIN[i][:, j:j + W], scalar=dnp[:, 3 * j + i, h:h + 1], in1=d[:, :],
                                                                 op0=ALU.mult, op1=ALU.add), reads=[PIN[i].b, dnp.b, d.b], writes=[d.b])
                S.op("act", lambda e: e.activation(out=d[:, :], in_=d[:, :], func=AF.Silu), reads=[d.b], writes=[d.b])
                src[nm] = d
            q_, k_, v_ = src["dq"], src["dk"], src["dv"]
            RS, QN, KN, B_, EGB, VB = F("RS"), F("KX"), F("KK"), F("B"), F("T1"), F("KH")
            l2_rstd(128, q_, RS)
            S.op("dve", lambda e: e.scalar_tensor_tensor(out=QN[:, :], in0=q_[:, :], scalar=128.0 ** -0.5, in1=RS[:, :], op0=ALU.mult, op1=ALU.mult),
                 reads=[q_.b, RS.b], writes=[QN.b])
            l2_rstd(128, k_, RS)
            dve_tt(KN[:, :], k_[:, :], RS[:, :], ALU.mult, [k_.b, RS.b], [KN.b])
            pbeta = nextp()
            S.op("pe", lambda e: e.matmul(pbeta[:, 0:W], lhsT=sel[:, h, :], rhs=BETA[:], start=True, stop=True), reads=[sel.b, BETA.b], writes=[pbeta.b])
            B0 = F("x_k")
            dve_tt(B0[:, :], KN[:, :], pbeta[:, 0:W], ALU.mult, [KN.b, pbeta.b], [B0.b])
            dve_tt(VB[:, :], v_[:, :], pbeta[:, 0:W], ALU.mult, [v_.b, pbeta.b], [VB.b])
            pg_ = nextp()
            S.op("pe", lambda e: e.matmul(pg_[:, 0:W], lhsT=sel[:, h, :], rhs=GG[:], start=True, stop=True), reads=[sel.b, GG.b], writes=[pg_.b])
            GB = F("SIG")
            S.op("act", lambda e: e.copy(out=GB[:, :], in_=pg_[:, 0:W]), reads=[pg_.b], writes=[GB.b])
            S.op("act", lambda e: e.activation(out=EGB[:, :], in_=pg_[:, 0:W], func=AF.Exp), reads=[pg_.b], writes=[EGB.b])
            dve_tt(B_[:, :], B0[:, :], EGB[:, :], ALU.mult, [B0.b, EGB.b], [B_.b])
            ZT = u["HO1"]
            rz = rows["z"] + h * 128
            S.dma("sp", lambda e: e.dma_start(out=ZT[:, :], in_=PT[rz:rz + 128, t0:t0 + W]), reads=[b_PT[rz // 128]], writes=[ZT.b])
            S.op("act", lambda e: e.activation(out=ZT[:, :], in_=ZT[:, :], func=AF.Silu), reads=[ZT.b], writes=[ZT.b])
            chunk_common(u, 128, GB[:, :], [GB.b], 1.0, QN, KN, B_, KN, VB, b0_=B0)

        e_X, e_DD = mk("e_X", [128, W]), mk("e_DD", [128, W])

        def flush_stage(h, Kd, base, t0):
            if (h * Kd + Kd) % SC == 0:
                cb = base + (h * Kd + Kd) - SC
                for i in range(W // 128):
                    S.dma("pool", lambda e: e.dma_start(out=OMIX[t0 + i * 128:t0 + (i + 1) * 128, cb:cb + SC], in_=stage[:, i, :]), reads=[stage.b], writes=[b_OMIX])

        def run_rw(u, h, t0):
            uu = dict(u); uu["H"] = Hrw[h]; uu["Hb"] = Hrwb[h]; uu["OT"] = OT
            uu.pop("QRg", None)
            dplr_segment(kb, c, uu, 64, NCH, P)
            BON, GT = u["HO1"], u["HO2"]
            SQ, MEAN, DD, RES = e_X, e_X, e_DD, e_X
            p = P["pm"]
            S.op("pe", lambda e: e.matmul(p[0:64, 0:W], lhsT=ones[0:64, 0:64], rhs=OT[0:64, :], start=True, stop=True), reads=[OT.b, bm], writes=[p.b])
            S.op("act", lambda e: e.mul(out=MEAN[0:64, :], in_=p[0:64, 0:W], mul=1.0 / 64), reads=[p.b], writes=[MEAN.b])
            dve_tt(DD[0:64, :], OT[0:64, :], MEAN[0:64, :], ALU.subtract, [OT.b, MEAN.b], [DD.b])
            S.op("act", lambda e: e.activation(out=SQ[0:64, :], in_=DD[0:64, :], func=AF.Square), reads=[DD.b], writes=[SQ.b])
            S.op("pe", lambda e: e.matmul(p[0:64, 0:W], lhsT=ones[0:64, 0:64], rhs=SQ[0:64, :], start=True, stop=True), reads=[SQ.b, bm], writes=[p.b])
            S.op("act", lambda e: e.activation(out=RES[0:64, :], in_=p[0:64, 0:W], func=AF.Ln, scale=1.0 / 64, bias=c["GNEPS"][0:64, :]), reads=[p.b, bcol], writes=[RES.b])
            S.op("act", lambda e: e.activation(out=RES[0:64, :], in_=RES[0:64, :], func=AF.Exp, scale=-0.5), reads=[RES.b], writes=[RES.b])
            dve_tt(DD[0:64, :], DD[0:64, :], RES[0:64, :], ALU.mult, [DD.b, RES.b], [DD.b])
            S.op("dve", lambda e: e.tensor_scalar(out=DD[0:64, :], in0=DD[0:64, :], scalar1=rwp[:, 8, h:h + 1], scalar2=rwp[:, 9, h:h + 1], op0=ALU.mult, op1=ALU.add),
                 reads=[DD.b, rwp.b], writes=[DD.b])
            dve_tt(DD[0:64, :], DD[0:64, :], BON[0:64, :], ALU.add, [DD.b, BON.b], [DD.b], eng="pool")
            dve_tt(RES[0:64, :], DD[0:64, :], GT[0:64, :], ALU.mult, [DD.b, GT.b], [RES.b])
            to_stage(64, RES, (h * 64) % SC)
            flush_stage(h, 64, 0, t0)

        def run_dn(u, h, t0):
            uu = dict(u); uu["H"] = Hdn[h]; uu["Hb"] = Hdnb[h]; uu["OT"] = OT; uu["E"] = P["E"]; uu["nones"] = nones
            dplr_segment(kb, c, uu, 128, NCH, P)
            ZT = u["HO1"]
            SQ, l2n, RES = e_X, e_X, e_DD
            p = P["pm"]
            S.op("act", lambda e: e.activation(out=SQ[:, :], in_=OT[:, :], func=AF.Square), reads=[OT.b], writes=[SQ.b])
            S.op("pe", lambda e: e.matmul(p[:, 0:W], lhsT=ones[:, :], rhs=SQ[:, :], start=True, stop=True), reads=[SQ.b, bm], writes=[p.b])
            S.op("act", lambda e: e.activation(out=l2n[:, :], in_=p[:, 0:W], func=AF.Ln, scale=1.0 / 128, bias=c["EPS6"]), reads=[p.b, bcol], writes=[l2n.b])
            S.op("act", lambda e: e.activation(out=l2n[:, :], in_=l2n[:, :], func=AF.Exp, scale=-0.5), reads=[l2n.b], writes=[l2n.b])
            S.op("dve", lambda e: e.scalar_tensor_tensor(out=RES[:, :], in0=OT[:, :], scalar=dnw[:, 0:1], in1=l2n[:, :], op0=ALU.mult, op1=ALU.mult),
                 reads=[OT.b, dnw.b, l2n.b], writes=[RES.b])
            dve_tt(RES[:, :], RES[:, :], ZT[:, :], ALU.mult, [RES.b, ZT.b], [RES.b])
            to_stage(128, RES, (h * 128) % SC)
            flush_stage(h, 128, RW, t0)

        units = []
        for seg in range(cfg.S // W):
            units += [("rw", h, seg * W) for h in range(RWH)] + [("dn", h, seg * W) for h in range(DNH)]

        def mkprep(i):
            kind, h, t0 = units[i]
            return (lambda: prep_rw(usets[i % 2], h, t0)) if kind == "rw" else (lambda: prep_dn(usets[i % 2], h, t0))

        def mkrun(i):
            kind, h, t0 = units[i]
            return (lambda: run_rw(usets[i % 2], h, t0)) if kind == "rw" else (lambda: run_dn(usets[i % 2], h, t0))
        mkprep(0)()
        for i in range(len(units)):
            nxt = mkprep(i + 1) if i + 1 < len(units) else None
            run_pair(S, nxt, mkrun(i), quota=BQ)
    S.barrier()


def build_hT(kb, c, S, src, row0, ntiles, D, KD, xt, hb, ss, gt, hT, psT, pcnt, nrm=True):
    for ti in range(ntiles):
        t0 = row0 + ti * 128
        S.dma("sp", lambda e: e.dma_start(out=xt[:], in_=src[t0:t0 + 128, :]), writes=[xt.b])
        S.op("act", lambda e: e.activation(out=hb[:], in_=xt[:], func=AF.Square, accum_out=ss[:]), reads=[xt.b], writes=[hb.b, ss.b])
        rmsnorm_rstd(kb, c, ss[:], ss.b, D)
        S.op("dve", lambda e: e.scalar_tensor_tensor(out=hb[:], in0=xt[:], scalar=ss[:, 0:1], in1=gt[:], op0=ALU.mult, op1=ALU.mult),
             reads=[xt.b, ss.b, gt.b], writes=[hb.b])
        transpose_bf(kb, c, S, hb, KD, hT, ti, psT, pcnt)


def transpose_bf(kb, c, S, src, KD, dstT, ti, psT, pcnt):
    for k0 in range(0, KD, 8):
        kn = min(8, KD - k0)
        p = psT[pcnt[0] % len(psT)]; pcnt[0] += 1
        for k in range(kn):
            S.op("pe", lambda e: e.transpose(out=p[:, k * 128:(k + 1) * 128], in_=src[:, (k0 + k) * 128:(k0 + k + 1) * 128], identity=c["identb"][:]),
                 reads=[src.b, c["b_ident"]], writes=[p.b])
        sv = p[:, 0:kn * 128].rearrange("p (a b) -> p a b", a=kn)
        dv = dstT[:, k0:k0 + kn, ti * 128:(ti + 1) * 128]
        if pcnt[0] % 2:
            S.op("act", lambda e: e.copy(out=dv, in_=sv), reads=[p.b], writes=[dstT.b])
        else:
            S.op("dve", lambda e: e.tensor_copy(out=dv, in_=sv), reads=[p.b], writes=[dstT.b])


def phase_A1(kb, c, xo, g1d, wG, gbd, GATES, b_G):
    cfg, S = kb.cfg, kb.S
    D, KD, TGO, KP = cfg.D, cfg.KD, cfg.TGO, cfg.KP
    NT = TGO // 128
    NPG = KD // KP
    with ExitStack() as es:
        mk = lambda name, shape, dt=F32: T(kb.sb(es, "G_" + name, shape, dt))
        hT = mk("hT", [128, KD, TGO], BF16); xt = mk("xt", [128, D]); hb = mk("hb", [128, D], BF16); ss = mk("ss", [128, 1]); g1 = mk("g1", [128, D])
        slf = [mk("slf%d" % i, [128, KP, 512]) for i in range(2)]; slb = [mk("slb%d" % i, [128, KP, 512], BF16) for i in range(2)]
        gb = [mk("gb%d" % i, [128, 512]) for i in range(2)]
        stg = [mk("stg%d" % i, [128, 512]) for i in range(2)]
        psT = [T(kb.ps(es, "G_psT%d" % i, [128, 1024], BF16), excl=True) for i in range(2)]
        psG = [T(kb.ps(es, "G_ps%d" % i, [128, 512]), excl=True) for i in range(NT)]
        S.dma("sp", lambda e: e.dma_start(out=g1[:], in_=g1d.partition_broadcast(128)), writes=[g1.b])
        pcnt = [0]
        it = 0
        for og in range(cfg.OWN // TGO):
            build_hT(kb, c, S, xo, og * TGO, NT, D, KD, xt, hb, ss, g1, hT, psT, pcnt)
            for gc in range(2 * D // 512):
                gbt = gb[gc % 2]
                S.dma("sp", lambda e: e.dma_start(out=gbt[:], in_=gbd[0:1, gc * 512:(gc + 1) * 512].partition_broadcast(128)), writes=[gbt.b])
                for q in range(NPG):
                    sl = it % 2; it += 1
                    S.dma("sp", lambda e: e.dma_start(out=slf[sl][:], in_=wG[gc, q]), writes=[slf[sl].b])
                    if it % 2:
                        S.op("act", lambda e: e.copy(out=slb[sl][:], in_=slf[sl][:]), reads=[slf[sl].b], writes=[slb[sl].b])
                    else:
                        S.op("dve", lambda e: e.tensor_copy(out=slb[sl][:], in_=slf[sl][:]), reads=[slf[sl].b], writes=[slb[sl].b])
                    for t in range(NT):
                        for k in range(KP):
                            kk = q * KP + k
                            S.op("pe", lambda e: e.matmul(psG[t][:, :], lhsT=hT[:, kk, t * 128:(t + 1) * 128], rhs=slb[sl][:, k, :],
                                                          start=(kk == 0), stop=(kk == KD - 1)), reads=[hT.b, slb[sl].b], writes=[psG[t].b])
                for t in range(NT):
                    st = stg[t % 2]
                    S.op("dve", lambda e: e.tensor_tensor(out=st[:], in0=psG[t][:, :], in1=gbt[:], op=ALU.add), reads=[psG[t].b, gbt.b], writes=[st.b])
                    S.op("act", lambda e: e.activation(out=st[:], in_=st[:], func=AF.Sigmoid), reads=[st.b], writes=[st.b])
                    r0 = og * TGO + t * 128
                    S.dma("pool", lambda e: e.dma_start(out=GATES[r0:r0 + 128, gc * 512:(gc + 1) * 512], in_=st[:]), reads=[st.b], writes=[b_G])
    S.barrier()


def phase_C1(kb, c, xo, own_idx, OMIX, b_OMIX, GATES, b_G, wBR, wO, XMID, b_XMID):
    cfg, S = kb.cfg, kb.S
    D, KD, KB, TGO, BW = cfg.D, cfg.KD, cfg.KB, cfg.TGO, cfg.BW
    NT = TGO // 128
    KBr = cfg.RW // 128
    KH = max(KD // 2, 1)
    with ExitStack() as es:
        mk = lambda name, shape, dt=F32: T(kb.sb(es, "C_" + name, shape, dt))
        KM = max(KD, KB)
        oT = mk("oT", [128, KM, TGO], BF16)
        mg = mk("mg", [128, NT, D], BF16)
        og_ = [mk("og%d" % i, [128, BW], BF16) for i in range(2)]
        idx = mk("idx", [128, cfg.OWN // 128], I32)
        SLW = min(8, KD, KBr)
        slf = [mk("slf%d" % i, [128, SLW, 512]) for i in range(2)]

        def pieces(k0, k1):
            return [(a, min(a + SLW, k1)) for a in range(k0, k1, SLW)]
        slb = [mk("slb%d" % i, [128, KM, 512], BF16) for i in range(2)]
        gt = [mk("gt%d" % i, [128, 2, 512]) for i in range(2)]
        tm = [mk("tm%d" % i, [128, 512]) for i in range(2)]
        xs = [mk("xs%d" % i, [128, 512]) for i in range(2)]
        psT = [T(kb.ps(es, "C_psT%d" % i, [128, 1024], BF16), excl=True) for i in range(2)]
        psY = [T(kb.ps(es, "C_psY%d" % i, [128, 512]), excl=True) for i in range(4)]
        S.dma("sp", lambda e: e.dma_start(out=idx[:], in_=own_idx), writes=[idx.b])
        pcnt = [0]
        it = 0
        yc = 0
        for g in range(cfg.OWN // TGO):
            for t in range(NT):
                o = og_[t % 2]
                col = g * NT + t
                S.dma("pool", lambda e: e.indirect_dma_start(out=o[:], out_offset=None, in_=OMIX,
                                                             in_offset=bass.IndirectOffsetOnAxis(ap=idx[:, col:col + 1], axis=0)),
                      reads=[idx.b, b_OMIX], writes=[o.b])
                transpose_bf(kb, c, S, o, KB, oT, t, psT, pcnt)
            for cc in range(D // 512):
                sb_ = slb[cc % 2]
                for br, (k0, k1) in enumerate(pieces(0, KBr) + pieces(KBr, KB)):
                    sl = it % 2; it += 1
                    S.dma("sp", lambda e: e.dma_start(out=slf[sl][:, 0:k1 - k0, :], in_=wBR[cc, :, k0:k1, :]), writes=[slf[sl].b])
                    if br % 2 == 0:
                        S.op("act", lambda e: e.copy(out=sb_[:, k0:k1, :], in_=slf[sl][:, 0:k1 - k0, :]), reads=[slf[sl].b], writes=[sb_.b])
                    else:
                        S.op("dve", lambda e: e.tensor_copy(out=sb_[:, k0:k1, :], in_=slf[sl][:, 0:k1 - k0, :]), reads=[slf[sl].b], writes=[sb_.b])
                for t in range(NT):
                    r0 = g * TGO + t * 128
                    gtt = gt[t % 2]
                    S.dma("sp", lambda e: e.dma_start(out=gtt[:, 0, :], in_=GATES[r0:r0 + 128, cc * 512:(cc + 1) * 512]), reads=[b_G], writes=[gtt.b])
                    S.dma("sp", lambda e: e.dma_start(out=gtt[:, 1, :], in_=GATES[r0:r0 + 128, D + cc * 512:D + (cc + 1) * 512]), reads=[b_G], writes=[gtt.b])
                    pr = psY[yc % 4]; pd = psY[(yc + 1) % 4]; yc += 2
                    for k in range(0, KBr):
                        S.op("pe", lambda e: e.matmul(pr[:, :], lhsT=oT[:, k, t * 128:(t + 1) * 128], rhs=sb_[:, k, :], start=(k == 0), stop=(k == KBr - 1)),
                             reads=[oT.b, sb_.b], writes=[pr.b])
                    for k in range(KBr, KB):
                        S.op("pe", lambda e: e.matmul(pd[:, :], lhsT=oT[:, k, t * 128:(t + 1) * 128], rhs=sb_[:, k, :], start=(k == KBr), stop=(k == KB - 1)),
                             reads=[oT.b, sb_.b], writes=[pd.b])
                    t1 = tm[t % 2]
                    S.op("dve", lambda e: e.tensor_tensor(out=t1[:], in0=pr[:, :], in1=gtt[:, 0, :], op=ALU.mult), reads=[pr.b, gtt.b], writes=[t1.b])
                    S.op("dve", lambda e: e.tensor_tensor(out=gtt[:, 1, :], in0=pd[:, :], in1=gtt[:, 1, :], op=ALU.mult), reads=[pd.b, gtt.b], writes=[gtt.b])
                    S.op("dve", lambda e: e.tensor_tensor(out=mg[:, t, cc * 512:(cc + 1) * 512], in0=t1[:], in1=gtt[:, 1, :], op=ALU.add),
                         reads=[t1.b, gtt.b], writes=[mg.b])
            for t in range(NT):
                mt = T(mg[:, t, :], b=mg.b)
                transpose_bf(kb, c, S, mt, KD, oT, t, psT, pcnt)
            for cc in range(D // 512):
                sb_ = slb[cc % 2]
                for hf, (k0, k1) in enumerate(pieces(0, KD)):
                    sl = it % 2; it += 1
                    S.dma("sp", lambda e: e.dma_start(out=slf[sl][:, 0:k1 - k0, :], in_=wO[cc, :, k0:k1, :]), writes=[slf[sl].b])
                    if hf % 2 == 0:
                        S.op("act", lambda e: e.copy(out=sb_[:, k0:k1, :], in_=slf[sl][:, 0:k1 - k0, :]), reads=[slf[sl].b], writes=[sb_.b])
                    else:
                        S.op("dve", lambda e: e.tensor_copy(out=sb_[:, k0:k1, :], in_=slf[sl][:, 0:k1 - k0, :]), reads=[slf[sl].b], writes=[sb_.b])
                for t in range(NT):
                    r0 = g * TGO + t * 128
                    x_ = xs[t % 2]
                    S.dma("sp", lambda e: e.dma_start(out=x_[:], in_=xo[r0:r0 + 128, cc * 512:(cc + 1) * 512]), writes=[x_.b])
                    pz = psY[yc % 4]; yc += 1
                    for k in range(KD):
                        S.op("pe", lambda e: e.matmul(pz[:, :], lhsT=oT[:, k, t * 128:(t + 1) * 128], rhs=sb_[:, k, :], start=(k == 0), stop=(k == KD - 1)),
                             reads=[oT.b, sb_.b], writes=[pz.b])
                    S.op("dve", lambda e: e.tensor_tensor(out=x_[:], in0=pz[:, :], in1=x_[:], op=ALU.add), reads=[pz.b, x_.b], writes=[x_.b])
                    S.dma("pool", lambda e: e.dma_start(out=XMID[r0:r0 + 128, cc * 512:(cc + 1) * 512], in_=x_[:]), reads=[x_.b], writes=[b_XMID])
    S.barrier()


def phase_C2(kb, c, XMID, b_XMID, g2d, wRd, bRd, X2, b_X2, R):
    cfg, S = kb.cfg, kb.S
    D, KD, NG, NE = cfg.D, cfg.KD, cfg.NG, cfg.NE
    NR = NG + NE
    NTO = cfg.OWN // 128
    with ExitStack() as es:
        mk = lambda name, shape, dt=F32: T(kb.sb(es, "R_" + name, shape, dt))
        xm = mk("xm", [128, D]); x2 = mk("x2", [128, D]); x2b = mk("x2b", [128, D], BF16); ss = mk("ss", [128, 1]); g2 = mk("g2", [128, D])
        x2T = mk("x2T", [128, KD, 128]); wR = mk("wR", [128, KD, NR]); bR = mk("bR", [128, NR])
        zt = mk("zt", [128, D], BF16)
        LG = mk("LG", [128, NR]); sm = mk("sm", [128, 16]); t3 = mk("t3", [128, NG, 8]); og = mk("og", [128, NG]); se = mk("se", [128, 8]); ee = mk("ee", [128, 8])
        m8 = mk("m8", [128, 8]); i8 = mk("i8", [128, 8], U32); i8f = mk("i8f", [128, 2]); tg = mk("tg", [128, NG]); oh = mk("oh", [128, NE])
        iog = mk("iog", [128, NG]); ioe = R["ioe"]
        psT = [T(kb.ps(es, "R_psT%d" % i, [128, 512]), excl=True) for i in range(2)]
        psL = T(kb.ps(es, "R_psL", [128, 512]), excl=True)
        S.dma("sp", lambda e: e.dma_start(out=g2[:], in_=g2d.partition_broadcast(128)), writes=[g2.b])
        S.dma("sp", lambda e: e.dma_start(out=wR[:], in_=wRd), writes=[wR.b])
        S.dma("sp", lambda e: e.dma_start(out=bR[:], in_=bRd.partition_broadcast(128)), writes=[bR.b])
        S.op("pool", lambda e: e.iota(iog[:], pattern=[[1, NG]], base=0, channel_multiplier=0, allow_small_or_imprecise_dtypes=True), writes=[iog.b])
        S.op("pool", lambda e: e.memset(zt[:], 0.0), writes=[zt.b])
        S.dma("pool", lambda e: e.dma_start(out=X2[cfg.OWN:cfg.OWN + 128, :], in_=zt[:]), reads=[zt.b], writes=[b_X2])
        M, RT = R["M"], R["RT"]
        pc = 0
        for i in range(NTO):
            r0 = i * 128
            S.dma("sp", lambda e: e.dma_start(out=xm[:], in_=XMID[r0:r0 + 128, :]), reads=[b_XMID], writes=[xm.b])
            S.op("act", lambda e: e.activation(out=x2[:], in_=xm[:], func=AF.Square, accum_out=ss[:]), reads=[xm.b], writes=[x2.b, ss.b])
            rmsnorm_rstd(kb, c, ss[:], ss.b, D)
            S.op("dve", lambda e: e.scalar_tensor_tensor(out=x2[:], in0=xm[:], scalar=ss[:, 0:1], in1=g2[:], op0=ALU.mult, op1=ALU.mult),
                 reads=[xm.b, ss.b, g2.b], writes=[x2.b])
            S.op("pool", lambda e: e.tensor_copy(out=x2b[:], in_=x2[:]), reads=[x2.b], writes=[x2b.b])
            S.dma("pool", lambda e: e.dma_start(out=X2[r0:r0 + 128, :], in_=x2b[:]), reads=[x2b.b], writes=[b_X2])
            for k0 in range(0, KD, 4):
                kn = min(4, KD - k0)
                p = psT[pc % 2]; pc += 1
                for k in range(kn):
                    S.op("pe", lambda e: e.transpose(out=p[:, k * 128:(k + 1) * 128], in_=x2[:, (k0 + k) * 128:(k0 + k + 1) * 128], identity=c["ident"][:]),
                         reads=[x2.b, c["b_ident"]], writes=[p.b])
                S.op("act", lambda e: e.copy(out=x2T[:, k0:k0 + kn, :], in_=p[:, 0:kn * 128].rearrange("p (a b) -> p a b", a=kn)), reads=[p.b], writes=[x2T.b])
            for k in range(KD):
                S.op("pe", lambda e: e.matmul(psL[:, 0:NR], lhsT=x2T[:, k, :], rhs=wR[:, k, :], start=(k == 0), stop=(k == KD - 1)),
                     reads=[x2T.b, wR.b], writes=[psL.b])
            D_ = lambda fn, reads, writes: S.op("dve", fn, reads=reads, writes=writes)
            D_(lambda e: e.tensor_tensor(out=LG[:], in0=psL[:, 0:NR], in1=bR[:], op=ALU.add), [psL.b, bR.b], [LG.b])
            D_(lambda e: e.tensor_reduce(out=sm[:, 0:1], in_=LG[:, 0:NG], axis=AX.X, op=ALU.max), [LG.b], [sm.b])
            D_(lambda e: e.tensor_scalar(out=og[:], in0=LG[:, 0:NG], scalar1=sm[:, 0:1], scalar2=None, op0=ALU.is_equal), [LG.b, sm.b], [og.b])
            D_(lambda e: e.tensor_scalar(out=sm[:, 1:2], in0=sm[:, 0:1], scalar1=-1.0, scalar2=None, op0=ALU.mult), [sm.b], [sm.b])
            S.op("act", lambda e: e.activation(out=tg[:], in_=LG[:, 0:NG], func=AF.Exp, bias=sm[:, 1:2], accum_out=sm[:, 2:3]), reads=[LG.b, sm.b], writes=[tg.b, sm.b])
            D_(lambda e: e.reciprocal(out=sm[:, 3:4], in_=sm[:, 2:3]), [sm.b], [sm.b])
            D_(lambda e: e.tensor_tensor(out=tg[:], in0=og[:], in1=iog[:], op=ALU.mult), [og.b, iog.b, tg.b], [tg.b])
            D_(lambda e: e.tensor_reduce(out=sm[:, 4:5], in_=tg[:], axis=AX.X, op=ALU.add), [tg.b], [sm.b])
            D_(lambda e: e.tensor_tensor(out=t3[:], in0=LG[:, NG:NR].rearrange("p (g e) -> p g e", e=8), in1=og[:].unsqueeze(2).to_broadcast([128, NG, 8]), op=ALU.mult),
               [LG.b, og.b], [t3.b])
            D_(lambda e: e.tensor_reduce(out=se[:], in_=t3[:].rearrange("p g e -> p e g"), axis=AX.X, op=ALU.add), [t3.b], [se.b])
            D_(lambda e: e.tensor_reduce(out=sm[:, 5:6], in_=se[:], axis=AX.X, op=ALU.max), [se.b], [sm.b])
            D_(lambda e: e.tensor_scalar(out=sm[:, 5:6], in0=sm[:, 5:6], scalar1=-1.0, scalar2=None, op0=ALU.mult), [sm.b], [sm.b])
            S.op("act", lambda e: e.activation(out=ee[:], in_=se[:], func=AF.Exp, bias=sm[:, 5:6]), reads=[se.b, sm.b], writes=[ee.b])
            D_(lambda e: e.max(out=m8[:], in_=ee[:]), [ee.b], [m8.b])
            D_(lambda e: e.max_index(out=i8[:], in_max=m8[:], in_values=ee[:]), [m8.b, ee.b], [i8.b])
            D_(lambda e: e.tensor_copy(out=i8f[:], in_=i8[:, 0:2]), [i8.b], [i8f.b])
            D_(lambda e: e.tensor_tensor(out=sm[:, 6:7], in0=m8[:, 0:1], in1=m8[:, 1:2], op=ALU.add), [m8.b], [sm.b])
            D_(lambda e: e.reciprocal(out=sm[:, 6:7], in_=sm[:, 6:7]), [sm.b], [sm.b])
            D_(lambda e: e.tensor_tensor(out=sm[:, 6:7], in0=sm[:, 6:7], in1=sm[:, 3:4], op=ALU.mult), [sm.b], [sm.b])
            D_(lambda e: e.tensor_scalar(out=RT[:, i, 2:4], in0=m8[:, 0:2], scalar1=sm[:, 6:7], scalar2=None, op0=ALU.mult), [m8.b, sm.b], [RT.b])
            D_(lambda e: e.scalar_tensor_tensor(out=RT[:, i, 0:2], in0=sm[:, 4:5].to_broadcast([128, 2]), scalar=8.0, in1=i8f[:], op0=ALU.mult, op1=ALU.add),
               [sm.b, i8f.b], [RT.b])
            D_(lambda e: e.tensor_scalar(out=M[:, i, :], in0=ioe[:], scalar1=RT[:, i, 0:1], scalar2=None, op0=ALU.is_equal), [ioe.b, RT.b], [M.b])
            D_(lambda e: e.tensor_scalar(out=oh[:], in0=ioe[:], scalar1=RT[:, i, 1:2], scalar2=None, op0=ALU.is_equal), [ioe.b, RT.b], [oh.b])
            D_(lambda e: e.tensor_tensor(out=M[:, i, :], in0=M[:, i, :], in1=oh[:], op=ALU.add), [M.b, oh.b], [M.b])
    S.barrier()


def phase_D0(kb, c, R, SLOT, b_SLOT):
    cfg, S = kb.cfg, kb.S
    NE, NBLK = cfg.NE, cfg.NBLK
    NTO = cfg.OWN // 128
    NQ1 = cfg.KD // min(4, cfg.KD)
    FE = cfg.FE
    with ExitStack() as es:
        mk = lambda name, shape, dt=F32: T(kb.sb(es, "Z_" + name, shape, dt))
        M, RT, ioe = R["M"], R["RT"], R["ioe"]
        triu = mk("triu", [128, 128]); ones = mk("ones", [128, 128])
        msum = mk("msum", [128, NE]); mcum = mk("mcum", [128, NE]); cnt = mk("cnt", [128, NE]); pcn = mk("pcn", [128, NE]); pend = mk("pend", [128, NE])
        poff = mk("poff", [128, NE]); ci = mk("ci", [128, NE], I32)
        j128 = mk("j128", [128, NBLK]); cmp = mk("cmp", [128, NBLK, NE]); be = mk("be", [128, NBLK])
        qp1 = mk("qp1", [128, NQ1]); qp2 = mk("qp2", [128, FE * 2]); f1 = mk("f1", [128, NBLK, NQ1]); f2 = mk("f2", [128, NBLK, FE * 2])
        dest = mk("dest", [128, NE]); oh = mk("oh", [128, NE]); dd = mk("dd", [128, NTO, 2])
        toki = mk("toki", [128, NTO, 16], I32); sinit = mk("sinit", [128, NBLK, 16], I32)
        ps = [T(kb.ps(es, "Z_ps%d" % i, [128, 512]), excl=True) for i in range(2)]
        P_ = lambda fn, reads, writes: S.op("pool", fn, reads=reads, writes=writes)
        D_ = lambda fn, reads, writes: S.op("dve", fn, reads=reads, writes=writes)
        P_(lambda e: e.memset(triu[:], 1.0), [], [triu.b])
        P_(lambda e: e.affine_select(out=triu[:], in_=triu[:], pattern=[[1, 128]], compare_op=ALU.is_gt, fill=0.0, base=0, channel_multiplier=-1), [triu.b], [triu.b])
        P_(lambda e: e.memset(ones[:], 1.0), [], [ones.b])
        P_(lambda e: e.iota(j128[:], pattern=[[128, NBLK]], base=0, channel_multiplier=0, allow_small_or_imprecise_dtypes=True), [], [j128.b])
        P_(lambda e: e.iota(qp1[:], pattern=[[128, NQ1]], base=0, channel_multiplier=1, allow_small_or_imprecise_dtypes=True), [], [qp1.b])
        P_(lambda e: e.iota(qp2[:], pattern=[[128, FE * 2]], base=0, channel_multiplier=1, allow_small_or_imprecise_dtypes=True), [], [qp2.b])
        P_(lambda e: e.iota(toki[:], pattern=[[128, NTO], [0, 16]], base=0, channel_multiplier=1), [], [toki.b])
        P_(lambda e: e.iota(sinit[:], pattern=[[0, NBLK], [0, 16]], base=cfg.OWN, channel_multiplier=0), [], [sinit.b])
        S.dma("sp", lambda e: e.dma_start(out=SLOT.rearrange("(j p) c -> p j c", p=128), in_=sinit[:]), reads=[sinit.b], writes=[b_SLOT])
        D_(lambda e: e.tensor_reduce(out=msum[:], in_=M[:].rearrange("p i e -> p e i"), axis=AX.X, op=ALU.add), [M.b], [msum.b])
        S.op("pe", lambda e: e.matmul(ps[0][:, 0:NE], lhsT=ones[:], rhs=msum[:], start=True, stop=True), reads=[ones.b, msum.b], writes=[ps[0].b])
        D_(lambda e: e.tensor_scalar(out=ci[:], in0=ps[0][:, 0:NE], scalar1=127.0, scalar2=None, op0=ALU.add), [ps[0].b], [ci.b])
        D_(lambda e: e.tensor_single_scalar(out=ci[:], in_=ci[:], scalar=7, op=ALU.arith_shift_right), [ci.b], [ci.b])
        D_(lambda e: e.tensor_single_scalar(out=ci[:], in_=ci[:], scalar=7, op=ALU.logical_shift_left), [ci.b], [ci.b])
        D_(lambda e: e.tensor_copy(out=pcn[:], in_=ci[:]), [ci.b], [pcn.b])
        D_(lambda e: e.tensor_tensor_scan(out=pend[:], data0=ones[:, 0:NE], data1=pcn[:], initial=0.0, op0=ALU.mult, op1=ALU.add), [ones.b, pcn.b], [pend.b])
        D_(lambda e: e.tensor_tensor(out=poff[:], in0=pend[:], in1=pcn[:], op=ALU.subtract), [pend.b, pcn.b], [poff.b])
        D_(lambda e: e.tensor_tensor(out=cmp[:], in0=pend[:].unsqueeze(1).to_broadcast([128, NBLK, NE]), in1=j128[:].unsqueeze(2).to_broadcast([128, NBLK, NE]), op=ALU.is_le),
           [pend.b, j128.b], [cmp.b])
        D_(lambda e: e.tensor_reduce(out=be[:], in_=cmp[:], axis=AX.X, op=ALU.add), [cmp.b], [be.b])
        big = mk("big", [128, NBLK])
        D_(lambda e: e.tensor_scalar(out=big[:], in0=be[:], scalar1=float(NE) - 0.5, scalar2=1.0e7, op0=ALU.is_ge, op1=ALU.mult), [be.b], [big.b])
        D_(lambda e: e.tensor_scalar(out=be[:], in0=be[:], scalar1=float(NE - 1), scalar2=None, op0=ALU.min), [be.b], [be.b])
        D_(lambda e: e.scalar_tensor_tensor(out=f1[:], in0=be[:].unsqueeze(2).to_broadcast([128, NBLK, NQ1]), scalar=float(NQ1 * 128),
                                            in1=qp1[:].unsqueeze(1).to_broadcast([128, NBLK, NQ1]), op0=ALU.mult, op1=ALU.add), [be.b, qp1.b], [f1.b])
        D_(lambda e: e.tensor_tensor(out=f1[:], in0=f1[:], in1=big[:].unsqueeze(2).to_broadcast([128, NBLK, NQ1]), op=ALU.add), [f1.b, big.b], [f1.b])
        D_(lambda e: e.tensor_copy(out=R["IDX1"][:], in_=f1[:]), [f1.b], [R["IDX1"].b])
        D_(lambda e: e.scalar_tensor_tensor(out=f2[:], in0=be[:].unsqueeze(2).to_broadcast([128, NBLK, FE * 2]), scalar=float(FE * 2 * 128),
                                            in1=qp2[:].unsqueeze(1).to_broadcast([128, NBLK, FE * 2]), op0=ALU.mult, op1=ALU.add), [be.b, qp2.b], [f2.b])
        D_(lambda e: e.tensor_tensor(out=f2[:], in0=f2[:], in1=big[:].unsqueeze(2).to_broadcast([128, NBLK, FE * 2]), op=ALU.add), [f2.b, big.b], [f2.b])
        D_(lambda e: e.tensor_copy(out=R["IDX2"][:], in_=f2[:]), [f2.b], [R["IDX2"].b])
        P_(lambda e: e.memset(mcum[:], 0.0), [], [mcum.b])
        for i in range(NTO):
            p = ps[i % 2]
            S.op("pe", lambda e: e.matmul(p[:, 0:NE], lhsT=triu[:], rhs=M[:, i, :], start=True, stop=False), reads=[triu.b, M.b], writes=[p.b])
            S.op("pe", lambda e: e.matmul(p[:, 0:NE], lhsT=ones[:], rhs=mcum[:], start=False, stop=True), reads=[ones.b, mcum.b], writes=[p.b])
            D_(lambda e: e.tensor_tensor(out=dest[:], in0=p[:, 0:NE], in1=poff[:], op=ALU.add), [p.b, poff.b], [dest.b])
            D_(lambda e: e.tensor_tensor(out=mcum[:], in0=mcum[:], in1=M[:, i, :], op=ALU.add), [mcum.b, M.b], [mcum.b])
            for a in range(2):
                D_(lambda e: e.tensor_scalar(out=oh[:], in0=ioe[:], scalar1=RT[:, i, a:a + 1], scalar2=None, op0=ALU.is_equal), [ioe.b, RT.b], [oh.b])
                D_(lambda e: e.tensor_tensor(out=oh[:], in0=oh[:], in1=dest[:], op=ALU.mult), [oh.b, dest.b], [oh.b])
                D_(lambda e: e.tensor_reduce(out=dd[:, i, a:a + 1], in_=oh[:], axis=AX.X, op=ALU.add), [oh.b], [dd.b])
        D_(lambda e: e.tensor_copy(out=R["DEST"][:], in_=dd[:]), [dd.b], [R["DEST"].b])
        for i in range(NTO):
            for a in range(2):
                S.dma("pool", lambda e: e.indirect_dma_start(out=SLOT, out_offset=bass.IndirectOffsetOnAxis(ap=R["DEST"][:, i, a:a + 1], axis=0),
                                                             in_=toki[:, i, :], in_offset=None), reads=[R["DEST"].b, toki.b], writes=[b_SLOT])
    S.barrier()


def phase_D(kb, c, R, SLOT, b_SLOT, X2, b_X2, w1L, w3L, w2L, Y, b_Y):
    cfg, S = kb.cfg, kb.S
    D, KD, DE, FE, NBLK = cfg.D, cfg.KD, cfg.DE, cfg.FE, cfg.NBLK
    KQ = min(4, KD)
    NQ1 = KD // KQ
    DH = D // 2
    CW = min(512, DH)
    ech = [(o, min(512, DE - o)) for o in range(0, DE, 512)]
    with ExitStack() as es:
        mk = lambda name, shape, dt=F32: T(kb.sb(es, "M_" + name, shape, dt))
        tok = [mk("tok%d" % i, [128, 16], I32) for i in range(2)]
        xg = [mk("xg%d" % i, [128, D], BF16) for i in range(2)]
        xbT = mk("xbT", [128, KD, 128], BF16)
        NWB = 4
        wb = [[mk("wb%d_%d" % (a, i), [128, KQ, DE], BF16) for i in range(NWB)] for a in range(2)]
        w2b = [mk("w2b%d" % i, [128, FE, DH], BF16) for i in range(3)]
        actf = mk("actf", [128, DE]); actb = mk("actb", [128, DE], BF16); actT = mk("actT", [128, FE, 128], BF16)
        yst = mk("yst", [128, D])
        psT = [T(kb.ps(es, "M_psT", [128, 1024], BF16), excl=True)]
        ph = [[T(kb.ps(es, "M_ph%d_%d" % (a, i), [128, 512]), excl=True) for i in range(len(ech))] for a in range(2)]
        py = [T(kb.ps(es, "M_py%d" % i, [128, 512]), excl=True) for i in range(2)]
        IDX1, IDX2 = R["IDX1"], R["IDX2"]
        bc1 = kb.nc.gpsimd.to_reg(cfg.NE * NQ1 * 128 - 1)
        bc2 = kb.nc.gpsimd.to_reg(cfg.NE * FE * 2 * 128 - 1)
        for t_ in wb[0] + wb[1] + w2b:
            S.op("pool", lambda e: e.memset(t_[:], 0.0), writes=[t_.b])
        w2c = 0
        pcnt = [0]
        cst = 0
        yc = 0
        for j in range(NBLK):
            tk = tok[j % 2]; x_ = xg[j % 2]
            S.dma("sp", lambda e: e.dma_start(out=tk[:], in_=SLOT[j * 128:(j + 1) * 128, :]), reads=[b_SLOT], writes=[tk.b])
            S.dma("pool", lambda e: e.indirect_dma_start(out=x_[:], out_offset=None, in_=X2, in_offset=bass.IndirectOffsetOnAxis(ap=tk[:, 0:1], axis=0)),
                  reads=[tk.b, b_X2], writes=[x_.b])
            for ti in range(1):
                transpose_bf(kb, c, S, x_, KD, xbT, 0, psT, pcnt)
            for q in range(NQ1):
                wq = (j * NQ1 + q) % NWB
                for a, wl in enumerate((w1L, w3L)):
                    b_ = wb[a][wq]
                    S.dma("pool", lambda e: e.indirect_dma_start(out=b_[:].rearrange("p a b -> p (a b)"), out_offset=None, in_=wl,
                                                                 in_offset=bass.IndirectOffsetOnAxis(ap=IDX1[:, j, q:q + 1], axis=0),
                                                                 bounds_check=bc1, oob_is_err=False),
                          reads=[IDX1.b], writes=[b_.b])
                for kk in range(KQ):
                    k = q * KQ + kk
                    for a in range(2):
                        for ci_, (o, wdt) in enumerate(ech):
                            S.op("pe", lambda e: e.matmul(ph[a][ci_][:, 0:wdt], lhsT=xbT[:, k, :], rhs=wb[a][wq][:, kk, o:o + wdt], start=(k == 0), stop=(k == KD - 1)),
                                 reads=[xbT.b, wb[a][wq].b], writes=[ph[a][ci_].b])
            for ci_, (o, wdt) in enumerate(ech):
                S.op("act", lambda e: e.activation(out=actf[:, o:o + wdt], in_=ph[0][ci_][:, 0:wdt], func=AF.Silu), reads=[ph[0][ci_].b], writes=[actf.b])
                S.op("dve", lambda e: e.tensor_tensor(out=actb[:, o:o + wdt], in0=actf[:, o:o + wdt], in1=ph[1][ci_][:, 0:wdt], op=ALU.mult),
                     reads=[actf.b, ph[1][ci_].b], writes=[actb.b])
            transpose_bf(kb, c, S, actb, FE, actT, 0, psT, pcnt)
            for hf in range(2):
                w2b_ = w2b[w2c % 3]; w2c += 1
                for f in range(FE):
                    col = f * 2 + hf
                    S.dma("pool", lambda e: e.indirect_dma_start(out=w2b_[:, f, :], out_offset=None, in_=w2L, in_offset=bass.IndirectOffsetOnAxis(ap=IDX2[:, j, col:col + 1], axis=0),
                                                                 bounds_check=bc2, oob_is_err=False),
                          reads=[IDX2.b], writes=[w2b_.b])
                for cc in range(DH // CW):
                    p = py[yc % 2]; yc += 1
                    for f in range(FE):
                        S.op("pe", lambda e: e.matmul(p[:, 0:CW], lhsT=actT[:, f, :], rhs=w2b_[:, f, cc * CW:(cc + 1) * CW], start=(f == 0), stop=(f == FE - 1)),
                             reads=[actT.b, w2b_.b], writes=[p.b])
                    o0 = hf * DH + cc * CW
                    if yc % 2:
                        S.op("act", lambda e: e.copy(out=yst[:, o0:o0 + CW], in_=p[:, 0:CW]), reads=[p.b], writes=[yst.b])
                    else:
                        S.op("dve", lambda e: e.tensor_copy(out=yst[:, o0:o0 + CW], in_=p[:, 0:CW]), reads=[p.b], writes=[yst.b])
            S.dma("sp", lambda e: e.dma_start(out=Y[j * 128:(j + 1) * 128, :], in_=yst[:]), reads=[yst.b], writes=[b_Y])
    S.barrier()


def phase_E(kb, c, R, XMID, b_XMID, Y, b_Y, fgd, out, b_out):
    cfg, S = kb.cfg, kb.S
    D = cfg.D
    NTO = cfg.OWN // 128
    with ExitStack() as es:
        mk = lambda name, shape, dt=F32: T(kb.sb(es, "E_" + name, shape, dt))
        xm = [mk("xm%d" % i, [128, D]) for i in range(2)]
        ya = [mk("ya%d" % i, [128, D]) for i in range(2)]
        yb = [mk("yb%d" % i, [128, D]) for i in range(2)]
        sq = mk("sq", [128, D]); ss = mk("ss", [128, 1]); fg = mk("fg", [128, D])
        S.dma("sp", lambda e: e.dma_start(out=fg[:], in_=fgd.partition_broadcast(128)), writes=[fg.b])
        RT, DEST = R["RT"], R["DEST"]
        for i in range(NTO):
            r0 = i * 128
            x_, a_, b_ = xm[i % 2], ya[i % 2], yb[i % 2]
            S.dma("sp", lambda e: e.dma_start(out=x_[:], in_=XMID[r0:r0 + 128, :]), reads=[b_XMID], writes=[x_.b])
            S.dma("pool", lambda e: e.indirect_dma_start(out=a_[:], out_offset=None, in_=Y, in_offset=bass.IndirectOffsetOnAxis(ap=DEST[:, i, 0:1], axis=0)),
                  reads=[DEST.b, b_Y], writes=[a_.b])
            S.dma("pool", lambda e: e.indirect_dma_start(out=b_[:], out_offset=None, in_=Y, in_offset=bass.IndirectOffsetOnAxis(ap=DEST[:, i, 1:2], axis=0)),
                  reads=[DEST.b, b_Y], writes=[b_.b])
            S.op("dve", lambda e: e.scalar_tensor_tensor(out=x_[:], in0=a_[:], scalar=RT[:, i, 2:3], in1=x_[:], op0=ALU.mult, op1=ALU.add),
                 reads=[a_.b, RT.b, x_.b], writes=[x_.b])
            S.op("dve", lambda e: e.scalar_tensor_tensor(out=x_[:], in0=b_[:], scalar=RT[:, i, 3:4], in1=x_[:], op0=ALU.mult, op1=ALU.add),
                 reads=[b_.b, RT.b, x_.b], writes=[x_.b])
            S.op("act", lambda e: e.activation(out=sq[:], in_=x_[:], func=AF.Square, accum_out=ss[:]), reads=[x_.b], writes=[sq.b, ss.b])
            rmsnorm_rstd(kb, c, ss[:], ss.b, D)
            S.op("dve", lambda e: e.scalar_tensor_tensor(out=a_[:], in0=x_[:], scalar=ss[:, 0:1], in1=fg[:], op0=ALU.mult, op1=ALU.mult),
                 reads=[x_.b, ss.b, fg.b, a_.b], writes=[a_.b])
            S.dma("sp", lambda e: e.dma_start(out=out[r0:r0 + 128, :], in_=a_[:]), reads=[a_.b], writes=[b_out])
    S.barrier()


def build_program(cfg, debug=(), phases="ABCDE"):
    kb = K(cfg, debug)
    nc = kb.nc
    D, S_, KD, OWN, NE, NG = cfg.D, cfg.S, cfg.KD, cfg.OWN, cfg.NE, cfg.NG
    NTO = OWN // 128
    KQ = min(4, KD); NQ1 = KD // KQ; FE = cfg.FE
    xb = kb.inp("xb", [S_, D])
    xo = kb.inp("xo", [OWN, D])
    own_idx = kb.inp("own_idx", [128, NTO], I32)
    g1d = kb.inp("norm1_g", [1, D])
    wA = kb.inp("wA", [cfg.NA, 128, KD, 128])
    wG = kb.inp("wG", [2 * D // 512, KD // cfg.KP, 128, cfg.KP, 512])
    gbd = kb.inp("gate_b", [1, 2 * D])
    prm = {}
    for name, shape in (("rwp", [64, 10, cfg.RWH]), ("lmu", [128, 4]), ("rw_w2", [96, cfg.RW]), ("rw_a2", [96, cfg.RW]),
                        ("rw_g2", [256, cfg.RW]), ("dnp", [128, 12, cfg.DNH]), ("dnw", [128, 1]), ("dnh", [16, 2])):
        prm[name] = kb.inp(name, shape)
    wBR = kb.inp("wBR", [D // 512, 128, cfg.KB, 512])
    wO = kb.inp("wO", [D // 512, 128, KD, 512])
    g2d = kb.inp("norm2_g", [1, D])
    fgd = kb.inp("final_g", [1, D])
    wRd = kb.inp("wR", [128, KD, NG + NE])
    bRd = kb.inp("bR", [1, NG + NE])
    w1L = kb.inp("w1L", [NE * NQ1 * 128, KQ * cfg.DE])
    w3L = kb.inp("w3L", [NE * NQ1 * 128, KQ * cfg.DE])
    w2L = kb.inp("w2L", [NE * FE * 2 * 128, D // 2])
    PT = kb.scratch("PT", [cfg.NA * 128, S_]); b_PT = [Buf() for _ in range(cfg.NA)]
    OMIX = kb.scratch("OMIX", [S_, cfg.BW], BF16); b_OMIX = Buf()
    GATES = kb.scratch("GATES", [OWN, 2 * D]); b_G = Buf()
    XMID = kb.scratch("XMID", [OWN, D]); b_XMID = Buf()
    X2 = kb.scratch("X2", [OWN + 128, D], BF16); b_X2 = Buf()
    SLOT = kb.scratch("SLOT", [cfg.NBLK * 128, 16], I32); b_SLOT = Buf()
    Y = kb.scratch("Y", [cfg.NBLK * 128, D]); b_Y = Buf()
    out = nc.dram_tensor("out", [OWN, D], F32, kind="ExternalOutput").ap(); b_out = Buf()
    with ExitStack() as es:
        kb.S = Sched(nc, es)
        S = kb.S
        c = build_consts(kb, es)
        R = {}
        R["M"] = T(kb.sb(es, "r_M", [128, NTO, NE])); R["RT"] = T(kb.sb(es, "r_RT", [128, NTO, 4])); R["ioe"] = T(kb.sb(es, "r_ioe", [128, NE]))
        R["IDX1"] = T(kb.sb(es, "r_IDX1", [128, cfg.NBLK, NQ1], I32)); R["IDX2"] = T(kb.sb(es, "r_IDX2", [128, cfg.NBLK, FE * 2], I32))
        R["DEST"] = T(kb.sb(es, "r_DEST", [128, NTO, 2], I32))
        S.op("pool", lambda e: e.iota(R["ioe"][:], pattern=[[1, NE]], base=0, channel_multiplier=0, allow_small_or_imprecise_dtypes=True), writes=[R["ioe"].b])
        if "A" in phases:
            phase_A(kb, c, xb, g1d, wA, PT, b_PT)
            phase_A1(kb, c, xo, g1d, wG, gbd, GATES, b_G)
        if "B" in phases:
            phase_B(kb, c, PT, b_PT, prm, OMIX, b_OMIX)
        if "C" in phases:
            phase_C1(kb, c, xo, own_idx, OMIX, b_OMIX, GATES, b_G, wBR, wO, XMID, b_XMID)
            phase_C2(kb, c, XMID, b_XMID, g2d, wRd, bRd, X2, b_X2, R)
        if "D" in phases:
            phase_D0(kb, c, R, SLOT, b_SLOT)
            phase_D(kb, c, R, SLOT, b_SLOT, X2, b_X2, w1L, w3L, w2L, Y, b_Y)
        if "E" in phases:
            phase_E(kb, c, R, XMID, b_XMID, Y, b_Y, fgd, out, b_out)
        S.barrier()
        print("ninstr", S.ninstr)
    return kb


def prep_shared(cfg, inp):
    sh = {}
    D, KD, RW, DN, RWH, DNH, NE, NG, DE, FE = cfg.D, cfg.KD, cfg.RW, cfg.DN, cfg.RWH, cfg.DNH, cfg.NE, cfg.NG, cfg.DE, cfg.FE
    w_in = inp["w_in"][0]
    ca = cfg.colsA()
    wa = np.zeros((D, cfg.NA * 128), np.float32)
    m = ca >= 0
    wa[:, m] = w_in[:, ca[m]]
    sh["wA"] = np.ascontiguousarray(wa.reshape(KD, 128, cfg.NA, 128).transpose(2, 1, 0, 3))
    del wa
    wg = w_in[:, cfg.GB0:cfg.GB0 + 2 * D]
    KP = cfg.KP
    sh["wG"] = np.ascontiguousarray(wg.reshape(KD // KP, KP, 128, 2 * D // 512, 512).transpose(3, 0, 2, 1, 4))
    sh["gate_b"] = np.ascontiguousarray(inp["gate_b"].reshape(1, 2 * D))
    sh["norm1_g"] = np.ascontiguousarray(inp["norm1_g"].reshape(1, D))
    sh["norm2_g"] = np.ascontiguousarray(inp["norm2_g"].reshape(1, D))
    sh["final_g"] = np.ascontiguousarray(inp["final_g"].reshape(1, D))
    mu = inp["rw_mu"][0]
    vecs = [mu[0:RW], mu[RW + 96:2 * RW + 96], mu[2 * RW + 96:3 * RW + 96], inp["rw_w0"][0], inp["rw_a0"][0], inp["rw_k_k"][0],
            inp["rw_k_a"][0], inp["rw_r_k"][0].reshape(-1), inp["rw_ln_w"][0], inp["rw_ln_b"][0]]
    sh["rwp"] = np.ascontiguousarray(np.stack([v.reshape(RWH, 64).T for v in vecs], axis=1).astype(np.float32))
    lmu = np.zeros((128, 4), np.float32)
    lmu[0:96, 0] = mu[RW:RW + 96]
    lmu[0:96, 1] = mu[3 * RW + 96:3 * RW + 192]
    lmu[:, 2] = mu[3 * RW + 192:3 * RW + 320]
    lmu[:, 3] = mu[3 * RW + 320:3 * RW + 448]
    sh["lmu"] = lmu
    sh["rw_w2"] = np.ascontiguousarray(inp["rw_w2"][0])
    sh["rw_a2"] = np.ascontiguousarray(inp["rw_a2"][0])
    sh["rw_g2"] = np.ascontiguousarray(inp["rw_g2"][0])
    cw = inp["dn_conv_w"][0]
    sh["dnp"] = np.ascontiguousarray(cw.reshape(4, 3, DNH, 128).transpose(3, 0, 1, 2).reshape(128, 12, DNH))
    sh["dnw"] = np.ascontiguousarray(inp["dn_norm_w"][0].reshape(128, 1))
    dnh = np.zeros((16, 2), np.float32)
    dnh[:DNH, 0] = inp["dn_a_log"][0]
    dnh[:DNH, 1] = inp["dn_dt_bias"][0]
    sh["dnh"] = dnh
    wb = inp["w_branch"][0]
    sh["wBR"] = np.ascontiguousarray(wb.reshape(cfg.KB, 128, D // 512, 512).transpose(2, 1, 0, 3))
    wo = inp["w_out"][0]
    sh["wO"] = np.ascontiguousarray(wo.reshape(KD, 128, D // 512, 512).transpose(2, 1, 0, 3))
    wr = np.concatenate([inp["moe_gr_w"][0], inp["moe_er_w"][0]], axis=1)
    sh["wR"] = np.ascontiguousarray(wr.reshape(KD, 128, NG + NE).transpose(1, 0, 2))
    sh["bR"] = np.ascontiguousarray(np.concatenate([inp["moe_gr_b"][0], inp["moe_er_b"][0]]).reshape(1, NG + NE))
    KQ = min(4, KD); NQ1 = KD // KQ
    for nm, key in (("w1L", "moe_w1"), ("w3L", "moe_w3")):
        w = inp[key][0]
        sh[nm] = np.ascontiguousarray(w.reshape(NE, NQ1, KQ, 128, DE).transpose(0, 1, 3, 2, 4)).reshape(NE * NQ1 * 128, KQ * DE)
    w = inp["moe_w2"][0]
    sh["w2L"] = np.ascontiguousarray(w.reshape(NE, FE, 128, 2, D // 2).transpose(0, 1, 3, 2, 4)).reshape(NE * FE * 2 * 128, D // 2)
    return sh


def core_inputs(cfg, inp, sh, b, s):
    d = dict(sh)
    OWN = cfg.OWN
    d["xb"] = np.ascontiguousarray(inp["x"][b])
    d["xo"] = np.ascontiguousarray(inp["x"][b, s * OWN:(s + 1) * OWN])
    d["own_idx"] = np.ascontiguousarray((s * OWN + np.arange(OWN, dtype=np.int32)).reshape(OWN // 128, 128).T)
    return d


_CACHE = {}


def kernel(**inputs):
    cfg = Cfg()
    inp = {k: np.asarray(v) for k, v in inputs.items()}
    if "kb" not in _CACHE:
        _CACHE["kb"] = build_program(cfg)
    kb = _CACHE["kb"]
    sh = prep_shared(cfg, inp)
    B = inp["x"].shape[0]
    ins = []
    for cid in range(8):
        b, s = cid // 2, cid % 2
        d = core_inputs(cfg, inp, sh, b % B, s)
        ins.append({k: v for k, v in d.items() if k in kb.din})
    res = run_bass_kernel_spmd(kb.nc, ins, core_ids=list(range(8)))
    out = np.zeros((B, cfg.S, cfg.D), np.float32)
    for cid in range(8):
        b, s = cid // 2, cid % 2
        out[b, s * cfg.OWN:(s + 1) * cfg.OWN] = res.results[cid]["out"]
    return out
```

```python
import os
import numpy as np
from contextlib import ExitStack
KSTOP = float(os.environ.get('KSTOP', '99'))
BQ = tuple(int(v) for v in os.environ.get('BQ', '1,6').split(','))
FILL = int(os.environ.get('FILL', '1'))
import concourse.bass as bass
import concourse.mybir as mybir
from concourse.bass_utils import run_bass_kernel_spmd

F32 = mybir.dt.float32
BF16 = mybir.dt.bfloat16
I32 = mybir.dt.int32
U32 = mybir.dt.uint32
AF = mybir.ActivationFunctionType
ALU = mybir.AluOpType
AX = mybir.AxisListType

CH = 64


class Cfg:
    def __init__(self, D=4096, S=4096, RWH=32, DNH=16, NG=8, DE=768, SEG=512):
        self.D, self.S, self.RWH, self.DNH, self.NG, self.DE = D, S, RWH, DNH, NG, DE
        self.KD = D // 128
        self.RW = RWH * 64
        self.DN = DNH * 128
        self.BW = self.RW + self.DN
        self.KB = self.BW // 128
        self.OWN = S // 2
        self.NE = NG * 8
        self.TG = min(1024, S)
        self.TGO = min(512, self.OWN)
        self.SEG = min(SEG, S)
        self.KP = min(8, self.KD)
        self.RW_COLS = 3 * self.RW + 448
        self.DN_COLS = 3 * self.DN + 2 * DNH + self.DN
        self.GB0 = self.RW_COLS + self.DN_COLS
        self.IN_COLS = self.GB0 + 2 * D
        rows = {}
        o = 0
        for name, n in (("r", self.RW), ("k", self.RW), ("v", self.RW), ("wd", 128), ("ad", 128), ("gd", 256),
                        ("dq", self.DN), ("dk", self.DN), ("dv", self.DN), ("ba", 128), ("z", self.DN)):
            rows[name] = o
            o += n
        self.rows = rows
        self.NA = o // 128
        self.NBLK = (2 * self.OWN + self.NE * 128) // 128
        self.FE = DE // 128

    def colsA(self):
        c = self
        RW, DN, DNH = c.RW, c.DN, c.DNH
        idx = -np.ones(c.NA * 128, np.int64)
        r = c.rows
        idx[r["r"]:r["r"] + RW] = np.arange(0, RW)
        idx[r["wd"]:r["wd"] + 96] = np.arange(RW, RW + 96)
        idx[r["k"]:r["k"] + RW] = np.arange(RW + 96, 2 * RW + 96)
        idx[r["v"]:r["v"] + RW] = np.arange(2 * RW + 96, 3 * RW + 96)
        idx[r["ad"]:r["ad"] + 96] = np.arange(3 * RW + 96, 3 * RW + 192)
        idx[r["gd"]:r["gd"] + 256] = np.arange(3 * RW + 192, 3 * RW + 448)
        b0 = c.RW_COLS
        idx[r["dq"]:r["dq"] + DN] = b0 + np.arange(0, DN)
        idx[r["dk"]:r["dk"] + DN] = b0 + np.arange(DN, 2 * DN)
        idx[r["dv"]:r["dv"] + DN] = b0 + np.arange(2 * DN, 3 * DN)
        idx[r["ba"]:r["ba"] + DNH] = b0 + 3 * DN + np.arange(0, DNH)
        idx[r["ba"] + 32:r["ba"] + 32 + DNH] = b0 + 3 * DN + DNH + np.arange(0, DNH)
        idx[r["z"]:r["z"] + DN] = b0 + 3 * DN + 2 * DNH + np.arange(0, DN)
        return idx


class Buf:
    __slots__ = ("w", "r", "x")

    def __init__(self, excl=False):
        self.w = {}
        self.r = {}
        self.x = excl


class Sched:
    def __init__(self, nc, es, n_dma_sems=24):
        self.nc = nc
        self.eng = {"pe": nc.tensor, "act": nc.scalar, "dve": nc.vector, "pool": nc.gpsimd, "sp": nc.sync}
        self.sems = []
        self.semidx = {}
        self.cnt = {}
        for k in ("pe", "act", "dve", "pool"):
            s = es.enter_context(nc.semaphore("s_" + k))
            self.semidx[k] = len(self.sems)
            self.sems.append(s)
            self.cnt[k] = 0
        self.dma_ids = []
        for i in range(n_dma_sems):
            s = es.enter_context(nc.semaphore("s_d%d" % i))
            self.dma_ids.append(len(self.sems))
            self.sems.append(s)
        self.dtotal = {i: 0 for i in self.dma_ids}
        self.dnext = 0
        self.known = {k: {} for k in self.eng}
        self.ninstr = 0
        self.tick = None

    def _wait(self, ek, need):
        kn = self.known[ek]
        e = self.eng[ek]
        own = self.semidx.get(ek)
        for s, v in need.items():
            if ek == "pe" and s == own:
                continue
            if kn.get(s, 0) < v:
                e.wait_ge(self.sems[s], v)
                kn[s] = v
                self.ninstr += 1

    def _deps(self, ek, reads, writes):
        need = {}
        for b in reads:
            if b.x:
                for s, v in b.r.items():
                    if need.get(s, 0) < v:
                        need[s] = v
            for s, v in b.w.items():
                if need.get(s, 0) < v:
                    need[s] = v
        for b in writes:
            for s, v in b.w.items():
                if need.get(s, 0) < v:
                    need[s] = v
            for s, v in b.r.items():
                if need.get(s, 0) < v:
                    need[s] = v
        self._wait(ek, need)

    def op(self, ek, fn, reads=(), writes=()):
        if self.tick is not None:
            self.tick()
        self._deps(ek, reads, writes)
        ins = fn(self.eng[ek])
        self.cnt[ek] += 1
        s = self.semidx[ek]
        ins.then_inc(self.sems[s], 1)
        v = self.cnt[ek]
        for b in reads:
            b.r[s] = v
            if b.x:
                b.w[s] = v
        for b in writes:
            b.w[s] = v
        self.ninstr += 1
        return ins

    def dma(self, qk, fn, reads=(), writes=()):
        if self.tick is not None:
            self.tick()
        self._deps(qk, reads, writes)
        s = self.dma_ids[self.dnext]
        self.dnext = (self.dnext + 1) % len(self.dma_ids)
        prev = self.dtotal[s]
        kn = self.known[qk]
        if prev and kn.get(s, 0) < prev:
            self.eng[qk].wait_ge(self.sems[s], prev)
            kn[s] = prev
        ins = fn(self.eng[qk])
        ins.then_inc(self.sems[s], 16)
        v = prev + 16
        self.dtotal[s] = v
        for b in reads:
            b.r[s] = v
        for b in writes:
            b.w[s] = v
        self.ninstr += 1
        return ins

    def barrier(self):
        need = {self.semidx[k]: self.cnt[k] for k in self.cnt if self.cnt[k]}
        for s, v in self.dtotal.items():
            if v:
                need[s] = v
        for ek in self.eng:
            self._wait(ek, dict(need))


import threading
_tl = threading.local()


def run_pair(S, f0, f1, quota=(1, 3)):
    if f0 is None:
        return f1()
    if f1 is None:
        return f0()
    st = {"turn": 0, "alive": [True, True], "used": 0, "err": None}
    cv = threading.Condition()

    def tick():
        tid = _tl.tid
        with cv:
            st["used"] += 1
            if st["used"] >= quota[tid] and st["alive"][1 - tid]:
                st["used"] = 0
                st["turn"] = 1 - tid
                cv.notify_all()
                while st["turn"] != tid:
                    cv.wait()

    def worker(tid, f):
        _tl.tid = tid
        with cv:
            while st["turn"] != tid:
                cv.wait()
        try:
            f()
        except BaseException as e:
            st["err"] = e
        finally:
            with cv:
                st["alive"][tid] = False
                st["used"] = 0
                st["turn"] = 1 - tid
                cv.notify_all()
    S.tick = tick
    ths = [threading.Thread(target=worker, args=(i, f)) for i, f in enumerate((f0, f1))]
    for t in ths:
        t.start()
    for t in ths:
        t.join()
    S.tick = None
    if st["err"] is not None:
        raise st["err"]


class K:
    def __init__(self, cfg, debug=()):
        self.cfg = cfg
        self.debug = set(debug)
        self.nc = bass.Bass("TRN2", target_bir_lowering=False)
        self.es = ExitStack()
        self.S = None
        self.din = {}

    def inp(self, name, shape, dt=F32):
        t = self.nc.dram_tensor(name, list(shape), dt, kind="ExternalInput").ap()
        self.din[name] = t
        return t

    def scratch(self, name, shape, dt=F32):
        kind = "ExternalOutput" if name in self.debug else "Internal"
        return self.nc.dram_tensor(name, list(shape), dt, kind=kind).ap()

    def sb(self, es, name, shape, dt=F32):
        return es.enter_context(self.nc.sbuf_tensor(name, list(shape), dt))

    def ps(self, es, name, shape, dt=F32):
        return es.enter_context(self.nc.psum_tensor(name, list(shape), dt))


def build_consts(kb, es):
    S, nc = kb.S, kb.nc
    c = {}
    ident = kb.sb(es, "ident", [128, 128]); b = Buf()
    S.op("pool", lambda e: e.memset(ident[:], 0.0), writes=[b])
    S.op("pool", lambda e: e.affine_select(out=ident[:], in_=ident[:], pattern=[[-1, 128]], compare_op=ALU.not_equal,
                                           fill=1.0, base=0, channel_multiplier=1), reads=[b], writes=[b])
    identb = kb.sb(es, "identb", [128, 128], BF16)
    S.op("dve", lambda e: e.tensor_copy(out=identb[:], in_=ident[:]), reads=[b], writes=[b])
    c["ident"], c["identb"], c["b_ident"] = ident, identb, b
    col = kb.sb(es, "ccol", [128, 8]); bc = Buf()
    for i, v in enumerate((1e-6, 64e-5, 1.0, 0.0, -1.0)):
        S.op("pool", lambda e: e.memset(col[:, i:i + 1], v), writes=[bc])
    c["col"], c["b_col"] = col, bc
    c["EPS6"], c["GNEPS"], c["ONE"], c["ZERO"] = col[:, 0:1], col[:, 1:2], col[:, 2:3], col[:, 3:4]
    return c


def rmsnorm_rstd(kb, c, ss, b_ss, n, nparts=128):
    S = kb.S
    S.op("dve", lambda e: e.tensor_scalar(out=ss, in0=ss, scalar1=1.0 / n, scalar2=1e-6, op0=ALU.mult, op1=ALU.add),
         reads=[b_ss], writes=[b_ss])
    S.op("act", lambda e: e.activation(out=ss, in_=ss, func=AF.Ln), reads=[b_ss], writes=[b_ss])
    S.op("act", lambda e: e.activation(out=ss, in_=ss, func=AF.Exp, scale=-0.5), reads=[b_ss], writes=[b_ss])


def phase_A(kb, c, xb, g1d, wA, PT, b_PT):
    cfg, S, nc = kb.cfg, kb.S, kb.nc
    D, KD, TG, NA = cfg.D, cfg.KD, cfg.TG, cfg.NA
    with ExitStack() as es:
        hT = kb.sb(es, "A_hT", [128, KD, TG], BF16); b_hT = Buf()
        xt = kb.sb(es, "A_xt", [128, D]); b_xt = Buf()
        hb = kb.sb(es, "A_hb", [128, D], BF16); b_hb = Buf()
        ss = kb.sb(es, "A_ss", [128, 1]); b_ss = Buf()
        g1 = kb.sb(es, "A_g1", [128, D]); b_g1 = Buf()
        slf = [kb.sb(es, "A_slf%d" % i, [128, KD, 128]) for i in range(2)]; b_slf = [Buf(), Buf()]
        slb = [kb.sb(es, "A_slb%d" % i, [128, KD, 128], BF16) for i in range(2)]; b_slb = [Buf(), Buf()]
        stg = [kb.sb(es, "A_stg%d" % i, [128, TG]) for i in range(2)]; b_stg = [Buf(), Buf()]
        psT = [kb.ps(es, "A_psT%d" % i, [128, 1024], BF16) for i in range(2)]; b_psT = [Buf(True), Buf(True)]
        psA = [kb.ps(es, "A_psA%d" % i, [128, 512]) for i in range(4)]; b_psA = [Buf(True) for _ in range(4)]
        S.dma("sp", lambda e: e.dma_start(out=g1[:], in_=g1d.partition_broadcast(128)), writes=[b_g1])
        it = 0
        pcnt = 0
        for tg in range(cfg.S // TG):
            for ti in range(TG // 128):
                t0 = tg * TG + ti * 128
                S.dma("sp", lambda e: e.dma_start(out=xt[:], in_=xb[t0:t0 + 128, :]), writes=[b_xt])
                S.op("act", lambda e: e.activation(out=hb[:], in_=xt[:], func=AF.Square, accum_out=ss[:]),
                     reads=[b_xt], writes=[b_hb, b_ss])
                rmsnorm_rstd(kb, c, ss[:], b_ss, D)
                S.op("dve", lambda e: e.scalar_tensor_tensor(out=hb[:], in0=xt[:], scalar=ss[:, 0:1], in1=g1[:],
                                                             op0=ALU.mult, op1=ALU.mult),
                     reads=[b_xt, b_ss, b_g1], writes=[b_hb])
                for k0 in range(0, KD, 8):
                    kn = min(8, KD - k0)
                    p = psT[pcnt % 2]; bp = b_psT[pcnt % 2]; pcnt += 1
                    for k in range(kn):
                        S.op("pe", lambda e: e.transpose(out=p[:, k * 128:(k + 1) * 128],
                                                         in_=hb[:, (k0 + k) * 128:(k0 + k + 1) * 128],
                                                         identity=c["identb"][:]),
                             reads=[b_hb, c["b_ident"]], writes=[bp])
                    ek = "act" if (pcnt % 2) else "dve"
                    src = p[:, 0:kn * 128].rearrange("p (a b) -> p a b", a=kn)
                    dst = hT[:, k0:k0 + kn, ti * 128:(ti + 1) * 128]
                    if ek == "act":
                        S.op("act", lambda e: e.copy(out=dst, in_=src), reads=[bp], writes=[b_hT])
                    else:
                        S.op("dve", lambda e: e.tensor_copy(out=dst, in_=src), reads=[bp], writes=[b_hT])
            for j in range(NA):
                sl = it % 2
                S.dma("sp", lambda e: e.dma_start(out=slf[sl][:], in_=wA[j]), writes=[b_slf[sl]])
                if it % 2 == 0:
                    S.op("act", lambda e: e.copy(out=slb[sl][:], in_=slf[sl][:]), reads=[b_slf[sl]], writes=[b_slb[sl]])
                else:
                    S.op("dve", lambda e: e.tensor_copy(out=slb[sl][:], in_=slf[sl][:]), reads=[b_slf[sl]], writes=[b_slb[sl]])
                for hf in range(TG // 512 if TG >= 512 else 1):
                    w = min(512, TG)
                    p = psA[pcnt % 4]; bp = b_psA[pcnt % 4]; pcnt += 1
                    for k in range(KD):
                        S.op("pe", lambda e: e.matmul(p[:, 0:w], lhsT=slb[sl][:, k, :], rhs=hT[:, k, hf * w:(hf + 1) * w],
                                                      start=(k == 0), stop=(k == KD - 1)),
                             reads=[b_slb[sl], b_hT], writes=[bp])
                    if pcnt % 2:
                        S.op("dve", lambda e: e.tensor_copy(out=stg[sl][:, hf * w:(hf + 1) * w], in_=p[:, 0:w]),
                             reads=[bp], writes=[b_stg[sl]])
                    else:
                        S.op("act", lambda e: e.copy(out=stg[sl][:, hf * w:(hf + 1) * w], in_=p[:, 0:w]),
                             reads=[bp], writes=[b_stg[sl]])
                S.dma("pool", lambda e: e.dma_start(out=PT[j * 128:(j + 1) * 128, tg * TG:(tg + 1) * TG], in_=stg[sl][:]),
                      reads=[b_stg[sl]], writes=[b_PT[j]])
                it += 1
    S.barrier()


RW_DECAY_SCALE = 0.606531


def build_masks(kb, es, c):
    S = kb.S
    m4 = kb.sb(es, "m4", [128, 128]); b = Buf()
    S.op("pool", lambda e: e.memset(m4[0:64, 0:64], 0.0), writes=[b])
    for (r0, c0, val, op) in ((0, 64, -1.0, ALU.is_ge), (64, 0, 1.0, ALU.is_gt), (64, 64, 1.0, ALU.is_ge)):
        blk = m4[r0:r0 + 64, c0:c0 + 64]
        S.op("pool", lambda e: e.memset(blk, val), writes=[b])
        S.op("pool", lambda e: e.affine_select(out=blk, in_=blk, pattern=[[1, 64]], compare_op=op, fill=0.0, base=0,
                                               channel_multiplier=-1), reads=[b], writes=[b])
    ml = kb.sb(es, "ml", [64, 64])
    S.op("pool", lambda e: e.memset(ml[:], 1.0), writes=[b])
    S.op("pool", lambda e: e.affine_select(out=ml[:], in_=ml[:], pattern=[[-1, 64]], compare_op=ALU.is_gt, fill=0.0, base=0,
                                           channel_multiplier=1), reads=[b], writes=[b])
    mup = kb.sb(es, "mup", [64, 64])
    S.op("pool", lambda e: e.memset(mup[:], 1.0), writes=[b])
    S.op("pool", lambda e: e.affine_select(out=mup[:], in_=mup[:], pattern=[[1, 64]], compare_op=ALU.is_gt, fill=0.0, base=0,
                                           channel_multiplier=-1), reads=[b], writes=[b])
    ones = kb.sb(es, "ones", [128, 128])
    S.op("pool", lambda e: e.memset(ones[:], 1.0), writes=[b])
    mneg = kb.sb(es, "mneg", [128, 128])
    S.op("pool", lambda e: e.memset(mneg[:], 0.0), writes=[b])
    for (r0, c0, op) in ((0, 0, ALU.is_gt), (0, 64, ALU.is_ge), (64, 0, ALU.is_gt), (64, 64, ALU.is_ge)):
        blk = mneg[r0:r0 + 64, c0:c0 + 64]
        S.op("pool", lambda e: e.affine_select(out=blk, in_=blk, pattern=[[1, 64]], compare_op=op, fill=-30000.0, base=0,
                                               channel_multiplier=-1), reads=[b], writes=[b])
    sgn = kb.sb(es, "sgn", [128, 1])
    S.op("pool", lambda e: e.memset(sgn[0:64, :], -1.0), writes=[b])
    S.op("pool", lambda e: e.memset(sgn[64:128, :], 1.0), writes=[b])
    c.update(m4=m4, ml=ml, mup=mup, ones=ones, sgn=sgn, mneg=mneg, b_mask=b)


class T:
    def __init__(self, ap, excl=False, b=None):
        self.ap = ap
        self.b = b if b is not None else Buf(excl)

    def __getitem__(self, k):
        return self.ap[k]


def dplr_segment(kb, c, u, Kd, NCH, P):
    S = kb.S
    V = Kd
    QR, BK, BKe, VV, wC, H, OT = (u[k] for k in ("QR", "BK", "BKe", "VV", "wC", "H", "OT"))
    Hb = u["Hb"]
    G4, A, AT, Y, YT, TT, Xp, Up, WmT, KKt, KBe, UV = (P[k] for k in ("G4", "A", "AT", "Y", "YT", "TT", "Xp", "Up", "WmT", "KKt", "KBe", "UV"))
    pg, pinv, pm, pseq = P["pg"], P["pinv"], P["pm"], P["pseq"]
    m4, ml, ident, sgn, bm, bi = c["m4"], c["ml"], c["identb"], c["sgn"], c["b_mask"], c["b_ident"]
    pmb = T(pm.ap.bitcast(BF16), b=pm.b)
    pAb = T(pinv[0].ap.bitcast(BF16), b=pinv[0].b)
    safe = "QRg" in u
    QRg = u["QRg"] if safe else QR
    for c0 in range(0, NCH, 4):
        n = min(4, NCH - c0)
        for i in range(n):
            S.op("pe", lambda e: e.matmul(pg[:, i * 128:(i + 1) * 128], lhsT=BK[0:Kd, c0 + i, :], rhs=QRg[0:Kd, c0 + i, :],
                                          start=True, stop=True), reads=[BK.b, QRg.b], writes=[pg.b])
        S.op("dve", lambda e: e.tensor_tensor(out=G4[:, c0:c0 + n, :], in0=pg[:, 0:n * 128].rearrange("p (a b) -> p a b", a=n),
                                              in1=m4[:].unsqueeze(1).to_broadcast([128, n, 128]), op=ALU.mult),
             reads=[pg.b, bm], writes=[G4.b])
        S.op("dve", lambda e: e.tensor_tensor(out=AT[0:64, c0:c0 + n, :], in0=pg[0:64, 0:n * 128].rearrange("p (a b) -> p a b", a=n)[:, :, 0:64],
                                              in1=c["mup"][:].unsqueeze(1).to_broadcast([64, n, 64]), op=ALU.mult),
             reads=[pg.b, bm], writes=[AT.b])
        if safe:
            pe_ = pinv[1]
            NA, E, nones = u["NA"], u["E"], u["nones"]
            ones = c["ones"]
            for i in range(n):
                cc_ = c0 + i
                for hf in range(2):
                    o_ = i * 128 + hf * 64
                    S.op("pe", lambda e: e.matmul(pe_[:, o_:o_ + 64], lhsT=ones[0:1, 0:128], rhs=NA[0:1, cc_, hf * 64:(hf + 1) * 64],
                                                  start=True, stop=False), reads=[NA.b, bm], writes=[pe_.b])
                    S.op("pe", lambda e: e.matmul(pe_[:, o_:o_ + 64], lhsT=NA[0:1, cc_, :], rhs=nones[0:1, 0:64],
                                                  start=False, stop=True), reads=[NA.b, nones.b], writes=[pe_.b])
            S.op("dve", lambda e: e.tensor_tensor(out=E[:, c0:c0 + n, :], in0=pe_[:, 0:n * 128].rearrange("p (a b) -> p a b", a=n),
                                                  in1=c["mneg"][:].unsqueeze(1).to_broadcast([128, n, 128]), op=ALU.add),
                 reads=[pe_.b, bm], writes=[E.b])
            S.op("act", lambda e: e.activation(out=E[:, c0:c0 + n, :], in_=E[:, c0:c0 + n, :], func=AF.Exp), reads=[E.b], writes=[E.b])
            S.op("pool", lambda e: e.tensor_tensor(out=G4[:, c0:c0 + n, :], in0=G4[:, c0:c0 + n, :], in1=E[:, c0:c0 + n, :], op=ALU.mult),
                 reads=[G4.b, E.b], writes=[G4.b])
            S.op("pool", lambda e: e.tensor_tensor(out=AT[0:64, c0:c0 + n, :], in0=AT[0:64, c0:c0 + n, :], in1=E[0:64, c0:c0 + n, 0:64], op=ALU.mult),
                 reads=[AT.b, E.b], writes=[AT.b])
    pA = pinv[0]
    if safe:
        for i in range(NCH):
            S.op("pe", lambda e: e.transpose(out=pAb[0:64, i * 64:(i + 1) * 64], in_=AT[0:64, i, :], identity=ident[0:64, 0:64]),
                 reads=[AT.b, bi], writes=[pA.b])
        S.op("dve", lambda e: e.tensor_copy(out=A[0:64, :, :], in_=pAb[0:64, 0:NCH * 64].rearrange("p (a b) -> p a b", a=NCH)),
             reads=[pA.b], writes=[A.b])
    else:
        for i in range(NCH):
            S.op("pe", lambda e: e.matmul(pA[0:64, i * 64:(i + 1) * 64], lhsT=QR[0:Kd, i, 0:64], rhs=BK[0:Kd, i, 0:64],
                                          start=True, stop=True), reads=[QR.b, BK.b], writes=[pA.b])
        S.op("dve", lambda e: e.tensor_tensor(out=A[0:64, :, :], in0=pA[0:64, 0:NCH * 64].rearrange("p (a b) -> p a b", a=NCH),
                                              in1=ml[:].unsqueeze(1).to_broadcast([64, NCH, 64]), op=ALU.mult),
             reads=[pA.b, bm], writes=[A.b])
    S.op("dve", lambda e: e.scalar_tensor_tensor(out=TT[0:64, :, :], in0=AT[0:64, :, :], scalar=-1.0,
                                                 in1=c["ident"][0:64, 0:64].unsqueeze(1).to_broadcast([64, NCH, 64]),
                                                 op0=ALU.mult, op1=ALU.add), reads=[AT.b, bi], writes=[TT.b])
    nb = 512 // V
    fillers = []

    def f_p8(c0, n):
        for i in range(n):
            S.op("pe", lambda e: e.transpose(out=pmb[:, i * V:(i + 1) * V], in_=VV[0:Kd, c0 + i, :], identity=ident[0:Kd, 0:Kd]),
                 reads=[VV.b, bi], writes=[pm.b])
        S.op("act", lambda e: e.copy(out=UV[64:128, c0:c0 + n, 0:V], in_=pmb[64:128, 0:n * V].rearrange("p (a b) -> p a b", a=n)),
             reads=[pm.b], writes=[UV.b])

    def f_p6(c0, n):
        for i in range(n):
            S.op("pe", lambda e: e.transpose(out=pmb[:, i * V:(i + 1) * V], in_=BKe[0:Kd, c0 + i, :], identity=ident[0:Kd, 0:Kd]),
                 reads=[BKe.b, bi], writes=[pm.b])
        S.op("dve", lambda e: e.tensor_scalar(out=KBe[:, c0:c0 + n, 0:Kd], in0=pmb[:, 0:n * V].rearrange("p (a b) -> p a b", a=n),
                                              scalar1=sgn[:, 0:1], scalar2=None, op0=ALU.mult), reads=[pm.b, bm], writes=[KBe.b])

    def f_p7(c0, n):
        for i in range(n):
            S.op("pe", lambda e: e.transpose(out=pmb[0:64, i * V:(i + 1) * V], in_=QR[0:Kd, c0 + i, 0:64], identity=ident[0:Kd, 0:Kd]),
                 reads=[QR.b, bi], writes=[pm.b])
        S.op("act", lambda e: e.copy(out=KKt[0:64, c0:c0 + n, 0:Kd], in_=pmb[0:64, 0:n * V].rearrange("p (a b) -> p a b", a=n)),
             reads=[pm.b], writes=[KKt.b])

    def f_p4(c0, n):
        for i in range(n):
            S.op("pe", lambda e: e.matmul(pm[0:64, i * V:(i + 1) * V], lhsT=G4[:, c0 + i, 0:64], rhs=UV[:, c0 + i, 0:V],
                                          start=True, stop=True), reads=[G4.b, UV.b], writes=[pm.b])
        S.op("dve", lambda e: e.tensor_copy(out=Xp[0:64, c0:c0 + n, 0:V], in_=pm[0:64, 0:n * V].rearrange("p (a b) -> p a b", a=n)),
             reads=[pm.b], writes=[Xp.b])
    for c0 in range(0, NCH, nb):
        n = min(nb, NCH - c0)
        for fn in (f_p8, f_p6, f_p7, f_p4):
            fillers.append((fn, c0, n))
    fillers.reverse()
    Yc, YTc = A, AT
    for lvl in range(5):
        Yn, YTn = Y[lvl % 2], YT[lvl % 2]
        pY, pYT, pT_ = pinv[0], pinv[1], pinv[2]
        for i in range(NCH):
            S.op("pe", lambda e: e.matmul(pY[0:64, i * 64:(i + 1) * 64], lhsT=YTc[0:64, i, :], rhs=Yc[0:64, i, :],
                                          start=True, stop=True), reads=[YTc.b, Yc.b], writes=[pY.b])
        S.op("act", lambda e: e.copy(out=Yn[0:64, :, :], in_=pY[0:64, 0:NCH * 64].rearrange("p (a b) -> p a b", a=NCH)),
             reads=[pY.b], writes=[Yn.b])
        if lvl < 4:
            for i in range(NCH):
                S.op("pe", lambda e: e.matmul(pYT[0:64, i * 64:(i + 1) * 64], lhsT=Yc[0:64, i, :], rhs=YTc[0:64, i, :],
                                              start=True, stop=True), reads=[YTc.b, Yc.b], writes=[pYT.b])
            S.op("dve", lambda e: e.tensor_copy(out=YTn[0:64, :, :], in_=pYT[0:64, 0:NCH * 64].rearrange("p (a b) -> p a b", a=NCH)),
                 reads=[pYT.b], writes=[YTn.b])
        for k_ in range(FILL):
            if fillers:
                fn, c0, n = fillers.pop()
                fn(c0, n)
        for i in range(NCH):
            S.op("pe", lambda e: e.matmul(pT_[0:64, i * 64:(i + 1) * 64], lhsT=Yn[0:64, i, :], rhs=TT[0:64, i, :],
                                          start=True, stop=True), reads=[Yn.b, TT.b], writes=[pT_.b])
        S.op("dve", lambda e: e.tensor_tensor(out=TT[0:64, :, :], in0=TT[0:64, :, :],
                                              in1=pT_[0:64, 0:NCH * 64].rearrange("p (a b) -> p a b", a=NCH), op=ALU.add),
             reads=[pT_.b, TT.b], writes=[TT.b])
        Yc, YTc = Yn, YTn
    while fillers:
        fn, c0, n = fillers.pop()
        fn(c0, n)
    for c0 in range(0, NCH, nb):
        n = min(nb, NCH - c0)
        for i in range(n):
            S.op("pe", lambda e: e.matmul(pm[0:64, i * V:(i + 1) * V], lhsT=TT[0:64, c0 + i, :], rhs=Xp[0:64, c0 + i, 0:V],
                                          start=True, stop=True), reads=[TT.b, Xp.b], writes=[pm.b])
        S.op("act", lambda e: e.copy(out=Up[0:64, c0:c0 + n, 0:V], in_=pm[0:64, 0:n * V].rearrange("p (a b) -> p a b", a=n)),
             reads=[pm.b], writes=[Up.b])
    if KSTOP < 4.55:
        return
    for i in range(NCH):
        S.op("pe", lambda e: e.matmul(pm[0:Kd, i * 64:(i + 1) * 64], lhsT=KKt[0:64, i, 0:Kd], rhs=TT[0:64, i, :],
                                      start=True, stop=True), reads=[KKt.b, TT.b], writes=[pm.b])
    S.op("dve", lambda e: e.tensor_copy(out=WmT[0:Kd, :, :], in_=pm[0:Kd, 0:NCH * 64].rearrange("p (a b) -> p a b", a=NCH)),
         reads=[pm.b], writes=[WmT.b])
    if KSTOP < 5:
        return
    pU, pO, pH = pseq
    for i in range(NCH):
        S.op("pe", lambda e: e.matmul(pU[0:64, 0:V], lhsT=WmT[0:Kd, i, :], rhs=Hb[0:Kd, :], start=True, stop=True),
             reads=[WmT.b, Hb.b], writes=[pU.b])
        S.op("dve", lambda e: e.tensor_tensor(out=UV[0:64, i, 0:V], in0=pU[0:64, 0:V], in1=Up[0:64, i, 0:V], op=ALU.add),
             reads=[pU.b, Up.b], writes=[UV.b])
        S.op("pe", lambda e: e.matmul(pO[0:V, 0:64], lhsT=Hb[0:Kd, :], rhs=QR[0:Kd, i, 64:128], start=True, stop=False),
             reads=[Hb.b, QR.b], writes=[pO.b])
        S.op("pe", lambda e: e.matmul(pO[0:V, 0:64], lhsT=UV[:, i, 0:V], rhs=G4[:, i, 64:128], start=False, stop=True),
             reads=[UV.b, G4.b], writes=[pO.b])
        S.op("act", lambda e: e.copy(out=OT[0:V, i * 64:(i + 1) * 64], in_=pO[0:V, 0:64]), reads=[pO.b], writes=[OT.b])
        S.op("pe", lambda e: e.matmul(pH[0:Kd, 0:V], lhsT=KBe[:, i, 0:Kd], rhs=UV[:, i, 0:V], start=True, stop=True),
             reads=[KBe.b, UV.b], writes=[pH.b])
        S.op("dve", lambda e: e.scalar_tensor_tensor(out=Hb[0:Kd, :], in0=H[0:Kd, :], scalar=wC[0:Kd, i:i + 1], in1=pH[0:Kd, 0:V],
                                                     op0=ALU.mult, op1=ALU.add), reads=[H.b, wC.b, pH.b], writes=[Hb.b])
        S.op("dve", lambda e: e.scalar_tensor_tensor(out=H[0:Kd, :], in0=H[0:Kd, :], scalar=wC[0:Kd, i:i + 1], in1=pH[0:Kd, 0:V],
                                                     op0=ALU.mult, op1=ALU.add), reads=[H.b, wC.b, pH.b], writes=[H.b])


def phase_B(kb, c, PT, b_PT, prm, OMIX, b_OMIX):
    cfg, S, nc = kb.cfg, kb.S, kb.nc
    W = cfg.SEG
    NCH = W // 64
    RWH, DNH, RW, DN = cfg.RWH, cfg.DNH, cfg.RW, cfg.DN
    rows = cfg.rows
    c0 = RW_DECAY_SCALE
    with ExitStack() as es:
        build_masks(kb, es, c)
        ones, ident = c["ones"], c["ident"]
        bm, bi, bcol = c["b_mask"], c["b_ident"], c["b_col"]
        mk = lambda name, shape, dt=F32: T(kb.sb(es, "B_" + name, shape, dt))
        rwp = mk("rwp", [64, 13, RWH])
        lmu = mk("lmu", [128, 8])
        w2 = mk("w2", [96, RW]); a2 = mk("a2", [96, RW]); g2 = mk("g2", [128, 2, RW])
        dnp = mk("dnp", [128, 12, DNH]); dnw = mk("dnw", [128, 1]); dnh = mk("dnh", [16, 3])
        sel = mk("sel", [16, 16, 128])
        omka = mk("omka", [64, RWH])
        S.dma("sp", lambda e: e.dma_start(out=rwp[:, 0:10, :], in_=prm["rwp"]), writes=[rwp.b])
        S.dma("sp", lambda e: e.dma_start(out=lmu[:, 0:4], in_=prm["lmu"]), writes=[lmu.b])
        S.dma("sp", lambda e: e.dma_start(out=w2[:], in_=prm["rw_w2"]), writes=[w2.b])
        S.dma("sp", lambda e: e.dma_start(out=a2[:], in_=prm["rw_a2"]), writes=[a2.b])
        S.dma("sp", lambda e: e.dma_start(out=g2[:], in_=prm["rw_g2"].rearrange("(a p) n -> p a n", p=128)), writes=[g2.b])
        S.dma("sp", lambda e: e.dma_start(out=dnp[:], in_=prm["dnp"]), writes=[dnp.b])
        S.dma("sp", lambda e: e.dma_start(out=dnw[:], in_=prm["dnw"]), writes=[dnw.b])
        S.dma("sp", lambda e: e.dma_start(out=dnh[:, 0:2], in_=prm["dnh"]), writes=[dnh.b])
        S.op("dve", lambda e: e.tensor_scalar(out=rwp[:, 10:13, :], in0=rwp[:, 0:3, :], scalar1=-1.0, scalar2=1.0, op0=ALU.mult, op1=ALU.add),
             reads=[rwp.b], writes=[rwp.b])
        S.op("dve", lambda e: e.tensor_scalar(out=lmu[:, 4:8], in0=lmu[:, 0:4], scalar1=-1.0, scalar2=1.0, op0=ALU.mult, op1=ALU.add),
             reads=[lmu.b], writes=[lmu.b])
        S.op("dve", lambda e: e.tensor_scalar(out=omka[:], in0=rwp[:, 6, :], scalar1=-1.0, scalar2=1.0, op0=ALU.mult, op1=ALU.add),
             reads=[rwp.b], writes=[omka.b])
        S.op("act", lambda e: e.activation(out=dnh[:, 2:3], in_=dnh[:, 0:1], func=AF.Exp), reads=[dnh.b], writes=[dnh.b])
        S.op("dve", lambda e: e.tensor_scalar(out=dnh[:, 2:3], in0=dnh[:, 2:3], scalar1=-1.0, scalar2=None, op0=ALU.mult),
             reads=[dnh.b], writes=[dnh.b])
        S.op("pool", lambda e: e.memset(sel[:], 1.0), writes=[sel.b])
        S.op("pool", lambda e: e.affine_select(out=sel[:], in_=sel[:], pattern=[[1, 16], [0, 128]], compare_op=ALU.is_equal, fill=0.0,
                                               base=0, channel_multiplier=-1), reads=[sel.b], writes=[sel.b])
        rmask = mk("rmask", [128, W])
        S.op("pool", lambda e: e.memset(rmask[:], 1.0), writes=[rmask.b])
        S.op("pool", lambda e: e.memset(rmask[:].rearrange("p (a b) -> p a b", b=64)[:, :, 0:1], 0.0), writes=[rmask.b])
        Hrw = [mk("Hrw%d" % h, [64, 64]) for h in range(RWH)]
        Hdn = [mk("Hdn%d" % h, [128, 128]) for h in range(DNH)]
        Hrwb = [mk("Hrwb%d" % h, [64, 64], BF16) for h in range(RWH)]
        Hdnb = [mk("Hdnb%d" % h, [128, 128], BF16) for h in range(DNH)]
        for Ht in Hrw + Hdn + Hrwb + Hdnb:
            S.op("pool", lambda e: e.memset(Ht[:], 0.0), writes=[Ht.b])
        def mku(par):
            d = {k: mk("%s_%d" % (k, par), [128, NCH, 128], BF16) for k in ("QR", "BK", "BKe", "VV", "QRg")}
            d["wC"] = mk("wC_%d" % par, [128, NCH])
            d["NA"] = mk("NA_%d" % par, [1, NCH, 128])
            d["HO1"] = mk("HO1_%d" % par, [128, W]); d["HO2"] = mk("HO2_%d" % par, [128, W])
            S.op("pool", lambda e: e.memset(d["VV"][:], 0.0), writes=[d["VV"].b])
            return d
        usets = [mku(0), mku(1)]
        OT = mk("OT", [128, W])
        nones = mk("nones", [1, 64])
        S.op("pool", lambda e: e.memset(nones[:], -1.0), writes=[nones.b])
        P = {k: mk(k, [128, NCH, 128], BF16) for k in ("G4", "Xp", "KKt", "KBe", "UV")}
        P["Up"] = mk("Up", [128, NCH, 128])
        P["E"] = mk("E", [128, NCH, 128])
        S.op("pool", lambda e: e.memset(P["UV"][:], 0.0), writes=[P["UV"].b])
        P["WmT"] = mk("WmT", [128, NCH, 64], BF16)
        for k in ("A", "AT", "TT"):
            P[k] = mk(k, [64, NCH, 64], BF16)
        P["Y"] = [mk("Y%d" % i, [64, NCH, 64], BF16) for i in range(2)]
        P["YT"] = [mk("YT%d" % i, [64, NCH, 64], BF16) for i in range(2)]
        pst = lambda name: T(kb.ps(es, "Bp_" + name, [128, 512]), excl=True)
        P["pg"] = pst("g"); P["pinv"] = [pst("i0"), pst("i1"), pst("i2")]; P["pm"] = pst("m")
        pseq = kb.ps(es, "Bp_seq", [128, 512])
        bseq = Buf(True)
        P["pseq"] = [T(pseq[:, 0:128], b=bseq), T(pseq[:, 128:256], b=bseq), T(pseq[:, 256:384], b=bseq)]
        pp = [pst("p0"), pst("p1")]
        ppc = [0]

        def nextp():
            ppc[0] += 1
            return pp[ppc[0] % 2]
        ft = {}

        def F(name, dt=F32):
            if name not in ft:
                ft[name] = mk("f_" + name, [128, W], dt)
            return ft[name]
        SC = min(512, RW, DN)
        stage = mk("stage", [128, W // 128, SC], BF16)
        LW = mk("LW", [128, 1, W + 1]); LWs = mk("LWs", [128, 4, W])
        BETA = T(LWs[0:16, 0, :], b=LWs.b); GG = T(LWs[0:16, 1, :], b=LWs.b)
        PIN = [mk("PIN%d" % i, [128, W + 4]) for i in range(3)]

        def v3(t, Kd):
            return t[0:Kd, :].rearrange("p (a b) -> p a b", b=64)

        def dve_tt(out, a, b_, op, reads, writes, eng="dve"):
            S.op(eng, lambda e: e.tensor_tensor(out=out, in0=a, in1=b_, op=op), reads=reads, writes=writes)

        def load_shift(dst, row0, nrows, t0, lead):
            bsrc = [b_PT[j] for j in range(row0 // 128, (row0 + nrows - 1) // 128 + 1)]
            if t0 == 0:
                S.op("pool", lambda e: e.memset(dst[0:nrows, 0:lead], 0.0), writes=[dst.b])
                S.dma("sp", lambda e: e.dma_start(out=dst[0:nrows, lead:lead + W], in_=PT[row0:row0 + nrows, 0:W]), reads=bsrc, writes=[dst.b])
            else:
                S.dma("sp", lambda e: e.dma_start(out=dst[0:nrows, 0:lead + W], in_=PT[row0:row0 + nrows, t0 - lead:t0 + W]), reads=bsrc, writes=[dst.b])

        def chunk_common(u, Kd, Gsrc, Gsrc_reads, scale, r_, kk_, b_, kh_, v_, b0_=None):
            G, GP, EG, ENG, EGP, EGC = F("G"), F("GP"), F("A"), F("SQ"), F("RS"), F("SIG")
            S.op("dve", lambda e: e.tensor_tensor_scan(out=G[0:Kd, :], data0=rmask[0:Kd, :], data1=Gsrc, initial=0.0, op0=ALU.mult, op1=ALU.add),
                 reads=[rmask.b] + Gsrc_reads, writes=[G.b])
            dve_tt(GP[0:Kd, :], G[0:Kd, :], Gsrc, ALU.subtract, [G.b] + Gsrc_reads, [GP.b])
            S.op("act", lambda e: e.activation(out=EG[0:Kd, :], in_=G[0:Kd, :], func=AF.Exp, scale=scale), reads=[G.b], writes=[EG.b])
            if b0_ is None:
                S.op("act", lambda e: e.activation(out=ENG[0:Kd, :], in_=G[0:Kd, :], func=AF.Exp, scale=-scale), reads=[G.b], writes=[ENG.b])
            S.op("act", lambda e: e.activation(out=EGP[0:Kd, :], in_=GP[0:Kd, :], func=AF.Exp, scale=scale), reads=[GP.b], writes=[EGP.b])
            gend = v3(G, Kd)[:, :, 63:64]
            dve_tt(v3(EGC, Kd), gend.to_broadcast([Kd, NCH, 64]), v3(G, Kd), ALU.subtract, [G.b], [EGC.b])
            S.op("act", lambda e: e.activation(out=EGC[0:Kd, :], in_=EGC[0:Kd, :], func=AF.Exp, scale=scale), reads=[EGC.b], writes=[EGC.b])
            S.op("act", lambda e: e.activation(out=u["wC"][0:Kd, :], in_=gend.rearrange("p a b -> p (a b)"), func=AF.Exp, scale=scale),
                 reads=[G.b], writes=[u["wC"].b])
            QR, BK, BKe, VV = u["QR"], u["BK"], u["BKe"], u["VV"]
            dve_tt(QR[0:Kd, :, 0:64], v3(kk_, Kd), v3(EGP, Kd), ALU.mult, [kk_.b, EGP.b], [QR.b])
            dve_tt(QR[0:Kd, :, 64:128], v3(r_, Kd), v3(EG, Kd), ALU.mult, [r_.b, EG.b], [QR.b], eng="pool")
            if b0_ is None:
                dve_tt(BK[0:Kd, :, 0:64], v3(b_, Kd), v3(ENG, Kd), ALU.mult, [b_.b, ENG.b], [BK.b])
                dve_tt(BK[0:Kd, :, 64:128], v3(kh_, Kd), v3(ENG, Kd), ALU.mult, [kh_.b, ENG.b], [BK.b], eng="pool")
            else:
                QRg, NA = u["QRg"], u["NA"]
                S.op("pool", lambda e: e.tensor_copy(out=BK[0:Kd, :, 0:64], in_=v3(b0_, Kd)), reads=[b0_.b], writes=[BK.b])
                S.op("pool", lambda e: e.tensor_copy(out=BK[0:Kd, :, 64:128], in_=v3(kh_, Kd)), reads=[kh_.b], writes=[BK.b])
                S.op("pool", lambda e: e.tensor_copy(out=QRg[0:Kd, :, 0:64], in_=v3(kk_, Kd)), reads=[kk_.b], writes=[QRg.b])
                S.op("pool", lambda e: e.tensor_copy(out=QRg[0:Kd, :, 64:128], in_=v3(r_, Kd)), reads=[r_.b], writes=[QRg.b])
                S.op("dve", lambda e: e.tensor_copy(out=NA[0:1, :, 0:64], in_=v3(GP, 1)), reads=[GP.b], writes=[NA.b])
                S.op("dve", lambda e: e.tensor_copy(out=NA[0:1, :, 64:128], in_=v3(G, 1)), reads=[G.b], writes=[NA.b])
            dve_tt(BKe[0:Kd, :, 0:64], v3(b_, Kd), v3(EGC, Kd), ALU.mult, [b_.b, EGC.b], [BKe.b])
            dve_tt(BKe[0:Kd, :, 64:128], v3(kh_, Kd), v3(EGC, Kd), ALU.mult, [kh_.b, EGC.b], [BKe.b], eng="pool")
            S.op("pool", lambda e: e.tensor_copy(out=VV[0:Kd, :, 64:128], in_=v3(v_, Kd)), reads=[v_.b], writes=[VV.b])

        def l2_rstd(Kd, src, dst):
            SQ = F("SQ")
            S.op("act", lambda e: e.activation(out=SQ[0:Kd, :], in_=src[0:Kd, :], func=AF.Square), reads=[src.b], writes=[SQ.b])
            p = nextp()
            S.op("pe", lambda e: e.matmul(p[0:Kd, 0:W], lhsT=ones[0:Kd, 0:Kd], rhs=SQ[0:Kd, :], start=True, stop=True), reads=[SQ.b, bm], writes=[p.b])
            S.op("act", lambda e: e.activation(out=dst[0:Kd, :], in_=p[0:Kd, 0:W], func=AF.Ln, bias=c["EPS6"][0:Kd, :]), reads=[p.b, bcol], writes=[dst.b])
            S.op("act", lambda e: e.activation(out=dst[0:Kd, :], in_=dst[0:Kd, :], func=AF.Exp, scale=-0.5), reads=[dst.b], writes=[dst.b])

        def to_stage(Kd, res, col0):
            p = P["pg"]
            nb_ = W // 128
            for i in range(nb_):
                S.op("pe", lambda e: e.transpose(out=p[:, i * Kd:(i + 1) * Kd], in_=res[0:Kd, i * 128:(i + 1) * 128], identity=ident[0:Kd, 0:Kd]),
                     reads=[res.b, bi], writes=[p.b])
            S.op("act", lambda e: e.copy(out=stage[:, :, col0:col0 + Kd], in_=p[:, 0:nb_ * Kd].rearrange("p (a b) -> p a b", a=nb_)),
                 reads=[p.b], writes=[stage.b])

        def prep_rw_shared(t0):
            for i, nm in enumerate(("wd", "ad", "gd", "gd")):
                r0 = rows[nm] + (128 if i == 3 else 0)
                bsrc = [b_PT[r0 // 128]]
                if t0 == 0:
                    S.op("pool", lambda e: e.memset(LW[:, 0, 0:1], 0.0), writes=[LW.b])
                    S.dma("sp", lambda e: e.dma_start(out=LW[:, 0, 1:1 + W], in_=PT[r0:r0 + 128, 0:W]), reads=bsrc, writes=[LW.b])
                else:
                    S.dma("sp", lambda e: e.dma_start(out=LW[:, 0, :], in_=PT[r0:r0 + 128, t0 - 1:t0 + W]), reads=bsrc, writes=[LW.b])
                S.op("dve", lambda e: e.tensor_scalar(out=LWs[:, i, :], in0=LW[:, 0, 0:W], scalar1=lmu[:, i:i + 1], scalar2=None, op0=ALU.mult),
                     reads=[LW.b, lmu.b], writes=[LWs.b])
                S.op("dve", lambda e: e.scalar_tensor_tensor(out=LWs[:, i, :], in0=LW[:, 0, 1:1 + W], scalar=lmu[:, 4 + i:5 + i], in1=LWs[:, i, :],
                                                             op0=ALU.mult, op1=ALU.add), reads=[LW.b, lmu.b, LWs.b], writes=[LWs.b])
                if i != 1:
                    fn = AF.Tanh if i == 0 else AF.Sigmoid
                    S.op("act", lambda e: e.activation(out=LWs[:, i, :], in_=LWs[:, i, :], func=fn), reads=[LWs.b], writes=[LWs.b])

        def prep_dn_shared(t0):
            rb = rows["ba"]
            S.dma("sp", lambda e: e.dma_start(out=BETA[:], in_=PT[rb:rb + 16, t0:t0 + W]), reads=[b_PT[rb // 128]], writes=[BETA.b])
            S.dma("sp", lambda e: e.dma_start(out=GG[:], in_=PT[rb + 32:rb + 48, t0:t0 + W]), reads=[b_PT[rb // 128]], writes=[GG.b])
            S.op("act", lambda e: e.activation(out=BETA[:], in_=BETA[:], func=AF.Sigmoid), reads=[BETA.b], writes=[BETA.b])
            S.op("act", lambda e: e.activation(out=GG[:], in_=GG[:], func=AF.Exp, bias=dnh[:, 1:2]), reads=[GG.b, dnh.b], writes=[GG.b])
            S.op("act", lambda e: e.activation(out=GG[:], in_=GG[:], func=AF.Ln, bias=c["ONE"][0:16, :]), reads=[GG.b, bcol], writes=[GG.b])
            S.op("dve", lambda e: e.tensor_scalar(out=GG[:], in0=GG[:], scalar1=dnh[:, 2:3], scalar2=None, op0=ALU.mult), reads=[GG.b, dnh.b], writes=[GG.b])

        def prep_rw(u, h, t0):
            if h == 0:
                prep_rw_shared(t0)
            src = {}
            for i, nm in enumerate(("r", "k", "v")):
                load_shift(PIN[i], rows[nm] + h * 64, 64, t0, 1)
                d = F("x_" + nm)
                S.op("dve", lambda e: e.tensor_scalar(out=d[0:64, :], in0=PIN[i][0:64, 0:W], scalar1=rwp[:, i, h:h + 1], scalar2=None, op0=ALU.mult),
                     reads=[PIN[i].b, rwp.b], writes=[d.b])
                S.op("dve", lambda e: e.scalar_tensor_tensor(out=d[0:64, :], in0=PIN[i][0:64, 1:1 + W], scalar=rwp[:, 10 + i, h:h + 1], in1=d[0:64, :],
                                                             op0=ALU.mult, op1=ALU.add), reads=[PIN[i].b, rwp.b, d.b], writes=[d.b])
                src[nm] = d
            r_, k_, v_ = src["r"], src["k"], src["v"]
            hs = slice(h * 64, (h + 1) * 64)
            SIG, A_, GT = F("SIG"), F("A"), u["HO2"]
            p = nextp()
            S.op("pe", lambda e: e.matmul(p[0:64, 0:W], lhsT=w2[0:96, hs], rhs=LWs[0:96, 0, :], start=True, stop=True), reads=[w2.b, LWs.b], writes=[p.b])
            S.op("act", lambda e: e.activation(out=SIG[0:64, :], in_=p[0:64, 0:W], func=AF.Sigmoid, bias=rwp[:, 3, h:h + 1]), reads=[p.b, rwp.b], writes=[SIG.b])
            p = nextp()
            S.op("pe", lambda e: e.matmul(p[0:64, 0:W], lhsT=a2[0:96, hs], rhs=LWs[0:96, 1, :], start=True, stop=True), reads=[a2.b, LWs.b], writes=[p.b])
            S.op("act", lambda e: e.activation(out=A_[0:64, :], in_=p[0:64, 0:W], func=AF.Sigmoid, bias=rwp[:, 4, h:h + 1]), reads=[p.b, rwp.b], writes=[A_.b])
            p = nextp()
            for a in range(2):
                S.op("pe", lambda e: e.matmul(p[0:64, 0:W], lhsT=g2[:, a, hs], rhs=LWs[:, 2 + a, :], start=(a == 0), stop=(a == 1)), reads=[g2.b, LWs.b], writes=[p.b])
            S.op("act", lambda e: e.copy(out=GT[0:64, :], in_=p[0:64, 0:W]), reads=[p.b], writes=[GT.b])
            KX, RS, KK, T1, KH, B_ = F("KX"), F("RS"), F("KK"), F("T1"), F("KH"), F("B")
            S.op("dve", lambda e: e.tensor_scalar(out=KX[0:64, :], in0=k_[0:64, :], scalar1=rwp[:, 5, h:h + 1], scalar2=None, op0=ALU.mult),
                 reads=[k_.b, rwp.b], writes=[KX.b])
            l2_rstd(64, KX, RS)
            dve_tt(KK[0:64, :], KX[0:64, :], RS[0:64, :], ALU.mult, [KX.b, RS.b], [KK.b])
            S.op("dve", lambda e: e.tensor_scalar(out=T1[0:64, :], in0=A_[0:64, :], scalar1=rwp[:, 6, h:h + 1], scalar2=omka[:, h:h + 1], op0=ALU.mult, op1=ALU.add),
                 reads=[A_.b, rwp.b, omka.b], writes=[T1.b])
            dve_tt(KH[0:64, :], k_[0:64, :], T1[0:64, :], ALU.mult, [k_.b, T1.b], [KH.b], eng="pool")
            dve_tt(B_[0:64, :], KK[0:64, :], A_[0:64, :], ALU.mult, [KK.b, A_.b], [B_.b], eng="pool")
            RK, BON = F("KX"), u["HO1"]
            S.op("dve", lambda e: e.scalar_tensor_tensor(out=RK[0:64, :], in0=r_[0:64, :], scalar=rwp[:, 7, h:h + 1], in1=KH[0:64, :], op0=ALU.mult, op1=ALU.mult),
                 reads=[r_.b, rwp.b, KH.b], writes=[RK.b])
            p = nextp()
            S.op("pe", lambda e: e.matmul(p[0:64, 0:W], lhsT=ones[0:64, 0:64], rhs=RK[0:64, :], start=True, stop=True), reads=[RK.b, bm], writes=[p.b])
            dve_tt(BON[0:64, :], p[0:64, 0:W], v_[0:64, :], ALU.mult, [p.b, v_.b], [BON.b])
            chunk_common(u, 64, SIG[0:64, :], [SIG.b], -c0, r_, KK, B_, KH, v_)

        def prep_dn(u, h, t0):
            if h == 0:
                prep_dn_shared(t0)
            src = {}
            for i, nm in enumerate(("dq", "dk", "dv")):
                load_shift(PIN[i], rows[nm] + h * 128, 128, t0, 3)
                d = F("x_" + ("r", "k", "v")[i])
                S.op("dve", lambda e: e.tensor_scalar(out=d[:, :], in0=PIN[i][:, 0:W], scalar1=dnp[:, i, h:h + 1], scalar2=None, op0=ALU.mult),
                     reads=[PIN[i].b, dnp.b], writes=[d.b])
                for j in range(1, 4):
                    S.op("dve", lambda e: e.scalar_tensor_tensor(out=d[:, :], in0=PIN[i][:, j:j + W], scalar=dnp[:, 3 * j + i, h:h + 1], in1=d[:, :],
                                                                 op0=ALU.mult, op1=ALU.add), reads=[PIN[i].b, dnp.b, d.b], writes=[d.b])
                S.op("act", lambda e: e.activation(out=d[:, :], in_=d[:, :], func=AF.Silu), reads=[d.b], writes=[d.b])
                src[nm] = d
            q_, k_, v_ = src["dq"], src["dk"], src["dv"]
            RS, QN, KN, B_, EGB, VB = F("RS"), F("KX"), F("KK"), F("B"), F("T1"), F("KH")
            l2_rstd(128, q_, RS)
            S.op("dve", lambda e: e.scalar_tensor_tensor(out=QN[:, :], in0=q_[:, :], scalar=128.0 ** -0.5, in1=RS[:, :], op0=ALU.mult, op1=ALU.mult),
                 reads=[q_.b, RS.b], writes=[QN.b])
            l2_rstd(128, k_, RS)
            dve_tt(KN[:, :], k_[:, :], RS[:, :], ALU.mult, [k_.b, RS.b], [KN.b])
            pbeta = nextp()
            S.op("pe", lambda e: e.matmul(pbeta[:, 0:W], lhsT=sel[:, h, :], rhs=BETA[:], start=True, stop=True), reads=[sel.b, BETA.b], writes=[pbeta.b])
            B0 = F("x_k")
            dve_tt(B0[:, :], KN[:, :], pbeta[:, 0:W], ALU.mult, [KN.b, pbeta.b], [B0.b])
            dve_tt(VB[:, :], v_[:, :], pbeta[:, 0:W], ALU.mult, [v_.b, pbeta.b], [VB.b])
            pg_ = nextp()
            S.op("pe", lambda e: e.matmul(pg_[:, 0:W], lhsT=sel[:, h, :], rhs=GG[:], start=True, stop=True), reads=[sel.b, GG.b], writes=[pg_.b])
            GB = F("SIG")
            S.op("act", lambda e: e.copy(out=GB[:, :], in_=pg_[:, 0:W]), reads=[pg_.b], writes=[GB.b])
            S.op("act", lambda e: e.activation(out=EGB[:, :], in_=pg_[:, 0:W], func=AF.Exp), reads=[pg_.b], writes=[EGB.b])
            dve_tt(B_[:, :], B0[:, :], EGB[:, :], ALU.mult, [B0.b, EGB.b], [B_.b])
            ZT = u["HO1"]
            rz = rows["z"] + h * 128
            S.dma("sp", lambda e: e.dma_start(out=ZT[:, :], in_=PT[rz:rz + 128, t0:t0 + W]), reads=[b_PT[rz // 128]], writes=[ZT.b])
            S.op("act", lambda e: e.activation(out=ZT[:, :], in_=ZT[:, :], func=AF.Silu), reads=[ZT.b], writes=[ZT.b])
            chunk_common(u, 128, GB[:, :], [GB.b], 1.0, QN, KN, B_, KN, VB, b0_=B0)

        e_X, e_DD = mk("e_X", [128, W]), mk("e_DD", [128, W])

        def flush_stage(h, Kd, base, t0):
            if (h * Kd + Kd) % SC == 0:
                cb = base + (h * Kd + Kd) - SC
                for i in range(W // 128):
                    S.dma("pool", lambda e: e.dma_start(out=OMIX[t0 + i * 128:t0 + (i + 1) * 128, cb:cb + SC], in_=stage[:, i, :]), reads=[stage.b], writes=[b_OMIX])

        def run_rw(u, h, t0):
            uu = dict(u); uu["H"] = Hrw[h]; uu["Hb"] = Hrwb[h]; uu["OT"] = OT
            uu.pop("QRg", None)
            dplr_segment(kb, c, uu, 64, NCH, P)
            BON, GT = u["HO1"], u["HO2"]
            SQ, MEAN, DD, RES = e_X, e_X, e_DD, e_X
            p = P["pm"]
            S.op("pe", lambda e: e.matmul(p[0:64, 0:W], lhsT=ones[0:64, 0:64], rhs=OT[0:64, :], start=True, stop=True), reads=[OT.b, bm], writes=[p.b])
            S.op("act", lambda e: e.mul(out=MEAN[0:64, :], in_=p[0:64, 0:W], mul=1.0 / 64), reads=[p.b], writes=[MEAN.b])
            dve_tt(DD[0:64, :], OT[0:64, :], MEAN[0:64, :], ALU.subtract, [OT.b, MEAN.b], [DD.b])
            S.op("act", lambda e: e.activation(out=SQ[0:64, :], in_=DD[0:64, :], func=AF.Square), reads=[DD.b], writes=[SQ.b])
            S.op("pe", lambda e: e.matmul(p[0:64, 0:W], lhsT=ones[0:64, 0:64], rhs=SQ[0:64, :], start=True, stop=True), reads=[SQ.b, bm], writes=[p.b])
            S.op("act", lambda e: e.activation(out=RES[0:64, :], in_=p[0:64, 0:W], func=AF.Ln, scale=1.0 / 64, bias=c["GNEPS"][0:64, :]), reads=[p.b, bcol], writes=[RES.b])
            S.op("act", lambda e: e.activation(out=RES[0:64, :], in_=RES[0:64, :], func=AF.Exp, scale=-0.5), reads=[RES.b], writes=[RES.b])
            dve_tt(DD[0:64, :], DD[0:64, :], RES[0:64, :], ALU.mult, [DD.b, RES.b], [DD.b])
            S.op("dve", lambda e: e.tensor_scalar(out=DD[0:64, :], in0=DD[0:64, :], scalar1=rwp[:, 8, h:h + 1], scalar2=rwp[:, 9, h:h + 1], op0=ALU.mult, op1=ALU.add),
                 reads=[DD.b, rwp.b], writes=[DD.b])
            dve_tt(DD[0:64, :], DD[0:64, :], BON[0:64, :], ALU.add, [DD.b, BON.b], [DD.b], eng="pool")
            dve_tt(RES[0:64, :], DD[0:64, :], GT[0:64, :], ALU.mult, [DD.b, GT.b], [RES.b])
            to_stage(64, RES, (h * 64) % SC)
            flush_stage(h, 64, 0, t0)

        def run_dn(u, h, t0):
            uu = dict(u); uu["H"] = Hdn[h]; uu["Hb"] = Hdnb[h]; uu["OT"] = OT; uu["E"] = P["E"]; uu["nones"] = nones
            dplr_segment(kb, c, uu, 128, NCH, P)
            ZT = u["HO1"]
            SQ, l2n, RES = e_X, e_X, e_DD
            p = P["pm"]
            S.op("act", lambda e: e.activation(out=SQ[:, :], in_=OT[:, :], func=AF.Square), reads=[OT.b], writes=[SQ.b])
            S.op("pe", lambda e: e.matmul(p[:, 0:W], lhsT=ones[:, :], rhs=SQ[:, :], start=True, stop=True), reads=[SQ.b, bm], writes=[p.b])
            S.op("act", lambda e: e.activation(out=l2n[:, :], in_=p[:, 0:W], func=AF.Ln, scale=1.0 / 128, bias=c["EPS6"]), reads=[p.b, bcol], writes=[l2n.b])
            S.op("act", lambda e: e.activation(out=l2n[:, :], in_=l2n[:, :], func=AF.Exp, scale=-0.5), reads=[l2n.b], writes=[l2n.b])
            S.op("dve", lambda e: e.scalar_tensor_tensor(out=RES[:, :], in0=OT[:, :], scalar=dnw[:, 0:1], in1=l2n[:, :], op0=ALU.mult, op1=ALU.mult),
                 reads=[OT.b, dnw.b, l2n.b], writes=[RES.b])
            dve_tt(RES[:, :], RES[:, :], ZT[:, :], ALU.mult, [RES.b, ZT.b], [RES.b])
            to_stage(128, RES, (h * 128) % SC)
            flush_stage(h, 128, RW, t0)

        units = []
        for seg in range(cfg.S // W):
            units += [("rw", h, seg * W) for h in range(RWH)] + [("dn", h, seg * W) for h in range(DNH)]

        def mkprep(i):
            kind, h, t0 = units[i]
            return (lambda: prep_rw(usets[i % 2], h, t0)) if kind == "rw" else (lambda: prep_dn(usets[i % 2], h, t0))

        def mkrun(i):
            kind, h, t0 = units[i]
            return (lambda: run_rw(usets[i % 2], h, t0)) if kind == "rw" else (lambda: run_dn(usets[i % 2], h, t0))
        mkprep(0)()
        for i in range(len(units)):
            nxt = mkprep(i + 1) if i + 1 < len(units) else None
            run_pair(S, nxt, mkrun(i), quota=BQ)
    S.barrier()


def build_hT(kb, c, S, src, row0, ntiles, D, KD, xt, hb, ss, gt, hT, psT, pcnt, nrm=True):
    for ti in range(ntiles):
        t0 = row0 + ti * 128
        S.dma("sp", lambda e: e.dma_start(out=xt[:], in_=src[t0:t0 + 128, :]), writes=[xt.b])
        S.op("act", lambda e: e.activation(out=hb[:], in_=xt[:], func=AF.Square, accum_out=ss[:]), reads=[xt.b], writes=[hb.b, ss.b])
        rmsnorm_rstd(kb, c, ss[:], ss.b, D)
        S.op("dve", lambda e: e.scalar_tensor_tensor(out=hb[:], in0=xt[:], scalar=ss[:, 0:1], in1=gt[:], op0=ALU.mult, op1=ALU.mult),
             reads=[xt.b, ss.b, gt.b], writes=[hb.b])
        transpose_bf(kb, c, S, hb, KD, hT, ti, psT, pcnt)


def transpose_bf(kb, c, S, src, KD, dstT, ti, psT, pcnt):
    for k0 in range(0, KD, 8):
        kn = min(8, KD - k0)
        p = psT[pcnt[0] % len(psT)]; pcnt[0] += 1
        for k in range(kn):
            S.op("pe", lambda e: e.transpose(out=p[:, k * 128:(k + 1) * 128], in_=src[:, (k0 + k) * 128:(k0 + k + 1) * 128], identity=c["identb"][:]),
                 reads=[src.b, c["b_ident"]], writes=[p.b])
        sv = p[:, 0:kn * 128].rearrange("p (a b) -> p a b", a=kn)
        dv = dstT[:, k0:k0 + kn, ti * 128:(ti + 1) * 128]
        if pcnt[0] % 2:
            S.op("act", lambda e: e.copy(out=dv, in_=sv), reads=[p.b], writes=[dstT.b])
        else:
            S.op("dve", lambda e: e.tensor_copy(out=dv, in_=sv), reads=[p.b], writes=[dstT.b])


def phase_A1(kb, c, xo, g1d, wG, gbd, GATES, b_G):
    cfg, S = kb.cfg, kb.S
    D, KD, TGO, KP = cfg.D, cfg.KD, cfg.TGO, cfg.KP
    NT = TGO // 128
    NPG = KD // KP
    with ExitStack() as es:
        mk = lambda name, shape, dt=F32: T(kb.sb(es, "G_" + name, shape, dt))
        hT = mk("hT", [128, KD, TGO], BF16); xt = mk("xt", [128, D]); hb = mk("hb", [128, D], BF16); ss = mk("ss", [128, 1]); g1 = mk("g1", [128, D])
        slf = [mk("slf%d" % i, [128, KP, 512]) for i in range(2)]; slb = [mk("slb%d" % i, [128, KP, 512], BF16) for i in range(2)]
        gb = [mk("gb%d" % i, [128, 512]) for i in range(2)]
        stg = [mk("stg%d" % i, [128, 512]) for i in range(2)]
        psT = [T(kb.ps(es, "G_psT%d" % i, [128, 1024], BF16), excl=True) for i in range(2)]
        psG = [T(kb.ps(es, "G_ps%d" % i, [128, 512]), excl=True) for i in range(NT)]
        S.dma("sp", lambda e: e.dma_start(out=g1[:], in_=g1d.partition_broadcast(128)), writes=[g1.b])
        pcnt = [0]
        it = 0
        for og in range(cfg.OWN // TGO):
            build_hT(kb, c, S, xo, og * TGO, NT, D, KD, xt, hb, ss, g1, hT, psT, pcnt)
            for gc in range(2 * D // 512):
                gbt = gb[gc % 2]
                S.dma("sp", lambda e: e.dma_start(out=gbt[:], in_=gbd[0:1, gc * 512:(gc + 1) * 512].partition_broadcast(128)), writes=[gbt.b])
                for q in range(NPG):
                    sl = it % 2; it += 1
                    S.dma("sp", lambda e: e.dma_start(out=slf[sl][:], in_=wG[gc, q]), writes=[slf[sl].b])
                    if it % 2:
                        S.op("act", lambda e: e.copy(out=slb[sl][:], in_=slf[sl][:]), reads=[slf[sl].b], writes=[slb[sl].b])
                    else:
                        S.op("dve", lambda e: e.tensor_copy(out=slb[sl][:], in_=slf[sl][:]), reads=[slf[sl].b], writes=[slb[sl].b])
                    for t in range(NT):
                        for k in range(KP):
                            kk = q * KP + k
                            S.op("pe", lambda e: e.matmul(psG[t][:, :], lhsT=hT[:, kk, t * 128:(t + 1) * 128], rhs=slb[sl][:, k, :],
                                                          start=(kk == 0), stop=(kk == KD - 1)), reads=[hT.b, slb[sl].b], writes=[psG[t].b])
                for t in range(NT):
                    st = stg[t % 2]
                    S.op("dve", lambda e: e.tensor_tensor(out=st[:], in0=psG[t][:, :], in1=gbt[:], op=ALU.add), reads=[psG[t].b, gbt.b], writes=[st.b])
                    S.op("act", lambda e: e.activation(out=st[:], in_=st[:], func=AF.Sigmoid), reads=[st.b], writes=[st.b])
                    r0 = og * TGO + t * 128
                    S.dma("pool", lambda e: e.dma_start(out=GATES[r0:r0 + 128, gc * 512:(gc + 1) * 512], in_=st[:]), reads=[st.b], writes=[b_G])
    S.barrier()


def phase_C1(kb, c, xo, own_idx, OMIX, b_OMIX, GATES, b_G, wBR, wO, XMID, b_XMID):
    cfg, S = kb.cfg, kb.S
    D, KD, KB, TGO, BW = cfg.D, cfg.KD, cfg.KB, cfg.TGO, cfg.BW
    NT = TGO // 128
    KBr = cfg.RW // 128
    KH = max(KD // 2, 1)
    with ExitStack() as es:
        mk = lambda name, shape, dt=F32: T(kb.sb(es, "C_" + name, shape, dt))
        KM = max(KD, KB)
        oT = mk("oT", [128, KM, TGO], BF16)
        mg = mk("mg", [128, NT, D], BF16)
        og_ = [mk("og%d" % i, [128, BW], BF16) for i in range(2)]
        idx = mk("idx", [128, cfg.OWN // 128], I32)
        SLW = min(8, KD, KBr)
        slf = [mk("slf%d" % i, [128, SLW, 512]) for i in range(2)]

        def pieces(k0, k1):
            return [(a, min(a + SLW, k1)) for a in range(k0, k1, SLW)]
        slb = [mk("slb%d" % i, [128, KM, 512], BF16) for i in range(2)]
        gt = [mk("gt%d" % i, [128, 2, 512]) for i in range(2)]
        tm = [mk("tm%d" % i, [128, 512]) for i in range(2)]
        xs = [mk("xs%d" % i, [128, 512]) for i in range(2)]
        psT = [T(kb.ps(es, "C_psT%d" % i, [128, 1024], BF16), excl=True) for i in range(2)]
        psY = [T(kb.ps(es, "C_psY%d" % i, [128, 512]), excl=True) for i in range(4)]
        S.dma("sp", lambda e: e.dma_start(out=idx[:], in_=own_idx), writes=[idx.b])
        pcnt = [0]
        it = 0
        yc = 0
        for g in range(cfg.OWN // TGO):
            for t in range(NT):
                o = og_[t % 2]
                col = g * NT + t
                S.dma("pool", lambda e: e.indirect_dma_start(out=o[:], out_offset=None, in_=OMIX,
                                                             in_offset=bass.IndirectOffsetOnAxis(ap=idx[:, col:col + 1], axis=0)),
                      reads=[idx.b, b_OMIX], writes=[o.b])
                transpose_bf(kb, c, S, o, KB, oT, t, psT, pcnt)
            for cc in range(D // 512):
                sb_ = slb[cc % 2]
                for br, (k0, k1) in enumerate(pieces(0, KBr) + pieces(KBr, KB)):
                    sl = it % 2; it += 1
                    S.dma("sp", lambda e: e.dma_start(out=slf[sl][:, 0:k1 - k0, :], in_=wBR[cc, :, k0:k1, :]), writes=[slf[sl].b])
                    if br % 2 == 0:
                        S.op("act", lambda e: e.copy(out=sb_[:, k0:k1, :], in_=slf[sl][:, 0:k1 - k0, :]), reads=[slf[sl].b], writes=[sb_.b])
                    else:
                        S.op("dve", lambda e: e.tensor_copy(out=sb_[:, k0:k1, :], in_=slf[sl][:, 0:k1 - k0, :]), reads=[slf[sl].b], writes=[sb_.b])
                for t in range(NT):
                    r0 = g * TGO + t * 128
                    gtt = gt[t % 2]
                    S.dma("sp", lambda e: e.dma_start(out=gtt[:, 0, :], in_=GATES[r0:r0 + 128, cc * 512:(cc + 1) * 512]), reads=[b_G], writes=[gtt.b])
                    S.dma("sp", lambda e: e.dma_start(out=gtt[:, 1, :], in_=GATES[r0:r0 + 128, D + cc * 512:D + (cc + 1) * 512]), reads=[b_G], writes=[gtt.b])
                    pr = psY[yc % 4]; pd = psY[(yc + 1) % 4]; yc += 2
                    for k in range(0, KBr):
                        S.op("pe", lambda e: e.matmul(pr[:, :], lhsT=oT[:, k, t * 128:(t + 1) * 128], rhs=sb_[:, k, :], start=(k == 0), stop=(k == KBr - 1)),
                             reads=[oT.b, sb_.b], writes=[pr.b])
                    for k in range(KBr, KB):
                        S.op("pe", lambda e: e.matmul(pd[:, :], lhsT=oT[:, k, t * 128:(t + 1) * 128], rhs=sb_[:, k, :], start=(k == KBr), stop=(k == KB - 1)),
                             reads=[oT.b, sb_.b], writes=[pd.b])
                    t1 = tm[t % 2]
                    S.op("dve", lambda e: e.tensor_tensor(out=t1[:], in0=pr[:, :], in1=gtt[:, 0, :], op=ALU.mult), reads=[pr.b, gtt.b], writes=[t1.b])
                    S.op("dve", lambda e: e.tensor_tensor(out=gtt[:, 1, :], in0=pd[:, :], in1=gtt[:, 1, :], op=ALU.mult), reads=[pd.b, gtt.b], writes=[gtt.b])
                    S.op("dve", lambda e: e.tensor_tensor(out=mg[:, t, cc * 512:(cc + 1) * 512], in0=t1[:], in1=gtt[:, 1, :], op=ALU.add),
                         reads=[t1.b, gtt.b], writes=[mg.b])
            for t in range(NT):
                mt = T(mg[:, t, :], b=mg.b)
                transpose_bf(kb, c, S, mt, KD, oT, t, psT, pcnt)
            for cc in range(D // 512):
                sb_ = slb[cc % 2]
                for hf, (k0, k1) in enumerate(pieces(0, KD)):
                    sl = it % 2; it += 1
                    S.dma("sp", lambda e: e.dma_start(out=slf[sl][:, 0:k1 - k0, :], in_=wO[cc, :, k0:k1, :]), writes=[slf[sl].b])
                    if hf % 2 == 0:
                        S.op("act", lambda e: e.copy(out=sb_[:, k0:k1, :], in_=slf[sl][:, 0:k1 - k0, :]), reads=[slf[sl].b], writes=[sb_.b])
                    else:
                        S.op("dve", lambda e: e.tensor_copy(out=sb_[:, k0:k1, :], in_=slf[sl][:, 0:k1 - k0, :]), reads=[slf[sl].b], writes=[sb_.b])
                for t in range(NT):
                    r0 = g * TGO + t * 128
                    x_ = xs[t % 2]
                    S.dma("sp", lambda e: e.dma_start(out=x_[:], in_=xo[r0:r0 + 128, cc * 512:(cc + 1) * 512]), writes=[x_.b])
                    pz = psY[yc % 4]; yc += 1
                    for k in range(KD):
                        S.op("pe", lambda e: e.matmul(pz[:, :], lhsT=oT[:, k, t * 128:(t + 1) * 128], rhs=sb_[:, k, :], start=(k == 0), stop=(k == KD - 1)),
                             reads=[oT.b, sb_.b], writes=[pz.b])
                    S.op("dve", lambda e: e.tensor_tensor(out=x_[:], in0=pz[:, :], in1=x_[:], op=ALU.add), reads=[pz.b, x_.b], writes=[x_.b])
                    S.dma("pool", lambda e: e.dma_start(out=XMID[r0:r0 + 128, cc * 512:(cc + 1) * 512], in_=x_[:]), reads=[x_.b], writes=[b_XMID])
    S.barrier()


def phase_C2(kb, c, XMID, b_XMID, g2d, wRd, bRd, X2, b_X2, R):
    cfg, S = kb.cfg, kb.S
    D, KD, NG, NE = cfg.D, cfg.KD, cfg.NG, cfg.NE
    NR = NG + NE
    NTO = cfg.OWN // 128
    with ExitStack() as es:
        mk = lambda name, shape, dt=F32: T(kb.sb(es, "R_" + name, shape, dt))
        xm = mk("xm", [128, D]); x2 = mk("x2", [128, D]); x2b = mk("x2b", [128, D], BF16); ss = mk("ss", [128, 1]); g2 = mk("g2", [128, D])
        x2T = mk("x2T", [128, KD, 128]); wR = mk("wR", [128, KD, NR]); bR = mk("bR", [128, NR])
        zt = mk("zt", [128, D], BF16)
        LG = mk("LG", [128, NR]); sm = mk("sm", [128, 16]); t3 = mk("t3", [128, NG, 8]); og = mk("og", [128, NG]); se = mk("se", [128, 8]); ee = mk("ee", [128, 8])
        m8 = mk("m8", [128, 8]); i8 = mk("i8", [128, 8], U32); i8f = mk("i8f", [128, 2]); tg = mk("tg", [128, NG]); oh = mk("oh", [128, NE])
        iog = mk("iog", [128, NG]); ioe = R["ioe"]
        psT = [T(kb.ps(es, "R_psT%d" % i, [128, 512]), excl=True) for i in range(2)]
        psL = T(kb.ps(es, "R_psL", [128, 512]), excl=True)
        S.dma("sp", lambda e: e.dma_start(out=g2[:], in_=g2d.partition_broadcast(128)), writes=[g2.b])
        S.dma("sp", lambda e: e.dma_start(out=wR[:], in_=wRd), writes=[wR.b])
        S.dma("sp", lambda e: e.dma_start(out=bR[:], in_=bRd.partition_broadcast(128)), writes=[bR.b])
        S.op("pool", lambda e: e.iota(iog[:], pattern=[[1, NG]], base=0, channel_multiplier=0, allow_small_or_imprecise_dtypes=True), writes=[iog.b])
        S.op("pool", lambda e: e.memset(zt[:], 0.0), writes=[zt.b])
        S.dma("pool", lambda e: e.dma_start(out=X2[cfg.OWN:cfg.OWN + 128, :], in_=zt[:]), reads=[zt.b], writes=[b_X2])
        M, RT = R["M"], R["RT"]
        pc = 0
        for i in range(NTO):
            r0 = i * 128
            S.dma("sp", lambda e: e.dma_start(out=xm[:], in_=XMID[r0:r0 + 128, :]), reads=[b_XMID], writes=[xm.b])
            S.op("act", lambda e: e.activation(out=x2[:], in_=xm[:], func=AF.Square, accum_out=ss[:]), reads=[xm.b], writes=[x2.b, ss.b])
            rmsnorm_rstd(kb, c, ss[:], ss.b, D)
            S.op("dve", lambda e: e.scalar_tensor_tensor(out=x2[:], in0=xm[:], scalar=ss[:, 0:1], in1=g2[:], op0=ALU.mult, op1=ALU.mult),
                 reads=[xm.b, ss.b, g2.b], writes=[x2.b])
            S.op("pool", lambda e: e.tensor_copy(out=x2b[:], in_=x2[:]), reads=[x2.b], writes=[x2b.b])
            S.dma("pool", lambda e: e.dma_start(out=X2[r0:r0 + 128, :], in_=x2b[:]), reads=[x2b.b], writes=[b_X2])
            for k0 in range(0, KD, 4):
                kn = min(4, KD - k0)
                p = psT[pc % 2]; pc += 1
                for k in range(kn):
                    S.op("pe", lambda e: e.transpose(out=p[:, k * 128:(k + 1) * 128], in_=x2[:, (k0 + k) * 128:(k0 + k + 1) * 128], identity=c["ident"][:]),
                         reads=[x2.b, c["b_ident"]], writes=[p.b])
                S.op("act", lambda e: e.copy(out=x2T[:, k0:k0 + kn, :], in_=p[:, 0:kn * 128].rearrange("p (a b) -> p a b", a=kn)), reads=[p.b], writes=[x2T.b])
            for k in range(KD):
                S.op("pe", lambda e: e.matmul(psL[:, 0:NR], lhsT=x2T[:, k, :], rhs=wR[:, k, :], start=(k == 0), stop=(k == KD - 1)),
                     reads=[x2T.b, wR.b], writes=[psL.b])
            D_ = lambda fn, reads, writes: S.op("dve", fn, reads=reads, writes=writes)
            D_(lambda e: e.tensor_tensor(out=LG[:], in0=psL[:, 0:NR], in1=bR[:], op=ALU.add), [psL.b, bR.b], [LG.b])
            D_(lambda e: e.tensor_reduce(out=sm[:, 0:1], in_=LG[:, 0:NG], axis=AX.X, op=ALU.max), [LG.b], [sm.b])
            D_(lambda e: e.tensor_scalar(out=og[:], in0=LG[:, 0:NG], scalar1=sm[:, 0:1], scalar2=None, op0=ALU.is_equal), [LG.b, sm.b], [og.b])
            D_(lambda e: e.tensor_scalar(out=sm[:, 1:2], in0=sm[:, 0:1], scalar1=-1.0, scalar2=None, op0=ALU.mult), [sm.b], [sm.b])
            S.op("act", lambda e: e.activation(out=tg[:], in_=LG[:, 0:NG], func=AF.Exp, bias=sm[:, 1:2], accum_out=sm[:, 2:3]), reads=[LG.b, sm.b], writes=[tg.b, sm.b])
            D_(lambda e: e.reciprocal(out=sm[:, 3:4], in_=sm[:, 2:3]), [sm.b], [sm.b])
            D_(lambda e: e.tensor_tensor(out=tg[:], in0=og[:], in1=iog[:], op=ALU.mult), [og.b, iog.b, tg.b], [tg.b])
            D_(lambda e: e.tensor_reduce(out=sm[:, 4:5], in_=tg[:], axis=AX.X, op=ALU.add), [tg.b], [sm.b])
            D_(lambda e: e.tensor_tensor(out=t3[:], in0=LG[:, NG:NR].rearrange("p (g e) -> p g e", e=8), in1=og[:].unsqueeze(2).to_broadcast([128, NG, 8]), op=ALU.mult),
               [LG.b, og.b], [t3.b])
            D_(lambda e: e.tensor_reduce(out=se[:], in_=t3[:].rearrange("p g e -> p e g"), axis=AX.X, op=ALU.add), [t3.b], [se.b])
            D_(lambda e: e.tensor_reduce(out=sm[:, 5:6], in_=se[:], axis=AX.X, op=ALU.max), [se.b], [sm.b])
            D_(lambda e: e.tensor_scalar(out=sm[:, 5:6], in0=sm[:, 5:6], scalar1=-1.0, scalar2=None, op0=ALU.mult), [sm.b], [sm.b])
            S.op("act", lambda e: e.activation(out=ee[:], in_=se[:], func=AF.Exp, bias=sm[:, 5:6]), reads=[se.b, sm.b], writes=[ee.b])
            D_(lambda e: e.max(out=m8[:], in_=ee[:]), [ee.b], [m8.b])
            D_(lambda e: e.max_index(out=i8[:], in_max=m8[:], in_values=ee[:]), [m8.b, ee.b], [i8.b])
            D_(lambda e: e.tensor_copy(out=i8f[:], in_=i8[:, 0:2]), [i8.b], [i8f.b])
            D_(lambda e: e.tensor_tensor(out=sm[:, 6:7], in0=m8[:, 0:1], in1=m8[:, 1:2], op=ALU.add), [m8.b], [sm.b])
            D_(lambda e: e.reciprocal(out=sm[:, 6:7], in_=sm[:, 6:7]), [sm.b], [sm.b])
            D_(lambda e: e.tensor_tensor(out=sm[:, 6:7], in0=sm[:, 6:7], in1=sm[:, 3:4], op=ALU.mult), [sm.b], [sm.b])
            D_(lambda e: e.tensor_scalar(out=RT[:, i, 2:4], in0=m8[:, 0:2], scalar1=sm[:, 6:7], scalar2=None, op0=ALU.mult), [m8.b, sm.b], [RT.b])
            D_(lambda e: e.scalar_tensor_tensor(out=RT[:, i, 0:2], in0=sm[:, 4:5].to_broadcast([128, 2]), scalar=8.0, in1=i8f[:], op0=ALU.mult, op1=ALU.add),
               [sm.b, i8f.b], [RT.b])
            D_(lambda e: e.tensor_scalar(out=M[:, i, :], in0=ioe[:], scalar1=RT[:, i, 0:1], scalar2=None, op0=ALU.is_equal), [ioe.b, RT.b], [M.b])
            D_(lambda e: e.tensor_scalar(out=oh[:], in0=ioe[:], scalar1=RT[:, i, 1:2], scalar2=None, op0=ALU.is_equal), [ioe.b, RT.b], [oh.b])
            D_(lambda e: e.tensor_tensor(out=M[:, i, :], in0=M[:, i, :], in1=oh[:], op=ALU.add), [M.b, oh.b], [M.b])
    S.barrier()


def phase_D0(kb, c, R, SLOT, b_SLOT):
    cfg, S = kb.cfg, kb.S
    NE, NBLK = cfg.NE, cfg.NBLK
    NTO = cfg.OWN // 128
    NQ1 = cfg.KD // min(4, cfg.KD)
    FE = cfg.FE
    with ExitStack() as es:
        mk = lambda name, shape, dt=F32: T(kb.sb(es, "Z_" + name, shape, dt))
        M, RT, ioe = R["M"], R["RT"], R["ioe"]
        triu = mk("triu", [128, 128]); ones = mk("ones", [128, 128])
        msum = mk("msum", [128, NE]); mcum = mk("mcum", [128, NE]); cnt = mk("cnt", [128, NE]); pcn = mk("pcn", [128, NE]); pend = mk("pend", [128, NE])
        poff = mk("poff", [128, NE]); ci = mk("ci", [128, NE], I32)
        j128 = mk("j128", [128, NBLK]); cmp = mk("cmp", [128, NBLK, NE]); be = mk("be", [128, NBLK])
        qp1 = mk("qp1", [128, NQ1]); qp2 = mk("qp2", [128, FE * 2]); f1 = mk("f1", [128, NBLK, NQ1]); f2 = mk("f2", [128, NBLK, FE * 2])
        dest = mk("dest", [128, NE]); oh = mk("oh", [128, NE]); dd = mk("dd", [128, NTO, 2])
        toki = mk("toki", [128, NTO, 16], I32); sinit = mk("sinit", [128, NBLK, 16], I32)
        ps = [T(kb.ps(es, "Z_ps%d" % i, [128, 512]), excl=True) for i in range(2)]
        P_ = lambda fn, reads, writes: S.op("pool", fn, reads=reads, writes=writes)
        D_ = lambda fn, reads, writes: S.op("dve", fn, reads=reads, writes=writes)
        P_(lambda e: e.memset(triu[:], 1.0), [], [triu.b])
        P_(lambda e: e.affine_select(out=triu[:], in_=triu[:], pattern=[[1, 128]], compare_op=ALU.is_gt, fill=0.0, base=0, channel_multiplier=-1), [triu.b], [triu.b])
        P_(lambda e: e.memset(ones[:], 1.0), [], [ones.b])
        P_(lambda e: e.iota(j128[:], pattern=[[128, NBLK]], base=0, channel_multiplier=0, allow_small_or_imprecise_dtypes=True), [], [j128.b])
        P_(lambda e: e.iota(qp1[:], pattern=[[128, NQ1]], base=0, channel_multiplier=1, allow_small_or_imprecise_dtypes=True), [], [qp1.b])
        P_(lambda e: e.iota(qp2[:], pattern=[[128, FE * 2]], base=0, channel_multiplier=1, allow_small_or_imprecise_dtypes=True), [], [qp2.b])
        P_(lambda e: e.iota(toki[:], pattern=[[128, NTO], [0, 16]], base=0, channel_multiplier=1), [], [toki.b])
        P_(lambda e: e.iota(sinit[:], pattern=[[0, NBLK], [0, 16]], base=cfg.OWN, channel_multiplier=0), [], [sinit.b])
        S.dma("sp", lambda e: e.dma_start(out=SLOT.rearrange("(j p) c -> p j c", p=128), in_=sinit[:]), reads=[sinit.b], writes=[b_SLOT])
        D_(lambda e: e.tensor_reduce(out=msum[:], in_=M[:].rearrange("p i e -> p e i"), axis=AX.X, op=ALU.add), [M.b], [msum.b])
        S.op("pe", lambda e: e.matmul(ps[0][:, 0:NE], lhsT=ones[:], rhs=msum[:], start=True, stop=True), reads=[ones.b, msum.b], writes=[ps[0].b])
        D_(lambda e: e.tensor_scalar(out=ci[:], in0=ps[0][:, 0:NE], scalar1=127.0, scalar2=None, op0=ALU.add), [ps[0].b], [ci.b])
        D_(lambda e: e.tensor_single_scalar(out=ci[:], in_=ci[:], scalar=7, op=ALU.arith_shift_right), [ci.b], [ci.b])
        D_(lambda e: e.tensor_single_scalar(out=ci[:], in_=ci[:], scalar=7, op=ALU.logical_shift_left), [ci.b], [ci.b])
        D_(lambda e: e.tensor_copy(out=pcn[:], in_=ci[:]), [ci.b], [pcn.b])
        D_(lambda e: e.tensor_tensor_scan(out=pend[:], data0=ones[:, 0:NE], data1=pcn[:], initial=0.0, op0=ALU.mult, op1=ALU.add), [ones.b, pcn.b], [pend.b])
        D_(lambda e: e.tensor_tensor(out=poff[:], in0=pend[:], in1=pcn[:], op=ALU.subtract), [pend.b, pcn.b], [poff.b])
        D_(lambda e: e.tensor_tensor(out=cmp[:], in0=pend[:].unsqueeze(1).to_broadcast([128, NBLK, NE]), in1=j128[:].unsqueeze(2).to_broadcast([128, NBLK, NE]), op=ALU.is_le),
           [pend.b, j128.b], [cmp.b])
        D_(lambda e: e.tensor_reduce(out=be[:], in_=cmp[:], axis=AX.X, op=ALU.add), [cmp.b], [be.b])
        big = mk("big", [128, NBLK])
        D_(lambda e: e.tensor_scalar(out=big[:], in0=be[:], scalar1=float(NE) - 0.5, scalar2=1.0e7, op0=ALU.is_ge, op1=ALU.mult), [be.b], [big.b])
        D_(lambda e: e.tensor_scalar(out=be[:], in0=be[:], scalar1=float(NE - 1), scalar2=None, op0=ALU.min), [be.b], [be.b])
        D_(lambda e: e.scalar_tensor_tensor(out=f1[:], in0=be[:].unsqueeze(2).to_broadcast([128, NBLK, NQ1]), scalar=float(NQ1 * 128),
                                            in1=qp1[:].unsqueeze(1).to_broadcast([128, NBLK, NQ1]), op0=ALU.mult, op1=ALU.add), [be.b, qp1.b], [f1.b])
        D_(lambda e: e.tensor_tensor(out=f1[:], in0=f1[:], in1=big[:].unsqueeze(2).to_broadcast([128, NBLK, NQ1]), op=ALU.add), [f1.b, big.b], [f1.b])
        D_(lambda e: e.tensor_copy(out=R["IDX1"][:], in_=f1[:]), [f1.b], [R["IDX1"].b])
        D_(lambda e: e.scalar_tensor_tensor(out=f2[:], in0=be[:].unsqueeze(2).to_broadcast([128, NBLK, FE * 2]), scalar=float(FE * 2 * 128),
                                            in1=qp2[:].unsqueeze(1).to_broadcast([128, NBLK, FE * 2]), op0=ALU.mult, op1=ALU.add), [be.b, qp2.b], [f2.b])
        D_(lambda e: e.tensor_tensor(out=f2[:], in0=f2[:], in1=big[:].unsqueeze(2).to_broadcast([128, NBLK, FE * 2]), op=ALU.add), [f2.b, big.b], [f2.b])
        D_(lambda e: e.tensor_copy(out=R["IDX2"][:], in_=f2[:]), [f2.b], [R["IDX2"].b])
        P_(lambda e: e.memset(mcum[:], 0.0), [], [mcum.b])
        for i in range(NTO):
            p = ps[i % 2]
            S.op("pe", lambda e: e.matmul(p[:, 0:NE], lhsT=triu[:], rhs=M[:, i, :], start=True, stop=False), reads=[triu.b, M.b], writes=[p.b])
            S.op("pe", lambda e: e.matmul(p[:, 0:NE], lhsT=ones[:], rhs=mcum[:], start=False, stop=True), reads=[ones.b, mcum.b], writes=[p.b])
            D_(lambda e: e.tensor_tensor(out=dest[:], in0=p[:, 0:NE], in1=poff[:], op=ALU.add), [p.b, poff.b], [dest.b])
            D_(lambda e: e.tensor_tensor(out=mcum[:], in0=mcum[:], in1=M[:, i, :], op=ALU.add), [mcum.b, M.b], [mcum.b])
            for a in range(2):
                D_(lambda e: e.tensor_scalar(out=oh[:], in0=ioe[:], scalar1=RT[:, i, a:a + 1], scalar2=None, op0=ALU.is_equal), [ioe.b, RT.b], [oh.b])
                D_(lambda e: e.tensor_tensor(out=oh[:], in0=oh[:], in1=dest[:], op=ALU.mult), [oh.b, dest.b], [oh.b])
                D_(lambda e: e.tensor_reduce(out=dd[:, i, a:a + 1], in_=oh[:], axis=AX.X, op=ALU.add), [oh.b], [dd.b])
        D_(lambda e: e.tensor_copy(out=R["DEST"][:], in_=dd[:]), [dd.b], [R["DEST"].b])
        for i in range(NTO):
            for a in range(2):
                S.dma("pool", lambda e: e.indirect_dma_start(out=SLOT, out_offset=bass.IndirectOffsetOnAxis(ap=R["DEST"][:, i, a:a + 1], axis=0),
                                                             in_=toki[:, i, :], in_offset=None), reads=[R["DEST"].b, toki.b], writes=[b_SLOT])
    S.barrier()


def phase_D(kb, c, R, SLOT, b_SLOT, X2, b_X2, w1L, w3L, w2L, Y, b_Y):
    cfg, S = kb.cfg, kb.S
    D, KD, DE, FE, NBLK = cfg.D, cfg.KD, cfg.DE, cfg.FE, cfg.NBLK
    KQ = min(4, KD)
    NQ1 = KD // KQ
    DH = D // 2
    CW = min(512, DH)
    ech = [(o, min(512, DE - o)) for o in range(0, DE, 512)]
    with ExitStack() as es:
        mk = lambda name, shape, dt=F32: T(kb.sb(es, "M_" + name, shape, dt))
        tokall = mk("tokall", [128, NBLK, 16], I32)
        xg = [mk("xg%d" % i, [128, D], BF16) for i in range(2)]
        xbT = mk("xbT", [128, KD, 128], BF16)
        NWB = 4
        wb = [[mk("wb%d_%d" % (a, i), [128, KQ, DE], BF16) for i in range(NWB)] for a in range(2)]
        w2b = [mk("w2b%d" % i, [128, FE, DH], BF16) for i in range(3)]
        actf = mk("actf", [128, DE]); actb = mk("actb", [128, DE], BF16); actT = mk("actT", [128, FE, 128], BF16)
        ysts = [mk("yst%d" % i, [128, D]) for i in range(2)]
        psT = [T(kb.ps(es, "M_psT%d" % i, [128, 1024], BF16), excl=True) for i in range(2)]
        ph = [[T(kb.ps(es, "M_ph%d_%d" % (a, i), [128, 512]), excl=True) for i in range(len(ech))] for a in range(2)]
        py = [T(kb.ps(es, "M_py%d" % i, [128, 512]), excl=True) for i in range(2)]
        IDX1, IDX2 = R["IDX1"], R["IDX2"]
        bc1 = kb.nc.gpsimd.to_reg(cfg.NE * NQ1 * 128 - 1)
        bc2 = kb.nc.gpsimd.to_reg(cfg.NE * FE * 2 * 128 - 1)
        for t_ in wb[0] + wb[1] + w2b:
            S.op("pool", lambda e: e.memset(t_[:], 0.0), writes=[t_.b])
        w2c = 0
        pcnt = [0]
        cst = 0
        yc = 0
        S.dma("sp", lambda e: e.dma_start(out=tokall[:], in_=SLOT.rearrange("(j p) c -> p j c", p=128)), reads=[b_SLOT], writes=[tokall.b])
        for j in range(NBLK):
            x_ = xg[j % 2]
            yst = ysts[j % 2]
            S.dma("pool", lambda e: e.indirect_dma_start(out=x_[:], out_offset=None, in_=X2, in_offset=bass.IndirectOffsetOnAxis(ap=tokall[:, j, 0:1], axis=0)),
                  reads=[tokall.b, b_X2], writes=[x_.b])
            for ti in range(1):
                transpose_bf(kb, c, S, x_, KD, xbT, 0, psT, pcnt)
            for q in range(NQ1):
                wq = (j * NQ1 + q) % NWB
                for a, wl in enumerate((w1L, w3L)):
                    b_ = wb[a][wq]
                    S.dma("pool", lambda e: e.indirect_dma_start(out=b_[:].rearrange("p a b -> p (a b)"), out_offset=None, in_=wl,
                                                                 in_offset=bass.IndirectOffsetOnAxis(ap=IDX1[:, j, q:q + 1], axis=0),
                                                                 bounds_check=bc1, oob_is_err=False),
                          reads=[IDX1.b], writes=[b_.b])
                for kk in range(KQ):
                    k = q * KQ + kk
                    for a in range(2):
                        for ci_, (o, wdt) in enumerate(ech):
                            S.op("pe", lambda e: e.matmul(ph[a][ci_][:, 0:wdt], lhsT=xbT[:, k, :], rhs=wb[a][wq][:, kk, o:o + wdt], start=(k == 0), stop=(k == KD - 1)),
                                 reads=[xbT.b, wb[a][wq].b], writes=[ph[a][ci_].b])
            for ci_, (o, wdt) in enumerate(ech):
                S.op("act", lambda e: e.activation(out=actf[:, o:o + wdt], in_=ph[0][ci_][:, 0:wdt], func=AF.Silu), reads=[ph[0][ci_].b], writes=[actf.b])
                S.op("dve", lambda e: e.tensor_tensor(out=actb[:, o:o + wdt], in0=actf[:, o:o + wdt], in1=ph[1][ci_][:, 0:wdt], op=ALU.mult),
                     reads=[actf.b, ph[1][ci_].b], writes=[actb.b])
            transpose_bf(kb, c, S, actb, FE, actT, 0, psT, pcnt)
            for hf in range(2):
                w2b_ = w2b[w2c % 3]; w2c += 1
                for f in range(FE):
                    col = f * 2 + hf
                    S.dma("pool", lambda e: e.indirect_dma_start(out=w2b_[:, f, :], out_offset=None, in_=w2L, in_offset=bass.IndirectOffsetOnAxis(ap=IDX2[:, j, col:col + 1], axis=0),
                                                                 bounds_check=bc2, oob_is_err=False),
                          reads=[IDX2.b], writes=[w2b_.b])
                for cc in range(DH // CW):
                    p = py[yc % 2]; yc += 1
                    for f in range(FE):
                        S.op("pe", lambda e: e.matmul(p[:, 0:CW], lhsT=actT[:, f, :], rhs=w2b_[:, f, cc * CW:(cc + 1) * CW], start=(f == 0), stop=(f == FE - 1)),
                             reads=[actT.b, w2b_.b], writes=[p.b])
                    o0 = hf * DH + cc * CW
                    if yc % 2:
                        S.op("act", lambda e: e.copy(out=yst[:, o0:o0 + CW], in_=p[:, 0:CW]), reads=[p.b], writes=[yst.b])
                    else:
                        S.op("dve", lambda e: e.tensor_copy(out=yst[:, o0:o0 + CW], in_=p[:, 0:CW]), reads=[p.b], writes=[yst.b])
            S.dma("sp", lambda e: e.dma_start(out=Y[j * 128:(j + 1) * 128, :], in_=yst[:]), reads=[yst.b], writes=[b_Y])
    S.barrier()


def phase_E(kb, c, R, XMID, b_XMID, Y, b_Y, fgd, out, b_out):
    cfg, S = kb.cfg, kb.S
    D = cfg.D
    NTO = cfg.OWN // 128
    with ExitStack() as es:
        mk = lambda name, shape, dt=F32: T(kb.sb(es, "E_" + name, shape, dt))
        xm = [mk("xm%d" % i, [128, D]) for i in range(2)]
        ya = [mk("ya%d" % i, [128, D]) for i in range(2)]
        yb = [mk("yb%d" % i, [128, D]) for i in range(2)]
        sq = mk("sq", [128, D]); ss = mk("ss", [128, 1]); fg = mk("fg", [128, D])
        S.dma("sp", lambda e: e.dma_start(out=fg[:], in_=fgd.partition_broadcast(128)), writes=[fg.b])
        RT, DEST = R["RT"], R["DEST"]
        for i in range(NTO):
            r0 = i * 128
            x_, a_, b_ = xm[i % 2], ya[i % 2], yb[i % 2]
            S.dma("sp", lambda e: e.dma_start(out=x_[:], in_=XMID[r0:r0 + 128, :]), reads=[b_XMID], writes=[x_.b])
            S.dma("pool", lambda e: e.indirect_dma_start(out=a_[:], out_offset=None, in_=Y, in_offset=bass.IndirectOffsetOnAxis(ap=DEST[:, i, 0:1], axis=0)),
                  reads=[DEST.b, b_Y], writes=[a_.b])
            S.dma("pool", lambda e: e.indirect_dma_start(out=b_[:], out_offset=None, in_=Y, in_offset=bass.IndirectOffsetOnAxis(ap=DEST[:, i, 1:2], axis=0)),
                  reads=[DEST.b, b_Y], writes=[b_.b])
            S.op("dve", lambda e: e.scalar_tensor_tensor(out=x_[:], in0=a_[:], scalar=RT[:, i, 2:3], in1=x_[:], op0=ALU.mult, op1=ALU.add),
                 reads=[a_.b, RT.b, x_.b], writes=[x_.b])
            S.op("dve", lambda e: e.scalar_tensor_tensor(out=x_[:], in0=b_[:], scalar=RT[:, i, 3:4], in1=x_[:], op0=ALU.mult, op1=ALU.add),
                 reads=[b_.b, RT.b, x_.b], writes=[x_.b])
            S.op("act", lambda e: e.activation(out=sq[:], in_=x_[:], func=AF.Square, accum_out=ss[:]), reads=[x_.b], writes=[sq.b, ss.b])
            rmsnorm_rstd(kb, c, ss[:], ss.b, D)
            S.op("dve", lambda e: e.scalar_tensor_tensor(out=a_[:], in0=x_[:], scalar=ss[:, 0:1], in1=fg[:], op0=ALU.mult, op1=ALU.mult),
                 reads=[x_.b, ss.b, fg.b, a_.b], writes=[a_.b])
            S.dma("sp", lambda e: e.dma_start(out=out[r0:r0 + 128, :], in_=a_[:]), reads=[a_.b], writes=[b_out])
    S.barrier()


def build_program(cfg, debug=(), phases="ABCDE"):
    kb = K(cfg, debug)
    nc = kb.nc
    D, S_, KD, OWN, NE, NG = cfg.D, cfg.S, cfg.KD, cfg.OWN, cfg.NE, cfg.NG
    NTO = OWN // 128
    KQ = min(4, KD); NQ1 = KD // KQ; FE = cfg.FE
    xb = kb.inp("xb", [S_, D])
    xo = kb.inp("xo", [OWN, D])
    own_idx = kb.inp("own_idx", [128, NTO], I32)
    g1d = kb.inp("norm1_g", [1, D])
    wA = kb.inp("wA", [cfg.NA, 128, KD, 128])
    wG = kb.inp("wG", [2 * D // 512, KD // cfg.KP, 128, cfg.KP, 512])
    gbd = kb.inp("gate_b", [1, 2 * D])
    prm = {}
    for name, shape in (("rwp", [64, 10, cfg.RWH]), ("lmu", [128, 4]), ("rw_w2", [96, cfg.RW]), ("rw_a2", [96, cfg.RW]),
                        ("rw_g2", [256, cfg.RW]), ("dnp", [128, 12, cfg.DNH]), ("dnw", [128, 1]), ("dnh", [16, 2])):
        prm[name] = kb.inp(name, shape)
    wBR = kb.inp("wBR", [D // 512, 128, cfg.KB, 512])
    wO = kb.inp("wO", [D // 512, 128, KD, 512])
    g2d = kb.inp("norm2_g", [1, D])
    fgd = kb.inp("final_g", [1, D])
    wRd = kb.inp("wR", [128, KD, NG + NE])
    bRd = kb.inp("bR", [1, NG + NE])
    w1L = kb.inp("w1L", [NE * NQ1 * 128, KQ * cfg.DE])
    w3L = kb.inp("w3L", [NE * NQ1 * 128, KQ * cfg.DE])
    w2L = kb.inp("w2L", [NE * FE * 2 * 128, D // 2])
    PT = kb.scratch("PT", [cfg.NA * 128, S_]); b_PT = [Buf() for _ in range(cfg.NA)]
    OMIX = kb.scratch("OMIX", [S_, cfg.BW], BF16); b_OMIX = Buf()
    GATES = kb.scratch("GATES", [OWN, 2 * D]); b_G = Buf()
    XMID = kb.scratch("XMID", [OWN, D]); b_XMID = Buf()
    X2 = kb.scratch("X2", [OWN + 128, D], BF16); b_X2 = Buf()
    SLOT = kb.scratch("SLOT", [cfg.NBLK * 128, 16], I32); b_SLOT = Buf()
    Y = kb.scratch("Y", [cfg.NBLK * 128, D]); b_Y = Buf()
    out = nc.dram_tensor("out", [OWN, D], F32, kind="ExternalOutput").ap(); b_out = Buf()
    with ExitStack() as es:
        kb.S = Sched(nc, es)
        S = kb.S
        c = build_consts(kb, es)
        R = {}
        R["M"] = T(kb.sb(es, "r_M", [128, NTO, NE])); R["RT"] = T(kb.sb(es, "r_RT", [128, NTO, 4])); R["ioe"] = T(kb.sb(es, "r_ioe", [128, NE]))
        R["IDX1"] = T(kb.sb(es, "r_IDX1", [128, cfg.NBLK, NQ1], I32)); R["IDX2"] = T(kb.sb(es, "r_IDX2", [128, cfg.NBLK, FE * 2], I32))
        R["DEST"] = T(kb.sb(es, "r_DEST", [128, NTO, 2], I32))
        S.op("pool", lambda e: e.iota(R["ioe"][:], pattern=[[1, NE]], base=0, channel_multiplier=0, allow_small_or_imprecise_dtypes=True), writes=[R["ioe"].b])
        if "A" in phases:
            phase_A(kb, c, xb, g1d, wA, PT, b_PT)
            phase_A1(kb, c, xo, g1d, wG, gbd, GATES, b_G)
        if "B" in phases:
            phase_B(kb, c, PT, b_PT, prm, OMIX, b_OMIX)
        if "C" in phases:
            phase_C1(kb, c, xo, own_idx, OMIX, b_OMIX, GATES, b_G, wBR, wO, XMID, b_XMID)
            phase_C2(kb, c, XMID, b_XMID, g2d, wRd, bRd, X2, b_X2, R)
        if "D" in phases:
            phase_D0(kb, c, R, SLOT, b_SLOT)
            phase_D(kb, c, R, SLOT, b_SLOT, X2, b_X2, w1L, w3L, w2L, Y, b_Y)
        if "E" in phases:
            phase_E(kb, c, R, XMID, b_XMID, Y, b_Y, fgd, out, b_out)
        S.barrier()
        print("ninstr", S.ninstr)
    return kb


def prep_shared(cfg, inp):
    sh = {}
    D, KD, RW, DN, RWH, DNH, NE, NG, DE, FE = cfg.D, cfg.KD, cfg.RW, cfg.DN, cfg.RWH, cfg.DNH, cfg.NE, cfg.NG, cfg.DE, cfg.FE
    w_in = inp["w_in"][0]
    ca = cfg.colsA()
    wa = np.zeros((D, cfg.NA * 128), np.float32)
    m = ca >= 0
    wa[:, m] = w_in[:, ca[m]]
    sh["wA"] = np.ascontiguousarray(wa.reshape(KD, 128, cfg.NA, 128).transpose(2, 1, 0, 3))
    del wa
    wg = w_in[:, cfg.GB0:cfg.GB0 + 2 * D]
    KP = cfg.KP
    sh["wG"] = np.ascontiguousarray(wg.reshape(KD // KP, KP, 128, 2 * D // 512, 512).transpose(3, 0, 2, 1, 4))
    sh["gate_b"] = np.ascontiguousarray(inp["gate_b"].reshape(1, 2 * D))
    sh["norm1_g"] = np.ascontiguousarray(inp["norm1_g"].reshape(1, D))
    sh["norm2_g"] = np.ascontiguousarray(inp["norm2_g"].reshape(1, D))
    sh["final_g"] = np.ascontiguousarray(inp["final_g"].reshape(1, D))
    mu = inp["rw_mu"][0]
    vecs = [mu[0:RW], mu[RW + 96:2 * RW + 96], mu[2 * RW + 96:3 * RW + 96], inp["rw_w0"][0], inp["rw_a0"][0], inp["rw_k_k"][0],
            inp["rw_k_a"][0], inp["rw_r_k"][0].reshape(-1), inp["rw_ln_w"][0], inp["rw_ln_b"][0]]
    sh["rwp"] = np.ascontiguousarray(np.stack([v.reshape(RWH, 64).T for v in vecs], axis=1).astype(np.float32))
    lmu = np.zeros((128, 4), np.float32)
    lmu[0:96, 0] = mu[RW:RW + 96]
    lmu[0:96, 1] = mu[3 * RW + 96:3 * RW + 192]
    lmu[:, 2] = mu[3 * RW + 192:3 * RW + 320]
    lmu[:, 3] = mu[3 * RW + 320:3 * RW + 448]
    sh["lmu"] = lmu
    sh["rw_w2"] = np.ascontiguousarray(inp["rw_w2"][0])
    sh["rw_a2"] = np.ascontiguousarray(inp["rw_a2"][0])
    sh["rw_g2"] = np.ascontiguousarray(inp["rw_g2"][0])
    cw = inp["dn_conv_w"][0]
    sh["dnp"] = np.ascontiguousarray(cw.reshape(4, 3, DNH, 128).transpose(3, 0, 1, 2).reshape(128, 12, DNH))
    sh["dnw"] = np.ascontiguousarray(inp["dn_norm_w"][0].reshape(128, 1))
    dnh = np.zeros((16, 2), np.float32)
    dnh[:DNH, 0] = inp["dn_a_log"][0]
    dnh[:DNH, 1] = inp["dn_dt_bias"][0]
    sh["dnh"] = dnh
    wb = inp["w_branch"][0]
    sh["wBR"] = np.ascontiguousarray(wb.reshape(cfg.KB, 128, D // 512, 512).transpose(2, 1, 0, 3))
    wo = inp["w_out"][0]
    sh["wO"] = np.ascontiguousarray(wo.reshape(KD, 128, D // 512, 512).transpose(2, 1, 0, 3))
    wr = np.concatenate([inp["moe_gr_w"][0], inp["moe_er_w"][0]], axis=1)
    sh["wR"] = np.ascontiguousarray(wr.reshape(KD, 128, NG + NE).transpose(1, 0, 2))
    sh["bR"] = np.ascontiguousarray(np.concatenate([inp["moe_gr_b"][0], inp["moe_er_b"][0]]).reshape(1, NG + NE))
    KQ = min(4, KD); NQ1 = KD // KQ
    for nm, key in (("w1L", "moe_w1"), ("w3L", "moe_w3")):
        w = inp[key][0]
        sh[nm] = np.ascontiguousarray(w.reshape(NE, NQ1, KQ, 128, DE).transpose(0, 1, 3, 2, 4)).reshape(NE * NQ1 * 128, KQ * DE)
    w = inp["moe_w2"][0]
    sh["w2L"] = np.ascontiguousarray(w.reshape(NE, FE, 128, 2, D // 2).transpose(0, 1, 3, 2, 4)).reshape(NE * FE * 2 * 128, D // 2)
    return sh


def core_inputs(cfg, inp, sh, b, s):
    d = dict(sh)
    OWN = cfg.OWN
    d["xb"] = np.ascontiguousarray(inp["x"][b])
    d["xo"] = np.ascontiguousarray(inp["x"][b, s * OWN:(s + 1) * OWN])
    d["own_idx"] = np.ascontiguousarray((s * OWN + np.arange(OWN, dtype=np.int32)).reshape(OWN // 128, 128).T)
    return d


_CACHE = {}


def kernel(**inputs):
    cfg = Cfg()
    inp = {k: np.asarray(v) for k, v in inputs.items()}
    if "kb" not in _CACHE:
        _CACHE["kb"] = build_program(cfg)
    kb = _CACHE["kb"]
    sh = prep_shared(cfg, inp)
    B = inp["x"].shape[0]
    ins = []
    for cid in range(8):
        b, s = cid // 2, cid % 2
        d = core_inputs(cfg, inp, sh, b % B, s)
        ins.append({k: v for k, v in d.items() if k in kb.din})
    res = run_bass_kernel_spmd(kb.nc, ins, core_ids=list(range(8)))
    out = np.zeros((B, cfg.S, cfg.D), np.float32)
    for cid in range(8):
        b, s = cid // 2, cid % 2
        out[b, s * cfg.OWN:(s + 1) * cfg.OWN] = res.results[cid]["out"]
    return out
```
